# Optimizing a Trainium2 kernel written in Bass

```python
import math
import jax, jax.numpy as jnp
from jax import lax
import numpy as np

D_MODEL = 4096
BATCH = 1
SEQ = 16384
DEPTH = 2

HA = 16
QK_NOPE = 128
QK_ROPE = 64
V_DIM_A = 128
Q_LORA = 1024
KV_LORA = 512
ROPE_THETA = 10000.0
HB = 8
DH_B = 128
N_GROUPS = 8
EXP_PER_GROUP = 8
N_EXPERTS = N_GROUPS * EXP_PER_GROUP
TOP_K = 2
D_EXPERT = 384
MOE_BLOCK = 128
Q_BLOCK = 128
EPS = 1e-6

QA_W = HA * (QK_NOPE + QK_ROPE)
KVA_W = HA * (QK_NOPE + V_DIM_A)
WIDTH_A = HA * V_DIM_A
WIDTH_B = HB * 2 * DH_B
IN_WIDTHS = (Q_LORA, KV_LORA, QK_ROPE, WIDTH_B, WIDTH_B, WIDTH_B, D_MODEL, D_MODEL)
D_IN = sum(IN_WIDTHS)

kernel_name = "hybrid_mla_diffattn_hiermoe"


def rms_norm(x, g, eps=EPS):
    xf = x.astype(jnp.float32)
    y = xf * lax.rsqrt(jnp.mean(xf * xf, axis=-1, keepdims=True) + eps)
    return (y * g.astype(jnp.float32)).astype(x.dtype)


def split_indices(widths):
    idx, acc = [], 0
    for w in widths[:-1]:
        acc += w
        idx.append(acc)
    return idx


def apply_rope(x, pos):
    half = x.shape[-1] // 2
    inv = ROPE_THETA ** (-jnp.arange(half, dtype=jnp.float32) / half)
    ang = pos.astype(jnp.float32)[..., None] * inv
    cos = jnp.cos(ang)[:, :, None, :]
    sin = jnp.sin(ang)[:, :, None, :]
    xf = x.astype(jnp.float32)
    x1, x2 = xf[..., :half], xf[..., half:]
    return jnp.concatenate([x1 * cos - x2 * sin, x1 * sin + x2 * cos], axis=-1).astype(x.dtype)


def alibi_slopes(n):
    return jnp.asarray(2.0 ** (-8.0 * (np.arange(n) + 1) / n), dtype=jnp.float32)


def mla_attention(q, k, v):
    B, S, H, Dqk = q.shape
    Dv = v.shape[-1]
    nb = S // Q_BLOCK
    scale = Dqk ** -0.5
    kidx = jnp.arange(S)

    def block(i):
        start = i * Q_BLOCK
        qb = lax.dynamic_slice_in_dim(q, start, Q_BLOCK, axis=1)
        s = jnp.einsum('bqhd,bkhd->bhqk', qb, k, preferred_element_type=jnp.float32) * scale
        qidx = start + jnp.arange(Q_BLOCK)
        s = jnp.where(kidx[None, :] <= qidx[:, None], s, -jnp.inf)
        p = jax.nn.softmax(s, axis=-1).astype(v.dtype)
        return jnp.einsum('bhqk,bkhd->bqhd', p, v)

    out = lax.map(block, jnp.arange(nb))
    return jnp.moveaxis(out, 0, 1).reshape(B, S, H, Dv)


def diff_attention(q, k, v, pos, lam, slopes):
    B, S, H, _, d = q.shape
    Dv = v.shape[-1]
    nb = S // Q_BLOCK
    scale = d ** -0.5
    kidx = jnp.arange(S)

    def block(i):
        start = i * Q_BLOCK
        qb = lax.dynamic_slice_in_dim(q, start, Q_BLOCK, axis=1)
        pq = lax.dynamic_slice_in_dim(pos, start, Q_BLOCK, axis=1)
        s = jnp.einsum('bqhcd,bkhcd->bchqk', qb, k, preferred_element_type=jnp.float32) * scale
        dist = jnp.abs(pq[:, :, None] - pos[:, None, :]).astype(jnp.float32)
        s = s - slopes[None, None, :, None, None] * dist[:, None, None]
        qidx = start + jnp.arange(Q_BLOCK)
        s = jnp.where(kidx[None, :] <= qidx[:, None], s, -jnp.inf)
        p = jax.nn.softmax(s, axis=-1)
        a = p[:, 0] - lam * p[:, 1]
        return jnp.einsum('bhqk,bkhd->bqhd', a.astype(v.dtype), v)

    out = lax.map(block, jnp.arange(nb))
    return jnp.moveaxis(out, 0, 1).reshape(B, S, H, Dv)


def hier_moe(h, w_rg, b_rg, w_re, b_re, w1, w3, w2):
    B, S, D = h.shape
    T = B * S
    ht = h.reshape(T, D)
    hf = ht.astype(jnp.float32)
    g_prob = jax.nn.softmax(hf @ w_rg.astype(jnp.float32) + b_rg.astype(jnp.float32), axis=-1)
    p_g, g_idx = lax.top_k(g_prob, 1)
    e_logit = (hf @ w_re.astype(jnp.float32) + b_re.astype(jnp.float32)).reshape(T, N_GROUPS, EXP_PER_GROUP)
    e_logit = jnp.take_along_axis(e_logit, g_idx[:, :, None], axis=1)[:, 0]
    p_e, e_loc = lax.top_k(jax.nn.softmax(e_logit, axis=-1), TOP_K)
    gate = p_g * p_e / jnp.sum(p_e, axis=-1, keepdims=True)
    expert = g_idx * EXP_PER_GROUP + e_loc

    A = T * TOP_K
    flat_e = expert.reshape(A)
    flat_tok = (jnp.arange(A) // TOP_K).astype(jnp.int32)
    flat_w = gate.reshape(A)
    order = jnp.argsort(flat_e)
    se, stok, sw = flat_e[order], flat_tok[order], flat_w[order]
    counts = jnp.bincount(flat_e, length=N_EXPERTS)
    padded = (counts + MOE_BLOCK - 1) // MOE_BLOCK * MOE_BLOCK
    pad_end = jnp.cumsum(padded)
    pad_start = pad_end - padded
    seg_start = jnp.cumsum(counts) - counts
    dest = pad_start[se] + jnp.arange(A) - seg_start[se]
    NB = -(-A // MOE_BLOCK) + N_EXPERTS
    P = NB * MOE_BLOCK
    buf_tok = jnp.full((P,), T, dtype=jnp.int32).at[dest].set(stok)
    buf_w = jnp.zeros((P,), jnp.float32).at[dest].set(sw)
    blk_exp = jnp.clip(jnp.searchsorted(pad_end, jnp.arange(NB) * MOE_BLOCK, side='right'), 0, N_EXPERTS - 1)
    ht_pad = jnp.concatenate([ht, jnp.zeros((1, D), ht.dtype)], axis=0)

    def step(acc, blk):
        tok, w, e = blk
        xb = ht_pad[tok]
        yb = (jax.nn.silu(xb @ w1[e]) * (xb @ w3[e])) @ w2[e]
        return acc.at[tok].add(yb * w[:, None].astype(yb.dtype)), None

    acc, _ = lax.scan(step, jnp.zeros((T + 1, D), ht.dtype),
                      (buf_tok.reshape(NB, MOE_BLOCK), buf_w.reshape(NB, MOE_BLOCK), blk_exp))
    return acc[:T].reshape(B, S, D)


def setup_inputs(seed: int = 0) -> dict:
    key = jax.random.key(seed)
    ks = jax.random.split(key, 32)
    L = DEPTH

    def nrm(k, shape, scale):
        return jax.random.normal(k, shape, jnp.float32) * scale

    def gain(k, shape):
        return 1.0 + 0.05 * jax.random.normal(k, shape, jnp.float32)

    start = jax.random.randint(ks[1], (BATCH, 1), 0, 1024, dtype=jnp.int32)
    positions = (start + jnp.arange(SEQ, dtype=jnp.int32)[None, :]).astype(jnp.int32)
    return {
        "x": nrm(ks[0], (BATCH, SEQ, D_MODEL), 1.0),
        "positions": positions,
        "norm_attn": gain(ks[2], (L, D_MODEL)),
        "w_in": nrm(ks[3], (L, D_MODEL, D_IN), D_MODEL ** -0.5),
        "q_norm": gain(ks[4], (L, Q_LORA)),
        "w_uq": nrm(ks[5], (L, Q_LORA, QA_W), Q_LORA ** -0.5),
        "kv_norm": gain(ks[6], (L, KV_LORA)),
        "w_ukv": nrm(ks[7], (L, KV_LORA, KVA_W), KV_LORA ** -0.5),
        "lam_q1": nrm(ks[8], (L, DH_B), 0.1),
        "lam_k1": nrm(ks[9], (L, DH_B), 0.1),
        "lam_q2": nrm(ks[10], (L, DH_B), 0.1),
        "lam_k2": nrm(ks[11], (L, DH_B), 0.1),
        "subln": gain(ks[12], (L, 2 * DH_B)),
        "w_oa": nrm(ks[13], (L, WIDTH_A, D_MODEL), WIDTH_A ** -0.5),
        "w_ob": nrm(ks[14], (L, WIDTH_B, D_MODEL), WIDTH_B ** -0.5),
        "w_out": nrm(ks[15], (L, D_MODEL, D_MODEL), D_MODEL ** -0.5),
        "norm_ffn": gain(ks[16], (L, D_MODEL)),
        "w_router_g": nrm(ks[17], (L, D_MODEL, N_GROUPS), D_MODEL ** -0.5),
        "b_router_g": nrm(ks[18], (L, N_GROUPS), 0.01),
        "w_router_e": nrm(ks[19], (L, D_MODEL, N_EXPERTS), D_MODEL ** -0.5),
        "b_router_e": nrm(ks[20], (L, N_EXPERTS), 0.01),
        "w1": nrm(ks[21], (L, N_EXPERTS, D_MODEL, D_EXPERT), D_MODEL ** -0.5),
        "w3": nrm(ks[22], (L, N_EXPERTS, D_MODEL, D_EXPERT), D_MODEL ** -0.5),
        "w2": nrm(ks[23], (L, N_EXPERTS, D_EXPERT, D_MODEL), D_EXPERT ** -0.5),
        "norm_final": gain(ks[24], (D_MODEL,)),
    }


def reference(x, positions, norm_attn, w_in, q_norm, w_uq, kv_norm, w_ukv, lam_q1, lam_k1, lam_q2, lam_k2,
              subln, w_oa, w_ob, w_out, norm_ffn, w_router_g, b_router_g, w_router_e, b_router_e,
              w1, w3, w2, norm_final):
    B, S, _ = x.shape
    slopes = alibi_slopes(HB)
    idx = split_indices(IN_WIDTHS)
    for l in range(DEPTH):
        h = rms_norm(x, norm_attn[l])
        proj = h @ w_in[l]
        c_q, c_kv, k_r, q_b, k_b, v_b, g_a, g_b = jnp.split(proj, idx, axis=-1)

        qa = (rms_norm(c_q, q_norm[l]) @ w_uq[l]).reshape(B, S, HA, QK_NOPE + QK_ROPE)
        q_rot = apply_rope(qa[..., QK_NOPE:], positions)
        kv = (rms_norm(c_kv, kv_norm[l]) @ w_ukv[l]).reshape(B, S, HA, QK_NOPE + V_DIM_A)
        k_nope, v_a = kv[..., :QK_NOPE], kv[..., QK_NOPE:]
        k_rot = jnp.broadcast_to(apply_rope(k_r[:, :, None, :], positions), (B, S, HA, QK_ROPE))
        q_a = jnp.concatenate([qa[..., :QK_NOPE], q_rot], axis=-1)
        k_a = jnp.concatenate([k_nope, k_rot], axis=-1)
        o_a = mla_attention(q_a, k_a, v_a).reshape(B, S, WIDTH_A)

        lam_init = 0.8 - 0.6 * math.exp(-0.3 * l)
        lam = (jnp.exp(jnp.sum(lam_q1[l].astype(jnp.float32) * lam_k1[l].astype(jnp.float32)))
               - jnp.exp(jnp.sum(lam_q2[l].astype(jnp.float32) * lam_k2[l].astype(jnp.float32))) + lam_init)
        o_b = diff_attention(q_b.reshape(B, S, HB, 2, DH_B), k_b.reshape(B, S, HB, 2, DH_B),
                             v_b.reshape(B, S, HB, 2 * DH_B), positions, lam, slopes)
        o_b = (rms_norm(o_b, subln[l]) * (1.0 - lam_init)).reshape(B, S, WIDTH_B)

        merged = jax.nn.sigmoid(g_a) * (o_a @ w_oa[l]) + jax.nn.sigmoid(g_b) * (o_b @ w_ob[l])
        x = x + merged @ w_out[l]

        h = rms_norm(x, norm_ffn[l])
        x = x + hier_moe(h, w_router_g[l], b_router_g[l], w_router_e[l], b_router_e[l], w1[l], w3[l], w2[l])
    return rms_norm(x, norm_final)
```

```python
import numpy as np
import concourse.bass as bass
import concourse.mybir as mybir
from concourse.bass_utils import run_bass_kernel_spmd

F32 = mybir.dt.float32
BF16 = mybir.dt.bfloat16
I32 = mybir.dt.int32
AF = mybir.ActivationFunctionType
ALU = mybir.AluOpType
AX = mybir.AxisListType


class V:
    def __init__(self, ap, buf):
        self.ap = ap
        self.buf = buf

    def __getitem__(self, k):
        return V(self.ap[k], self.buf)

    def rearrange(self, *a, **kw):
        return V(self.ap.rearrange(*a, **kw), self.buf)


class Buf:
    def __init__(self, ap, name):
        self.base = ap
        self.name = name
        self.lw = None
        self.rd = {}

    def __getitem__(self, k):
        return V(self.base[k], self)

    @property
    def v(self):
        return V(self.base, self)


NDMA = 4


class KB:
    def __init__(self, name="k"):
        self.nc = bass.Bass("TRN2", target_bir_lowering=False)
        self.ops = []
        self.nbuf = 0

    def sb(self, shape, dtype, name=None):
        self.nbuf += 1
        name = name or f"sb{self.nbuf}"
        t = self.nc.alloc_sbuf_tensor(name, list(shape), dtype)
        return Buf(t[:], name)

    def ps(self, shape, dtype=F32, name=None):
        self.nbuf += 1
        name = name or f"ps{self.nbuf}"
        t = self.nc.alloc_psum_tensor(name, list(shape), dtype)
        return Buf(t[:], name)

    def dram(self, name, shape, dtype, kind="Internal"):
        t = self.nc.dram_tensor(name, list(shape), dtype, kind=kind)
        return Buf(t.ap(), name)

    def op(self, eng, fn, reads, writes, dma=False):
        rb = [x.buf for x in reads if isinstance(x, V)]
        wb = [x.buf for x in writes if isinstance(x, V)]
        self.ops.append((eng, fn, rb, wb, dma))

    @staticmethod
    def _a(x):
        return x.ap if isinstance(x, V) else x

    def mm(self, out, lhsT, rhs, start=True, stop=True):
        a = self._a
        self.op("pe", lambda q: q.matmul(a(out), a(lhsT), a(rhs), start=start, stop=stop),
                [lhsT, rhs] + ([] if start else [out]), [out])

    def tr(self, out, in_, ident):
        a = self._a
        self.op("pe", lambda q: q.transpose(a(out), a(in_), a(ident)), [in_, ident], [out])

    def act(self, out, in_, func, bias=None, scale=1.0, accum_out=None, eng="act"):
        a = self._a
        kw = {}
        if bias is not None:
            kw["bias"] = a(bias)
        if accum_out is not None:
            kw["accum_out"] = a(accum_out)
        self.op(eng, lambda q: q.activation(a(out), a(in_), func, scale=a(scale), **kw),
                [in_, bias, scale], [out, accum_out])

    def tt(self, eng, out, in0, in1, op):
        a = self._a
        self.op(eng, lambda q: q.tensor_tensor(a(out), a(in0), a(in1), op), [in0, in1], [out])

    def ts(self, eng, out, in0, s1, s2, op0, op1=None, accum_out=None):
        a = self._a
        kw = {}
        if op1 is not None:
            kw["op1"] = op1
        if accum_out is not None:
            kw["accum_out"] = a(accum_out)
        self.op(eng, lambda q: q.tensor_scalar(a(out), a(in0), a(s1), a(s2) if s2 is not None else None, op0, **kw),
                [in0, s1, s2], [out, accum_out])

    def stt(self, eng, out, in0, scalar, in1, op0, op1, accum_out=None):
        a = self._a
        kw = {}
        if accum_out is not None:
            kw["accum_out"] = a(accum_out)
        self.op(eng, lambda q: q.scalar_tensor_tensor(a(out), a(in0), a(scalar), a(in1), op0, op1, **kw),
                [in0, scalar, in1], [out, accum_out])

    def copy(self, eng, out, in_):
        a = self._a
        if eng == "act":
            self.op(eng, lambda q: q.copy(a(out), a(in_)), [in_], [out])
        else:
            self.op(eng, lambda q: q.tensor_copy(a(out), a(in_)), [in_], [out])

    def memset(self, eng, out, val):
        a = self._a
        self.op(eng, lambda q: q.memset(a(out), val), [], [out])

    def reduce(self, eng, out, in_, op, axis=AX.X):
        a = self._a
        self.op(eng, lambda q: q.tensor_reduce(a(out), a(in_), axis, op), [in_], [out])

    def recip(self, out, in_):
        a = self._a
        self.op("dve", lambda q: q.reciprocal(a(out), a(in_)), [in_], [out])

    def gen(self, eng, fn, reads, writes):
        self.op(eng, fn, reads, writes)

    def dma(self, eng, out, in_, **kw):
        a = self._a
        self.op(eng, lambda q: q.dma_start(out=a(out), in_=a(in_), **kw), [in_], [out], dma=True)

    def dma_gen(self, eng, fn, reads, writes):
        self.op(eng, fn, reads, writes, dma=True)

    def finish(self):
        nc = self.nc
        engs = ["pe", "act", "dve", "pool", "sp"]
        qs = {"pe": nc.tensor, "act": nc.scalar, "dve": nc.vector, "pool": nc.gpsimd, "sp": nc.sync}
        sems = {e: nc.alloc_semaphore(f"s_{e}") for e in engs}
        dsems = {e: [nc.alloc_semaphore(f"d_{e}{i}") for i in range(NDMA)] for e in ("act", "pool", "sp")}
        semobj = {}
        for e in engs:
            semobj[("c", e)] = sems[e]
        for e in dsems:
            for i in range(NDMA):
                semobj[("d", e, i)] = dsems[e][i]
        cnt = {e: 0 for e in engs}
        dcnt = {e: 0 for e in dsems}
        dval = {k: 0 for k in semobj}
        waited = {e: {} for e in engs}
        per_eng = {e: [] for e in engs}
        for (eng, fn, rb, wb, dma) in self.ops:
            deps = {}

            def add(tok):
                if tok is None:
                    return
                k, v = tok
                if deps.get(k, 0) < v:
                    deps[k] = v
            for b in rb:
                add(b.lw)
            for b in wb:
                add(b.lw)
                for k, v in b.rd.items():
                    add((k, v))
            waits = []
            for k, v in deps.items():
                if k == ("c", eng) and eng == "pe":
                    continue
                if waited[eng].get(k, 0) >= v:
                    continue
                waited[eng][k] = v
                waits.append((k, v))
            if dma:
                i = dcnt[eng] % NDMA
                dcnt[eng] += 1
                key = ("d", eng, i)
                dval[key] += 16
                tok = (key, dval[key])
                inc = 16
            else:
                cnt[eng] += 1
                key = ("c", eng)
                tok = (key, cnt[eng])
                inc = 1
            per_eng[eng].append((fn, waits, key, inc))
            for b in rb:
                if b.rd.get(tok[0], 0) < tok[1]:
                    b.rd[tok[0]] = tok[1]
            for b in wb:
                b.lw = tok
                b.rd = {}
        fin = []
        for e in engs:
            if cnt[e]:
                fin.append((("c", e), cnt[e]))
        for k, v in dval.items():
            if v:
                fin.append((k, v))
        self.stats = dict(cnt=dict(cnt), dcnt=dict(dcnt))

        with nc.Block() as block:
            def body(eng):
                def f(q):
                    for (fn, waits, key, inc) in per_eng[eng]:
                        for (k, v) in waits:
                            q.wait_ge(semobj[k], v)
                        fn(q).then_inc(semobj[key], inc)
                    if eng == "sp":
                        for (k, v) in fin:
                            q.wait_ge(semobj[k], v)
                return f
            block.tensor(body("pe"))
            block.scalar(body("act"))
            block.vector(body("dve"))
            block.gpsimd(body("pool"))
            block.sync(body("sp"))
        return nc


D = 4096
S_FULL = 16384
NCORE = 8
HA, QKN, QKR, DVA = 16, 128, 64, 128
QL, KVL = 1024, 512
HB, DHB = 8, 128
NG, EPG, DE = 8, 8, 384
EPS = 1e-6
D_IN = 15936
OFF_CQ, OFF_CKV, OFF_KR, OFF_QB, OFF_KB, OFF_VB, OFF_GA, OFF_GB = 0, 1024, 1536, 1600, 3648, 5696, 7744, 11840
KC = D // 128
TWO_PI = 6.283185307179586
PI = 3.141592653589793
PI_LO = 3.1415925
MAGIC = 12582912.0
CW1 = 6.28125
CW2 = TWO_PI - 6.28125


def make_ident(k, dtype, name):
    idf = k.sb([128, 128], F32, name + "_f")
    k.memset("dve", idf.v, 0.0)
    k.gen("pool", lambda q: q.affine_select(idf.base, idf.base, [[-1, 128]], ALU.not_equal, 1.0, base=0,
                                            channel_multiplier=1), [idf.v], [idf.v])
    if dtype == F32:
        return idf
    idb = k.sb([128, 128], dtype, name)
    k.copy("dve", idb.v, idf.v)
    return idb


class Rot:
    def __init__(self, bufs):
        self.bufs = bufs
        self.i = 0

    def next(self):
        b = self.bufs[self.i % len(self.bufs)]
        self.i += 1
        return b


def build_t1(TPC):
    TT = 512
    NT = TPC // TT
    k = KB()
    x = k.dram("x", [TPC, D], F32, "ExternalInput")
    pos = k.dram("pos", [1, TPC], I32, "ExternalInput")
    invf = k.dram("invf", [64, 1], F32, "ExternalInput")
    g_attn = k.dram("g_attn", [D], F32, "ExternalInput")
    w_in = k.dram("w_in", [D, D_IN], F32, "ExternalInput")
    g_q = k.dram("g_q", [QL], F32, "ExternalInput")
    w_uq = k.dram("w_uq", [QL, HA * 192], F32, "ExternalInput")
    g_kv = k.dram("g_kv", [KVL], F32, "ExternalInput")
    w_ukv = k.dram("w_ukv", [KVL, HA * 256], F32, "ExternalInput")
    o_qn = k.dram("o_qn", [HA, 128, TPC], BF16, "ExternalOutput")
    o_qr = k.dram("o_qr", [HA, 64, TPC], BF16, "ExternalOutput")
    o_kn = k.dram("o_kn", [HA, 128, TPC], BF16, "ExternalOutput")
    o_kr = k.dram("o_kr", [64, TPC], BF16, "ExternalOutput")
    o_va = k.dram("o_va", [TPC, HA * 128], BF16, "ExternalOutput")
    o_qb = k.dram("o_qb", [16, 128, TPC], BF16, "ExternalOutput")
    o_kb = k.dram("o_kb", [16, 128, TPC], BF16, "ExternalOutput")
    o_vb = k.dram("o_vb", [TPC, 2048], BF16, "ExternalOutput")
    o_ga = k.dram("o_ga", [32, 128, TPC], BF16, "ExternalOutput")
    o_gb = k.dram("o_gb", [32, 128, TPC], BF16, "ExternalOutput")

    ident = make_ident(k, BF16, "ident")
    ones_f = k.sb([128, 128], F32, "ones_f")
    k.memset("dve", ones_f.v, 1.0)
    gT = k.sb([128, KC], F32, "gT")
    k.dma("sp", gT.v, g_attn.v.rearrange("(kc p) -> p kc", p=128), allow_slow_non_contiguous=True)
    gqT = k.sb([128, 8], F32, "gqT")
    k.dma("sp", gqT.v, g_q.v.rearrange("(kc p) -> p kc", p=128), allow_slow_non_contiguous=True)
    gkvT = k.sb([128, 4], F32, "gkvT")
    k.dma("sp", gkvT.v, g_kv.v.rearrange("(kc p) -> p kc", p=128), allow_slow_non_contiguous=True)
    invt = k.sb([64, 1], F32, "invt")
    k.dma("sp", invt.v, invf.v)

    w_in_v = w_in.v.rearrange("(kc p) n -> p kc n", p=128)
    w_uq_v = w_uq.v.rearrange("(kc p) n -> p kc n", p=128)
    w_ukv_v = w_ukv.v.rearrange("(kc p) n -> p kc n", p=128)

    wkr = k.sb([128, KC, 64], BF16, "wkr")
    wkrr = k.sb([128, KC, 64], BF16, "wkrr")
    k.dma("pool", wkr.v, w_in_v[:, :, OFF_KR:OFF_KR + 64])
    k.dma("pool", wkrr[:, :, 0:32], w_in_v[:, :, OFF_KR + 32:OFF_KR + 64])
    k.dma("pool", wkrr[:, :, 32:64], w_in_v[:, :, OFF_KR:OFF_KR + 32])
    k.gen("act", lambda q: q.mul(wkrr.base[:, :, 0:32], wkrr.base[:, :, 0:32], -1.0), [wkrr.v], [wkrr.v])
    wukv_g = k.sb([128, 4, 1024], BF16, "wukv_g")
    wuq_g = k.sb([128, 8, 768], BF16, "wuq_g")
    wuqr = k.sb([128, 8, HA, 64], BF16, "wuqr")
    wuq_h = w_uq_v.rearrange("p kc (h c) -> p kc h c", c=192)
    for kc in range(8):
        k.dma("pool", wuqr[:, kc, :, 0:32], wuq_h[:, kc, :, 160:192])
        k.dma("pool", wuqr[:, kc, :, 32:64], wuq_h[:, kc, :, 128:160])
    k.gen("act", lambda q: q.mul(wuqr.base[:, :, :, 0:32], wuqr.base[:, :, :, 0:32], -1.0), [wuqr.v], [wuqr.v])

    xin = Rot([k.sb([128, D], F32, f"xin{i}") for i in range(1)])
    hbf = Rot([k.sb([128, D], BF16, f"hbf{i}") for i in range(1)])
    stat = Rot([k.sb([128, 4], F32, f"stat{i}") for i in range(4)])
    hT = k.sb([128, KC, TT], BF16, "hT")
    wbuf = Rot([k.sb([128, KC, 256], BF16, f"wbuf{i}") for i in range(2)])
    cqT = k.sb([128, 8, TT], F32, "cqT")
    ckvT = k.sb([128, 4, TT], F32, "ckvT")
    cqn = k.sb([128, 8, TT], BF16, "cqn")
    ckvn = k.sb([128, 4, TT], BF16, "ckvn")
    sq32 = Rot([k.sb([128, TT], F32, f"sq32_{i}") for i in range(2)])
    rstd_q = k.sb([128, TT], F32, "rstd_q")
    rstd_kv = k.sb([128, TT], F32, "rstd_kv")
    stg = Rot([k.sb([128, TT], BF16, f"stg{i}") for i in range(6)])
    posi = k.sb([64, TT], I32, "posi")
    posf = k.sb([64, TT], F32, "posf")
    ang = k.sb([64, TT], F32, "ang")
    tmpa = k.sb([64, TT], F32, "tmpa")
    cos2 = k.sb([64, TT], F32, "cos2")
    sin2 = k.sb([64, TT], F32, "sin2")
    r1 = Rot([k.sb([64, TT], F32, f"r1_{i}") for i in range(2)])
    r2 = Rot([k.sb([64, TT], F32, f"r2_{i}") for i in range(2)])
    tmpb = k.sb([64, TT], F32, "tmpb")
    epst = k.sb([128, 1], F32, "epst")
    k.memset("dve", epst.v, EPS)

    ptr = Rot([k.ps([128, 8, 128], BF16, f"ptr{i}") for i in range(2)])
    pacc = Rot([k.ps([128, 512], F32, f"pacc{i}") for i in range(5)])
    pss = k.ps([128, 512], F32, "pss")
    evac_i = [0]

    def evac_copy(out, in_):
        evac_i[0] += 1
        k.copy("dve" if evac_i[0] % 2 else "act", out, in_)

    def rope(pa, pb, dst_dram):
        a1 = r1.next()
        a2 = r2.next()
        k.tt("dve", a1.v, pa, cos2.v, ALU.mult)
        k.tt("dve", a2.v, pb, sin2.v, ALU.mult)
        o = stg.next()
        k.tt("dve", o[0:64, :], a1.v, a2.v, ALU.add)
        k.dma("sp", dst_dram, o[0:64, :])

    for t in range(NT):
        t0 = t * TT
        k.dma("sp", posi.v, pos[0:1, t0:t0 + TT].partition_broadcast(64) if False else pos.v[0:1, t0:t0 + TT].ap.partition_broadcast(64) if False else V(pos.base[0:1, t0:t0 + TT].partition_broadcast(64), pos))
        k.copy("dve", posf.v, posi.v)
        k.ts("dve", ang.v, posf.v, invt[:, 0:1], None, ALU.mult)
        def sin_of(dst, src):
            k.ts("dve", tmpa.v, src, 1.0 / TWO_PI, MAGIC, ALU.mult, ALU.add)
            k.ts("dve", tmpa.v, tmpa.v, MAGIC, None, ALU.subtract)
            k.stt("dve", tmpb.v, tmpa.v, -CW1, src, ALU.mult, ALU.add)
            k.stt("dve", tmpb.v, tmpa.v, -CW2, tmpb.v, ALU.mult, ALU.add)
            k.ts("dve", tmpb.v, tmpb.v, PI_LO, -PI_LO, ALU.min, ALU.max)
            k.act(dst, tmpb.v, AF.Sin)
        sin_of(sin2.v, ang.v)
        k.ts("dve", ang.v, ang.v, PI / 2, None, ALU.add)
        sin_of(cos2.v, ang.v)
        for s in range(TT // 128):
            xt = xin.next()
            k.dma("sp", xt.v, x[t0 + s * 128:t0 + (s + 1) * 128, :])
            st = stat.next()
            hb = hbf.next()
            k.act(hb.v, xt.v, AF.Square, accum_out=st[:, 0:1])
            k.act(st[:, 1:2], st[:, 0:1], AF.Sqrt, scale=1.0 / D, bias=epst[:, 0:1])
            k.recip(st[:, 2:3], st[:, 1:2])
            k.ts("dve", hb.v, xt.v, st[:, 2:3], None, ALU.mult)
            for g8 in range(KC // 8):
                pt = ptr.next()
                for j in range(8):
                    kc = g8 * 8 + j
                    k.tr(pt[:, j, :], hb[:, kc * 128:(kc + 1) * 128], ident.v)
                gb = V(gT.base[:, g8 * 8:(g8 + 1) * 8].unsqueeze(2).to_broadcast([128, 8, 128]), gT)
                k.tt("dve", hT[:, g8 * 8:(g8 + 1) * 8, s * 128:(s + 1) * 128], pt.v, gb, ALU.mult)

        def fm_chunk(wb, j, evac):
            pa = pacc.next()
            for kc in range(KC):
                k.mm(pa.v, wb[:, kc, j * 128:(j + 1) * 128], hT[:, kc, :], start=(kc == 0), stop=(kc == KC - 1))
            evac(pa)

        def stream_fm(col0, ncols, evac_fn):
            for c in range(ncols // 256):
                wb = wbuf.next()
                k.dma("pool", wb.v, w_in_v[:, :, col0 + c * 256:col0 + (c + 1) * 256])
                for j in range(2):
                    fm_chunk(wb, j, lambda pa, idx=c * 2 + j: evac_fn(pa, idx))

        stream_fm(OFF_CQ, QL, lambda pa, idx: evac_copy(cqT[:, idx, :], pa.v))
        stream_fm(OFF_CKV, KVL, lambda pa, idx: evac_copy(ckvT[:, idx, :], pa.v))

        pa = pacc.next()
        pb = pacc.next()
        for kc in range(KC):
            k.mm(pa[0:64, :], wkr[:, kc, :], hT[:, kc, :], start=(kc == 0), stop=(kc == KC - 1))
        for kc in range(KC):
            k.mm(pb[0:64, :], wkrr[:, kc, :], hT[:, kc, :], start=(kc == 0), stop=(kc == KC - 1))
        rope(pa[0:64, :], pb[0:64, :], o_kr[:, t0:t0 + TT])

        def subnorm(srcT, nch, width, gvec, rstd, dst):
            for j in range(nch):
                sq = sq32.next()
                k.tt("pool", sq.v, srcT[:, j, :], srcT[:, j, :], ALU.mult)
                k.mm(pss.v, ones_f.v, sq.v, start=(j == 0), stop=(j == nch - 1))
            k.act(rstd.v, pss.v, AF.Sqrt, scale=1.0 / width, bias=epst[:, 0:1])
            k.recip(rstd.v, rstd.v)
            for j in range(nch):
                k.stt("dve", dst[:, j, :], srcT[:, j, :], gvec[:, j:j + 1], rstd.v, ALU.mult, ALU.mult)

        subnorm(cqT, 8, QL, gqT, rstd_q, cqn)
        subnorm(ckvT, 4, KVL, gkvT, rstd_kv, ckvn)

        for h in range(HA):
            if h % 4 == 0:
                k.dma("pool", wuq_g.v, w_uq_v[:, :, h * 192:(h + 4) * 192])
            hl = h % 4
            pa = pacc.next()
            for kc in range(8):
                k.mm(pa.v, wuq_g[:, kc, hl * 192:hl * 192 + 128], cqn[:, kc, :], start=(kc == 0), stop=(kc == 7))
            o = stg.next()
            evac_copy(o.v, pa.v)
            k.dma("sp", o_qn[h, :, t0:t0 + TT], o.v)
            pa = pacc.next()
            pb = pacc.next()
            for kc in range(8):
                k.mm(pa[0:64, :], wuq_g[:, kc, hl * 192 + 128:hl * 192 + 192], cqn[:, kc, :], start=(kc == 0), stop=(kc == 7))
            for kc in range(8):
                k.mm(pb[0:64, :], wuqr[:, kc, h, :], cqn[:, kc, :], start=(kc == 0), stop=(kc == 7))
            rope(pa[0:64, :], pb[0:64, :], o_qr[h, :, t0:t0 + TT])
        wv = V(wukv_g.base.rearrange("p kc (h two c) -> p kc h two c", two=2, c=128), wukv_g)
        for hg in range(4):
            k.dma("pool", wukv_g.v, w_ukv_v[:, :, hg * 1024:(hg + 1) * 1024])
            for hl in range(4):
                h = hg * 4 + hl
                pa = pacc.next()
                for kc in range(4):
                    k.mm(pa.v, wukv_g[:, kc, hl * 256:hl * 256 + 128], ckvn[:, kc, :], start=(kc == 0), stop=(kc == 3))
                o = stg.next()
                evac_copy(o.v, pa.v)
                k.dma("sp", o_kn[h, :, t0:t0 + TT], o.v)
            for s in range(TT // 128):
                pa = pacc.next()
                pav = V(pa.base.rearrange("p (h c) -> p h c", c=128), pa)
                for kc in range(4):
                    k.mm(pav, ckvn[:, kc, s * 128:(s + 1) * 128], wv[:, kc, :, 1, :],
                         start=(kc == 0), stop=(kc == 3))
                o = stg.next()
                evac_copy(o.v, pa.v)
                k.dma("sp", o_va[t0 + s * 128:t0 + (s + 1) * 128, hg * 512:(hg + 1) * 512], o.v)

        def ev_out(dst):
            def f(pa, idx):
                o = stg.next()
                evac_copy(o.v, pa.v)
                k.dma("sp", dst[idx, :, t0:t0 + TT], o.v)
            return f

        def ev_sig(dst):
            def f(pa, idx):
                o = stg.next()
                k.act(o.v, pa.v, AF.Sigmoid)
                k.dma("sp", dst[idx, :, t0:t0 + TT], o.v)
            return f
        stream_fm(OFF_QB, 2048, ev_out(o_qb))
        stream_fm(OFF_KB, 2048, ev_out(o_kb))
        for c in range(2048 // 256):
            wb = wbuf.next()
            k.dma("pool", wb.v, w_in_v[:, :, OFF_VB + c * 256:OFF_VB + (c + 1) * 256])
            for s in range(TT // 128):
                pa = pacc.next()
                for kc in range(KC):
                    k.mm(pa[:, 0:256], hT[:, kc, s * 128:(s + 1) * 128], wb[:, kc, :], start=(kc == 0), stop=(kc == KC - 1))
                o = stg.next()
                evac_copy(o[:, 0:256], pa[:, 0:256])
                k.dma("sp", o_vb[t0 + s * 128:t0 + (s + 1) * 128, c * 256:(c + 1) * 256], o[:, 0:256])
        stream_fm(OFF_GA, D, ev_sig(o_ga))
        stream_fm(OFF_GB, D, ev_sig(o_gb))
    nc = k.finish()
    return nc, k


def build_attn(S):
    NB = S // 128
    NQ = S // 512
    k = KB()
    qn = k.dram("qn", [2, 128, S], BF16, "ExternalInput")
    qr = k.dram("qr", [2, 64, S], BF16, "ExternalInput")
    kn = k.dram("kn", [2, 128, S], BF16, "ExternalInput")
    kr = k.dram("kr", [64, S], BF16, "ExternalInput")
    va = k.dram("va", [2, 128, NB, 128], BF16, "ExternalInput")
    qb = k.dram("qb", [2, 128, S], BF16, "ExternalInput")
    kb = k.dram("kb", [2, 128, S], BF16, "ExternalInput")
    vb = k.dram("vb", [128, NB, 256], BF16, "ExternalInput")
    pos = k.dram("pos", [1, S], I32, "ExternalInput")
    posk = k.dram("posk", [128, NB], I32, "ExternalInput")
    lam = k.dram("lam", [1, 4, 128], F32, "ExternalInput")
    subln = k.dram("subln", [256], F32, "ExternalInput")
    consts = k.dram("consts", [128, 4], F32, "ExternalInput")
    oa = k.dram("oa", [2, 128, S], BF16, "ExternalOutput")
    ob = k.dram("ob", [2, 128, S], BF16, "ExternalOutput")

    SC_A = float((QKN + QKR) ** -0.5)
    SC_B = float(DHB ** -0.5)

    ones_b = k.sb([128, 128], BF16, "ones_b")
    k.memset("dve", ones_b.v, 1.0)
    ones_f = k.sb([128, 128], F32, "ones_f")
    k.memset("dve", ones_f.v, 1.0)
    tri_f = k.sb([128, 128], F32, "tri_f")
    k.memset("dve", tri_f.v, 1.0)
    k.gen("pool", lambda q: q.affine_select(tri_f.base, tri_f.base, [[1, 128]], ALU.is_ge, 0.0, base=0,
                                            channel_multiplier=-1), [tri_f.v], [tri_f.v])
    tri = k.sb([128, 128], BF16, "tri")
    k.copy("dve", tri.v, tri_f.v)
    epst = k.sb([128, 1], F32, "epst")
    k.memset("dve", epst.v, EPS)

    cst = k.sb([128, 4], F32, "cst")
    k.dma("sp", cst.v, consts.v)
    lamt = k.sb([1, 4, 128], F32, "lamt")
    k.dma("sp", lamt.v, lam.v)
    lw = k.sb([1, 8], F32, "lw")
    lj = k.sb([1, 128], F32, "lj")
    k.tt("dve", lj.v, lamt[:, 0, :], lamt[:, 1, :], ALU.mult)
    k.reduce("dve", lw[:, 0:1], lj.v, ALU.add)
    k.tt("dve", lj.v, lamt[:, 2, :], lamt[:, 3, :], ALU.mult)
    k.reduce("dve", lw[:, 1:2], lj.v, ALU.add)
    k.act(lw[:, 2:4], lw[:, 0:2], AF.Exp)
    k.tt("dve", lw[:, 4:5], lw[:, 2:3], lw[:, 3:4], ALU.subtract)
    k.tt("dve", lw[:, 5:6], lw[:, 4:5], cst[0:1, 1:2], ALU.add)
    k.ts("dve", lw[:, 6:7], lw[:, 5:6], -1.0, None, ALU.mult)
    pmisc = k.ps([128, 512], F32, "pmisc")
    k.mm(pmisc[:, 0:1], ones_f[0:1, :], lw[0:1, 6:7])
    neglam = k.sb([128, 1], F32, "neglam")
    k.copy("dve", neglam.v, pmisc[:, 0:1])
    subs = k.sb([128, 2], F32, "subs")
    k.dma("sp", subs.v, subln.v.rearrange("(c p) -> p c", p=128), allow_slow_non_contiguous=True)
    k.ts("dve", subs.v, subs.v, cst[:, 2:3], None, ALU.mult)
    pk_i = k.sb([128, NB], I32, "pk_i")
    k.dma("sp", pk_i.v, posk.v)
    pos0_i = k.sb([128, 1], I32, "pos0_i")
    k.dma("sp", pos0_i.v, V(pos.base[0:1, 0:1].partition_broadcast(128), pos))
    pos0 = k.sb([128, 1], F32, "pos0")
    k.copy("dve", pos0.v, pos0_i.v)
    mps = k.sb([128, 2], F32, "mps")
    k.ts("dve", mps[:, 0:1], cst[:, 0:1], 1.0 / SC_B, None, ALU.mult)
    k.ts("dve", mps[:, 1:2], cst[:, 0:1], -1.0 / SC_B, None, ALU.mult)
    pks = k.sb([128, NB], F32, "pks")
    k.copy("dve", pks.v, pk_i.v)
    k.ts("dve", pks.v, pks.v, pos0[:, 0:1], mps[:, 0:1], ALU.subtract, ALU.mult)

    bufK = k.sb([128, S], BF16, "bufK")
    bufKR = k.sb([64, S], BF16, "bufKR")
    bufV = k.sb([128, NB, 256], BF16, "bufV")
    k.dma("sp", bufKR.v, kr.v)

    qt_a = Rot([k.sb([128, 512], BF16, f"qt_a{i}") for i in range(2)])
    qt_r = Rot([k.sb([64, 512], BF16, f"qt_r{i}") for i in range(2)])
    pT = Rot([k.sb([128, 512], BF16, f"pT{i}") for i in range(4)])
    sfp = Rot([k.sb([128, 512], F32, f"sfp{i}") for i in range(2)])
    qbs_i = k.sb([128, 512], I32, "qbs_i")
    qbs = k.sb([128, 512], F32, "qbs")
    rec = k.sb([128, 512], F32, "rec")
    o1 = k.sb([128, 2, 512], F32, "o1")
    o2 = k.sb([128, 2, 512], F32, "o2")
    sqt = Rot([k.sb([128, 512], F32, f"sqt{i}") for i in range(2)])
    rstd = k.sb([128, 512], F32, "rstd")
    ostg = Rot([k.sb([128, 512], BF16, f"ostg{i}") for i in range(3)])

    psS = Rot([k.ps([128, 512], F32, f"psS{i}") for i in range(2)])
    po = [k.ps([128, 512], F32, f"po{i}") for i in range(2)]
    pd = k.ps([128, 512], F32, "pd")

    def run_pass(j, ndv, qk_fn, exp_fn):
        q0 = j * 512
        nkb = 4 * (j + 1)
        pend = None

        def flush(pp):
            (P, i, c0) = pp
            for c in range(ndv):
                k.mm(po[c][:, c0:], bufV[:, i, c * 128:(c + 1) * 128], P[:, c0:], start=(i == 0), stop=(i == nkb - 1))
            k.mm(pd[:, c0:], ones_b.v, P[:, c0:], start=(i == 0), stop=(i == nkb - 1))
        for i in range(nkb):
            c0 = max(0, 128 * (i - 4 * j))
            ps = psS.next()
            qk_fn(ps, i, c0)
            P = pT.next()
            exp_fn(P, ps, i, c0)
            if i >= 4 * j:
                k.tt("pool", P[:, c0:c0 + 128], P[:, c0:c0 + 128], tri.v, ALU.mult)
            if pend is not None:
                flush(pend)
            pend = (P, i, c0)
        flush(pend)

    for h in range(2):
        k.dma("sp", bufK.v, kn[h])
        k.dma("sp", bufV[:, :, 0:128], va[h])
        for j in range(NQ):
            q0 = j * 512
            qa_t = qt_a.next()
            qr_t = qt_r.next()
            k.dma("sp", qa_t.v, qn[h, :, q0:q0 + 512])
            k.dma("sp", qr_t.v, qr[h, :, q0:q0 + 512])

            def qk_fn(ps, i, c0):
                k.mm(ps[:, c0:], bufK[:, i * 128:(i + 1) * 128], qa_t[:, c0:], start=True, stop=False)
                k.mm(ps[:, c0:], bufKR[:, i * 128:(i + 1) * 128], qr_t[:, c0:], start=False, stop=True)

            def exp_fn(P, ps, i, c0):
                k.act(P[:, c0:], ps[:, c0:], AF.Exp, scale=SC_A)
            run_pass(j, 1, qk_fn, exp_fn)
            k.recip(rec.v, pd.v)
            o = ostg.next()
            k.tt("dve", o.v, po[0].v, rec.v, ALU.mult)
            k.dma("sp", oa[h, :, q0:q0 + 512], o.v)

    k.dma("sp", bufV.v, vb.v)
    osave = [o1, o2]
    bufK2 = bufK
    kmaps = [bufK, None]
    bufKb2 = k.sb([128, S], BF16, "bufKb2")
    kmaps[1] = bufKb2
    k.dma("sp", bufK.v, kb[0])
    k.dma("sp", bufKb2.v, kb[1])
    for j in range(NQ):
        q0 = j * 512
        k.dma("sp", qbs_i.v, V(pos.base[0:1, q0:q0 + 512].partition_broadcast(128), pos))
        k.copy("dve", qbs.v, qbs_i.v)
        k.ts("dve", qbs.v, qbs.v, pos0[:, 0:1], mps[:, 1:2], ALU.subtract, ALU.mult)
        for m in range(2):
            qa_t = qt_a.next()
            k.dma("sp", qa_t.v, qb[m, :, q0:q0 + 512])
            Kb = kmaps[m]

            def qk_fn(ps, i, c0):
                k.mm(ps[:, c0:], Kb[:, i * 128:(i + 1) * 128], qa_t[:, c0:], start=True, stop=True)

            def exp_fn(P, ps, i, c0):
                sf = sfp.next()
                k.stt("dve", sf[:, c0:], ps[:, c0:], pks[:, i:i + 1], qbs[:, c0:], ALU.add, ALU.add)
                k.act(P[:, c0:], sf[:, c0:], AF.Exp, scale=SC_B)
            run_pass(j, 2, qk_fn, exp_fn)
            k.recip(rec.v, pd.v)
            for c in range(2):
                k.tt("dve", osave[m][:, c, :], po[c].v, rec.v, ALU.mult)
        for c in range(2):
            k.stt("dve", o1[:, c, :], o2[:, c, :], neglam[:, 0:1], o1[:, c, :], ALU.mult, ALU.add)
            sq = sqt.next()
            k.tt("pool", sq.v, o1[:, c, :], o1[:, c, :], ALU.mult)
            k.mm(pmisc.v, ones_f.v, sq.v, start=(c == 0), stop=(c == 1))
        k.act(rstd.v, pmisc.v, AF.Sqrt, scale=1.0 / 256, bias=epst[:, 0:1])
        k.recip(rstd.v, rstd.v)
        for c in range(2):
            o = ostg.next()
            k.stt("dve", o.v, o1[:, c, :], subs[:, c:c + 1], rstd.v, ALU.mult, ALU.mult)
            k.dma("sp", ob[c, :, q0:q0 + 512], o.v)
    nc = k.finish()
    return nc, k


def build_t2(TPC, CAP):
    TT = 512
    NT = TPC // TT
    NSLOT = NG * CAP
    k = KB()
    oa = k.dram("oa", [16, 128, TPC], BF16, "ExternalInput")
    ob = k.dram("ob", [16, 128, TPC], BF16, "ExternalInput")
    ga = k.dram("ga", [32, 128, TPC], BF16, "ExternalInput")
    gb = k.dram("gb", [32, 128, TPC], BF16, "ExternalInput")
    x = k.dram("x", [TPC, D], F32, "ExternalInput")
    w_oa = k.dram("w_oa", [2048, D], F32, "ExternalInput")
    w_ob = k.dram("w_ob", [2048, D], F32, "ExternalInput")
    w_out = k.dram("w_out", [D, D], F32, "ExternalInput")
    g_ffn = k.dram("g_ffn", [1, D], F32, "ExternalInput")
    w_r = k.dram("w_r", [D, 72], F32, "ExternalInput")
    b_r = k.dram("b_r", [1, 72], F32, "ExternalInput")
    goff = k.dram("goff", [1, 8], F32, "ExternalInput")
    x1 = k.dram("x1", [TPC, D], F32, "ExternalOutput")
    xg = k.dram("xg", [NSLOT, D], BF16, "ExternalOutput")
    gate = k.dram("gate", [NSLOT, 8], F32, "ExternalOutput")
    slot = k.dram("slot", [TPC, 1], I32, "ExternalOutput")

    ident_f = make_ident(k, F32, "ident")
    ones_f = k.sb([128, 128], F32, "ones_f")
    k.memset("dve", ones_f.v, 1.0)
    ustr = k.sb([128, 128], F32, "ustr")
    k.memset("dve", ustr.v, 1.0)
    k.gen("pool", lambda q: q.affine_select(ustr.base, ustr.base, [[1, 128]], ALU.is_gt, 0.0, base=0,
                                            channel_multiplier=-1), [ustr.v], [ustr.v])
    epst = k.sb([128, 1], F32, "epst")
    k.memset("dve", epst.v, EPS)
    gft = k.sb([128, D], F32, "gft")
    k.dma("sp", gft.v, V(g_ffn.base[0:1, :].partition_broadcast(128), g_ffn))
    brt = k.sb([128, 72], F32, "brt")
    k.dma("sp", brt.v, V(b_r.base[0:1, :].partition_broadcast(128), b_r))
    gofft = k.sb([128, 8], F32, "gofft")
    k.dma("sp", gofft.v, V(goff.base[0:1, :].partition_broadcast(128), goff))
    wr = k.sb([128, KC, 72], F32, "wr")
    k.dma("sp", wr.v, w_r.v.rearrange("(kc p) n -> p kc n", p=128))
    h2b = k.sb([128, D], BF16, "h2b")
    zb = h2b
    k.memset("pool", zb.v, 0.0)
    zg = k.sb([128, 8], F32, "zg")
    k.memset("pool", zg.v, 0.0)
    for r in range(NSLOT // 128):
        k.dma("sp", xg[r * 128:(r + 1) * 128, :], zb.v)
        k.dma("sp", gate[r * 128:(r + 1) * 128, :], zg.v)

    w_oa_v = w_oa.v.rearrange("(kc p) n -> p kc n", p=128)
    w_ob_v = w_ob.v.rearrange("(kc p) n -> p kc n", p=128)
    w_out_v = w_out.v.rearrange("(kc p) n -> p kc n", p=128)

    oaT = k.sb([128, 16, TT], BF16, "oaT")
    obT = k.sb([128, 16, TT], BF16, "obT")
    mT = k.sb([128, KC, TT], BF16, "mT")
    wab = Rot([k.sb([128, 16, 256], BF16, f"wab{i}") for i in range(2)])
    wo = Rot([k.sb([128, KC, 256], BF16, f"wo{i}") for i in range(2)])
    gt = Rot([k.sb([128, TT], BF16, f"gt{i}") for i in range(4)])
    tmp1 = Rot([k.sb([128, TT], F32, f"tmp1_{i}") for i in range(2)])
    tmp2 = Rot([k.sb([128, TT], F32, f"tmp2_{i}") for i in range(2)])
    xs = Rot([k.sb([128, 256], F32, f"xs{i}") for i in range(3)])
    xo = Rot([k.sb([128, 256], F32, f"xo{i}") for i in range(3)])
    x1t = k.sb([128, D], F32, "x1t")
    h2f = x1t
    h2T = k.sb([128, KC, 128], F32, "h2T")
    gcar = k.sb([128, 8], F32, "gcar")
    k.memset("dve", gcar.v, 0.0)
    sm = Rot([k.sb([128, 256], F32, f"sm{i}") for i in range(2)])
    sloti = Rot([k.sb([128, 1], I32, f"sloti{i}") for i in range(2)])
    g8 = Rot([k.sb([128, 8], F32, f"g8_{i}") for i in range(2)])

    pacc = Rot([k.ps([128, 512], F32, f"pacc{i}") for i in range(4)])
    ptr = Rot([k.ps([128, 4, 128], F32, f"ptr{i}") for i in range(2)])
    plg = k.ps([128, 512], F32, "plg")
    prk = k.ps([128, 512], F32, "prk")

    for t in range(NT):
        t0 = t * TT
        k.dma("sp", oaT.v, oa.v[:, :, t0:t0 + TT].rearrange("c p t -> p c t"))
        k.dma("sp", obT.v, ob.v[:, :, t0:t0 + TT].rearrange("c p t -> p c t"))
        for c in range(D // 256):
            wa = wab.next()
            wb = wab.next()
            k.dma("pool", wa.v, w_oa_v[:, :, c * 256:(c + 1) * 256])
            k.dma("pool", wb.v, w_ob_v[:, :, c * 256:(c + 1) * 256])
            for j in range(2):
                idx = c * 2 + j
                pa = pacc.next()
                pb = pacc.next()
                for kc in range(16):
                    k.mm(pa.v, wa[:, kc, j * 128:(j + 1) * 128], oaT[:, kc, :], start=(kc == 0), stop=(kc == 15))
                for kc in range(16):
                    k.mm(pb.v, wb[:, kc, j * 128:(j + 1) * 128], obT[:, kc, :], start=(kc == 0), stop=(kc == 15))
                gat = gt.next()
                gbt = gt.next()
                k.dma("sp", gat.v, ga[idx, :, t0:t0 + TT])
                k.dma("sp", gbt.v, gb[idx, :, t0:t0 + TT])
                a1 = tmp1.next()
                a2 = tmp2.next()
                k.tt("dve", a1.v, pa.v, gat.v, ALU.mult)
                k.tt("dve", a2.v, pb.v, gbt.v, ALU.mult)
                k.tt("pool", mT[:, idx, :], a1.v, a2.v, ALU.add)
        for c in range(D // 256):
            wt = wo.next()
            k.dma("pool", wt.v, w_out_v[:, :, c * 256:(c + 1) * 256])
            for s in range(TT // 128):
                r0 = t0 + s * 128
                pa = pacc.next()
                for kc in range(KC):
                    k.mm(pa[:, 0:256], mT[:, kc, s * 128:(s + 1) * 128], wt[:, kc, :], start=(kc == 0), stop=(kc == KC - 1))
                xi = xs.next()
                k.dma("sp", xi.v, x[r0:r0 + 128, c * 256:(c + 1) * 256])
                xq = xo.next()
                k.tt("dve", xq.v, pa[:, 0:256], xi.v, ALU.add)
                k.dma("sp", x1[r0:r0 + 128, c * 256:(c + 1) * 256], xq.v)
        for s in range(TT // 128):
            r0 = t0 + s * 128
            k.dma("sp", x1t.v, x1[r0:r0 + 128, :])
            w = sm.next()
            k.act(h2b.v, x1t.v, AF.Square, accum_out=w[:, 0:1])
            k.act(w[:, 1:2], w[:, 0:1], AF.Sqrt, scale=1.0 / D, bias=epst[:, 0:1])
            k.recip(w[:, 2:3], w[:, 1:2])
            k.stt("dve", h2f.v, x1t.v, w[:, 2:3], gft.v, ALU.mult, ALU.mult)
            k.copy("pool", h2b.v, h2f.v)
            for g4 in range(KC // 4):
                pt = ptr.next()
                for j in range(4):
                    kc = g4 * 4 + j
                    k.tr(pt[:, j, :], h2f[:, kc * 128:(kc + 1) * 128], ident_f.v)
                k.copy("act" if g4 % 2 else "dve", h2T[:, g4 * 4:(g4 + 1) * 4, :], pt.v)
            for kc in range(KC):
                k.mm(plg[:, 0:72], h2T[:, kc, :], wr[:, kc, :], start=(kc == 0), stop=(kc == KC - 1))
            lgb = w[:, 96:160]
            k.tt("dve", w[:, 32:40], plg[:, 0:8], brt[:, 0:8], ALU.add)
            k.tt("dve", lgb, plg[:, 8:72], brt[:, 8:72], ALU.add)
            k.reduce("dve", w[:, 3:4], w[:, 32:40], ALU.max)
            k.ts("dve", w[:, 40:48], w[:, 32:40], w[:, 3:4], None, ALU.is_equal)
            k.ts("dve", w[:, 4:5], w[:, 3:4], -1.0, None, ALU.mult)
            k.act(w[:, 80:88], w[:, 32:40], AF.Exp, bias=w[:, 4:5], accum_out=w[:, 5:6])
            k.recip(w[:, 6:7], w[:, 5:6])
            el3 = V(w.base[:, 96:160].rearrange("p (g e) -> p g e", e=8), w)
            pr3 = V(w.base[:, 160:224].rearrange("p (g e) -> p g e", e=8), w)
            Gb = V(w.base[:, 40:48].unsqueeze(2).to_broadcast([128, 8, 8]), w)
            k.tt("dve", pr3, el3, Gb, ALU.mult)
            k.reduce("dve", w[:, 48:56], V(w.base[:, 160:224].rearrange("p (g e) -> p e g", e=8), w), ALU.add)
            k.reduce("dve", w[:, 7:8], w[:, 48:56], ALU.max)
            k.ts("dve", w[:, 56:64], w[:, 48:56], w[:, 7:8], None, ALU.is_equal)
            k.stt("dve", w[:, 64:72], w[:, 56:64], -1e30, w[:, 48:56], ALU.mult, ALU.add)
            k.reduce("dve", w[:, 8:9], w[:, 64:72], ALU.max)
            k.ts("dve", w[:, 72:80], w[:, 64:72], w[:, 8:9], None, ALU.is_equal)
            k.tt("dve", w[:, 9:10], w[:, 8:9], w[:, 7:8], ALU.subtract)
            k.act(w[:, 10:11], w[:, 9:10], AF.Exp)
            k.ts("dve", w[:, 11:12], w[:, 10:11], 1.0, None, ALU.add)
            k.recip(w[:, 12:13], w[:, 11:12])
            k.tt("dve", w[:, 13:14], w[:, 10:11], w[:, 12:13], ALU.mult)
            k.tt("dve", w[:, 14:15], w[:, 12:13], w[:, 6:7], ALU.mult)
            k.tt("dve", w[:, 15:16], w[:, 13:14], w[:, 6:7], ALU.mult)
            gg = g8.next()
            k.ts("dve", gg.v, w[:, 56:64], w[:, 14:15], None, ALU.mult)
            k.stt("dve", gg.v, w[:, 72:80], w[:, 15:16], gg.v, ALU.mult, ALU.add)
            k.mm(prk[:, 0:8], ustr.v, w[:, 40:48], start=True, stop=False)
            k.mm(prk[:, 0:8], ones_f.v, gcar.v, start=False, stop=True)
            k.tt("dve", w[:, 88:96], prk[:, 0:8], gofft.v, ALU.add)
            k.tt("dve", w[:, 88:96], w[:, 88:96], w[:, 40:48], ALU.mult)
            k.reduce("dve", w[:, 16:17], w[:, 88:96], ALU.add)
            k.tt("dve", gcar.v, gcar.v, w[:, 40:48], ALU.add)
            si = sloti.next()
            k.copy("dve", si.v, w[:, 16:17])
            k.dma("sp", slot[r0:r0 + 128, :], si.v)
            k.dma_gen("pool", lambda q, si=si: q.indirect_dma_start(
                out=xg.base[:, :], out_offset=bass.IndirectOffsetOnAxis(ap=si.base[:, :], axis=0),
                in_=h2b.base[:, :], in_offset=None, bounds_check=NSLOT - 1, oob_is_err=False),
                [si.v, h2b.v], [xg.v])
            k.dma_gen("pool", lambda q, si=si, gg=gg: q.indirect_dma_start(
                out=gate.base[:, :], out_offset=bass.IndirectOffsetOnAxis(ap=si.base[:, :], axis=0),
                in_=gg.base[:, :], in_offset=None, bounds_check=NSLOT - 1, oob_is_err=False),
                [si.v, gg.v], [gate.v])
    nc = k.finish()
    return nc, k


def build_e(NSL, SLT):
    NTL = NSL // SLT
    NSUB = SLT // 128
    k = KB()
    xg = k.dram("xg", [NSL, D], BF16, "ExternalInput")
    gate = k.dram("gate", [NSL, 8], F32, "ExternalInput")
    w1 = k.dram("w1", [EPG, D, DE], F32, "ExternalInput")
    w3 = k.dram("w3", [EPG, D, DE], F32, "ExternalInput")
    w2 = k.dram("w2", [EPG, DE, D], F32, "ExternalInput")
    yg = k.dram("yg", [NSL, D], F32, "ExternalOutput")

    ident = make_ident(k, BF16, "ident")
    xrow = Rot([k.sb([128, D], BF16, f"xrow{i}") for i in range(2)])
    XgT = k.sb([128, KC, SLT], BF16, "XgT")
    gt = k.sb([128, NSUB, 8], F32, "gt")
    acc = k.sb([128, NSUB, D], F32, "acc")
    w13 = Rot([k.sb([128, KC, 128], BF16, f"w13_{i}") for i in range(4)])
    w2b = Rot([k.sb([128, 3, D], BF16, f"w2b{i}") for i in range(2)])
    GT = Rot([k.sb([128, 3, SLT], BF16, f"GT{i}") for i in range(2)])
    sl = Rot([k.sb([128, SLT], F32, f"sl{i}") for i in range(2)])
    ptr = Rot([k.ps([128, 8, 128], BF16, f"ptr{i}") for i in range(2)])
    ph = Rot([k.ps([128, 512], F32, f"ph{i}") for i in range(4)])
    py = Rot([k.ps([128, 512], F32, f"py{i}") for i in range(2)])

    for t in range(NTL):
        s0 = t * SLT
        for s in range(NSUB):
            xr = xrow.next()
            k.dma("sp", xr.v, xg[s0 + s * 128:s0 + (s + 1) * 128, :])
            k.dma("sp", gt[:, s, :], gate[s0 + s * 128:s0 + (s + 1) * 128, :])
            for g8 in range(KC // 8):
                pt = ptr.next()
                for j in range(8):
                    kc = g8 * 8 + j
                    k.tr(pt[:, j, :], xr[:, kc * 128:(kc + 1) * 128], ident.v)
                k.copy("dve" if g8 % 2 else "act", XgT[:, g8 * 8:(g8 + 1) * 8, s * 128:(s + 1) * 128], pt.v)
        for e in range(EPG):
            w1v = w1.v[e].rearrange("(kc p) f -> p kc f", p=128)
            w3v = w3.v[e].rearrange("(kc p) f -> p kc f", p=128)
            G = GT.next()
            for fc in range(3):
                wa = w13.next()
                wb = w13.next()
                k.dma("pool", wa.v, w1v[:, :, fc * 128:(fc + 1) * 128])
                k.dma("pool", wb.v, w3v[:, :, fc * 128:(fc + 1) * 128])
                p1 = ph.next()
                p3 = ph.next()
                for kc in range(KC):
                    k.mm(p1[:, 0:SLT], wa[:, kc, :], XgT[:, kc, :], start=(kc == 0), stop=(kc == KC - 1))
                for kc in range(KC):
                    k.mm(p3[:, 0:SLT], wb[:, kc, :], XgT[:, kc, :], start=(kc == 0), stop=(kc == KC - 1))
                sv = sl.next()
                k.act(sv.v, p1[:, 0:SLT], AF.Silu)
                k.tt("dve", G[:, fc, :], sv.v, p3[:, 0:SLT], ALU.mult)
            w2t = w2b.next()
            k.dma("pool", w2t.v, w2.v[e].rearrange("(fc p) d -> p fc d", p=128))
            for s in range(NSUB):
                for dc in range(D // 512):
                    pp = py.next()
                    for fc in range(3):
                        k.mm(pp.v, G[:, fc, s * 128:(s + 1) * 128], w2t[:, fc, dc * 512:(dc + 1) * 512],
                             start=(fc == 0), stop=(fc == 2))
                    dst = acc[:, s, dc * 512:(dc + 1) * 512]
                    if e == 0:
                        k.ts("dve", dst, pp.v, gt[:, s, e:e + 1], None, ALU.mult)
                    else:
                        k.stt("dve", dst, pp.v, gt[:, s, e:e + 1], dst, ALU.mult, ALU.add)
        for s in range(NSUB):
            k.dma("sp", yg[s0 + s * 128:s0 + (s + 1) * 128, :], acc[:, s, :])
    nc = k.finish()
    return nc, k


def build_c(TPC, NSLOT, final):
    k = KB()
    x1 = k.dram("x1", [TPC, D], F32, "ExternalInput")
    slot = k.dram("slot", [TPC, 1], I32, "ExternalInput")
    yg = k.dram("yg", [NSLOT, D], F32, "ExternalInput")
    g_fin = k.dram("g_fin", [1, D], F32, "ExternalInput")
    out = k.dram("out", [TPC, D], F32, "ExternalOutput")
    epst = k.sb([128, 1], F32, "epst")
    k.memset("dve", epst.v, EPS)
    gft = k.sb([128, D], F32, "gft")
    k.dma("sp", gft.v, V(g_fin.base[0:1, :].partition_broadcast(128), g_fin))
    xt = Rot([k.sb([128, D], F32, f"xt{i}") for i in range(2)])
    yt = Rot([k.sb([128, D], F32, f"yt{i}") for i in range(2)])
    si = Rot([k.sb([128, 1], I32, f"si{i}") for i in range(2)])
    st = Rot([k.sb([128, 4], F32, f"st{i}") for i in range(2)])
    for s in range(TPC // 128):
        r0 = s * 128
        x_ = xt.next()
        y_ = yt.next()
        i_ = si.next()
        k.dma("sp", x_.v, x1[r0:r0 + 128, :])
        k.dma("sp", i_.v, slot[r0:r0 + 128, :])
        k.dma_gen("pool", lambda q, i_=i_, y_=y_: q.indirect_dma_start(
            out=y_.base[:, :], out_offset=None, in_=yg.base[:, :],
            in_offset=bass.IndirectOffsetOnAxis(ap=i_.base[:, :], axis=0),
            bounds_check=NSLOT - 1, oob_is_err=False), [i_.v, yg.v], [y_.v])
        k.tt("dve", x_.v, x_.v, y_.v, ALU.add)
        if final:
            w = st.next()
            k.act(y_.v, x_.v, AF.Square, accum_out=w[:, 0:1])
            k.act(w[:, 1:2], w[:, 0:1], AF.Sqrt, scale=1.0 / D, bias=epst[:, 0:1])
            k.recip(w[:, 2:3], w[:, 1:2])
            k.stt("dve", x_.v, x_.v, w[:, 2:3], gft.v, ALU.mult, ALU.mult)
        k.dma("sp", out[r0:r0 + 128, :], x_.v)
    nc = k.finish()
    return nc, k


_PROG = {}


def _prog(key, fn):
    if key not in _PROG:
        _PROG[key] = fn()[0]
    return _PROG[key]


def _run(nc, in_maps):
    res = run_bass_kernel_spmd(nc, in_maps, core_ids=list(range(len(in_maps))))
    return res.results


def _c(a):
    return np.ascontiguousarray(a)


def kernel(x, positions, norm_attn, w_in, q_norm, w_uq, kv_norm, w_ukv, lam_q1, lam_k1, lam_q2, lam_k2,
           subln, w_oa, w_ob, w_out, norm_ffn, w_router_g, b_router_g, w_router_e, b_router_e,
           w1, w3, w2, norm_final):
    import math
    x = np.asarray(x)
    S = x.shape[1]
    depth = np.asarray(w_in).shape[0]
    TPC = S // NCORE
    NB = S // 128
    CAP = max(128, ((TPC // NG) * 3 // 2 + 127) // 128 * 128)
    NSL = NCORE * CAP
    SLT = CAP if CAP <= 512 else 384
    pos = np.asarray(positions).astype(np.int32).reshape(1, S)
    posk = _c(pos[0].reshape(NB, 128).T)
    half = QKR // 2
    inv = (np.float32(10000.0) ** (-np.arange(half, dtype=np.float32) / np.float32(half))).astype(np.float32)
    invf = np.concatenate([inv, inv]).reshape(64, 1).astype(np.float32)
    goff = (np.arange(NG, dtype=np.float32) * CAP).reshape(1, NG)
    X = x.reshape(S, D).astype(np.float32)
    cs = [slice(c * TPC, (c + 1) * TPC) for c in range(NCORE)]

    p_t1 = _prog(("t1", TPC), lambda: build_t1(TPC))
    p_a = _prog(("a", S), lambda: build_attn(S))
    p_t2 = _prog(("t2", TPC, CAP), lambda: build_t2(TPC, CAP))
    p_e = _prog(("e", NSL, SLT), lambda: build_e(NSL, SLT))

    for l in range(depth):
        w_in_l = _c(np.asarray(w_in[l], dtype=np.float32))
        w_uq_l = _c(np.asarray(w_uq[l], dtype=np.float32))
        w_ukv_l = _c(np.asarray(w_ukv[l], dtype=np.float32))
        ins = [dict(x=_c(X[cs[c]]), pos=_c(pos[:, cs[c]]), invf=invf, g_attn=_c(np.asarray(norm_attn[l], np.float32)),
                    w_in=w_in_l, g_q=_c(np.asarray(q_norm[l], np.float32)), w_uq=w_uq_l,
                    g_kv=_c(np.asarray(kv_norm[l], np.float32)), w_ukv=w_ukv_l) for c in range(NCORE)]
        r1 = _run(p_t1, ins)
        del ins

        def cat(name, axis):
            return np.concatenate([np.asarray(r1[c][name]) for c in range(NCORE)], axis=axis)
        QN, QR, KN, KR = cat("o_qn", 2), cat("o_qr", 2), cat("o_kn", 2), cat("o_kr", 1)
        VA, QB, KB_, VB = cat("o_va", 0), cat("o_qb", 2), cat("o_kb", 2), cat("o_vb", 0)
        GA = [np.asarray(r1[c]["o_ga"]) for c in range(NCORE)]
        GB = [np.asarray(r1[c]["o_gb"]) for c in range(NCORE)]
        del r1
        lam_init = 0.8 - 0.6 * math.exp(-0.3 * l)
        lam = np.stack([np.asarray(lam_q1[l], np.float32), np.asarray(lam_k1[l], np.float32),
                        np.asarray(lam_q2[l], np.float32), np.asarray(lam_k2[l], np.float32)]).reshape(1, 4, 128)
        ins = []
        for c in range(NCORE):
            consts = np.zeros((128, 4), np.float32)
            consts[:, 0] = 2.0 ** (-8.0 * (c + 1) / HB)
            consts[:, 1] = lam_init
            consts[:, 2] = 1.0 - lam_init
            va_c = np.stack([_c(VA[:, (2 * c + h) * 128:(2 * c + h + 1) * 128].reshape(NB, 128, 128).transpose(1, 0, 2))
                             for h in range(2)])
            vb_c = _c(VB[:, c * 256:(c + 1) * 256].reshape(NB, 128, 256).transpose(1, 0, 2))
            ins.append(dict(qn=_c(QN[2 * c:2 * c + 2]), qr=_c(QR[2 * c:2 * c + 2]), kn=_c(KN[2 * c:2 * c + 2]), kr=_c(KR),
                            va=_c(va_c), qb=_c(QB[2 * c:2 * c + 2]), kb=_c(KB_[2 * c:2 * c + 2]), vb=vb_c,
                            pos=pos, posk=posk, lam=lam, subln=_c(np.asarray(subln[l], np.float32)), consts=consts))
        del QN, QR, KN, KR, VA, QB, KB_, VB
        ra = _run(p_a, ins)
        del ins
        OA = np.concatenate([np.asarray(ra[c]["oa"]) for c in range(NCORE)], axis=0)
        OB = np.concatenate([np.asarray(ra[c]["ob"]) for c in range(NCORE)], axis=0)
        del ra
        w_oa_l = _c(np.asarray(w_oa[l], np.float32))
        w_ob_l = _c(np.asarray(w_ob[l], np.float32))
        w_out_l = _c(np.asarray(w_out[l], np.float32))
        w_r = _c(np.concatenate([np.asarray(w_router_g[l], np.float32), np.asarray(w_router_e[l], np.float32)], axis=1))
        b_r = _c(np.concatenate([np.asarray(b_router_g[l], np.float32), np.asarray(b_router_e[l], np.float32)]).reshape(1, 72))
        ins = [dict(oa=_c(OA[:, :, cs[c]]), ob=_c(OB[:, :, cs[c]]), ga=GA[c], gb=GB[c], x=_c(X[cs[c]]),
                    w_oa=w_oa_l, w_ob=w_ob_l, w_out=w_out_l, g_ffn=_c(np.asarray(norm_ffn[l], np.float32).reshape(1, D)),
                    w_r=w_r, b_r=b_r, goff=goff) for c in range(NCORE)]
        del OA, OB, GA, GB
        r2 = _run(p_t2, ins)
        del ins
        ins = []
        for g in range(NG):
            xg_g = np.concatenate([np.asarray(r2[c]["xg"])[g * CAP:(g + 1) * CAP] for c in range(NCORE)], axis=0)
            gate_g = np.concatenate([np.asarray(r2[c]["gate"])[g * CAP:(g + 1) * CAP] for c in range(NCORE)], axis=0)
            ins.append(dict(xg=_c(xg_g), gate=_c(gate_g),
                            w1=_c(np.asarray(w1[l, g * EPG:(g + 1) * EPG], np.float32)),
                            w3=_c(np.asarray(w3[l, g * EPG:(g + 1) * EPG], np.float32)),
                            w2=_c(np.asarray(w2[l, g * EPG:(g + 1) * EPG], np.float32))))
        re_ = _run(p_e, ins)
        del ins
        final = (l == depth - 1)
        p_c = _prog(("c", TPC, NG * CAP, final), lambda: build_c(TPC, NG * CAP, final))
        ins = []
        for c in range(NCORE):
            yg_c = np.concatenate([np.asarray(re_[g]["yg"])[c * CAP:(c + 1) * CAP] for g in range(NG)], axis=0)
            ins.append(dict(x1=np.asarray(r2[c]["x1"]), slot=np.asarray(r2[c]["slot"]), yg=_c(yg_c),
                            g_fin=_c(np.asarray(norm_final, np.float32).reshape(1, D))))
        del re_, r2
        rc = _run(p_c, ins)
        del ins
        X = np.concatenate([np.asarray(rc[c]["out"]) for c in range(NCORE)], axis=0)
        del rc
    return X.reshape(1, S, D).astype(np.float32)
```

```python
import numpy as np
import concourse.bass as bass
import concourse.mybir as mybir
from concourse.bass_utils import run_bass_kernel_spmd

F32 = mybir.dt.float32
BF16 = mybir.dt.bfloat16
I32 = mybir.dt.int32
AF = mybir.ActivationFunctionType
ALU = mybir.AluOpType
AX = mybir.AxisListType


class V:
    def __init__(self, ap, buf):
        self.ap = ap
        self.buf = buf

    def __getitem__(self, k):
        return V(self.ap[k], self.buf)

    def rearrange(self, *a, **kw):
        return V(self.ap.rearrange(*a, **kw), self.buf)


class Buf:
    def __init__(self, ap, name):
        self.base = ap
        self.name = name
        self.lw = None
        self.rd = {}

    def __getitem__(self, k):
        return V(self.base[k], self)

    @property
    def v(self):
        return V(self.base, self)


NDMA = 4


class KB:
    def __init__(self, name="k"):
        self.nc = bass.Bass("TRN2", target_bir_lowering=False)
        self.ops = []
        self.nbuf = 0

    def sb(self, shape, dtype, name=None):
        self.nbuf += 1
        name = name or f"sb{self.nbuf}"
        t = self.nc.alloc_sbuf_tensor(name, list(shape), dtype)
        return Buf(t[:], name)

    def ps(self, shape, dtype=F32, name=None):
        self.nbuf += 1
        name = name or f"ps{self.nbuf}"
        t = self.nc.alloc_psum_tensor(name, list(shape), dtype)
        return Buf(t[:], name)

    def dram(self, name, shape, dtype, kind="Internal"):
        t = self.nc.dram_tensor(name, list(shape), dtype, kind=kind)
        return Buf(t.ap(), name)

    def op(self, eng, fn, reads, writes, dma=False):
        rb = [x.buf for x in reads if isinstance(x, V)]
        wb = [x.buf for x in writes if isinstance(x, V)]
        self.ops.append((eng, fn, rb, wb, dma))

    @staticmethod
    def _a(x):
        return x.ap if isinstance(x, V) else x

    def mm(self, out, lhsT, rhs, start=True, stop=True):
        a = self._a
        self.op("pe", lambda q: q.matmul(a(out), a(lhsT), a(rhs), start=start, stop=stop),
                [lhsT, rhs] + ([] if start else [out]), [out])

    def tr(self, out, in_, ident):
        a = self._a
        self.op("pe", lambda q: q.transpose(a(out), a(in_), a(ident)), [in_, ident], [out])

    def act(self, out, in_, func, bias=None, scale=1.0, accum_out=None, eng="act"):
        a = self._a
        kw = {}
        if bias is not None:
            kw["bias"] = a(bias)
        if accum_out is not None:
            kw["accum_out"] = a(accum_out)
        self.op(eng, lambda q: q.activation(a(out), a(in_), func, scale=a(scale), **kw),
                [in_, bias, scale], [out, accum_out])

    def tt(self, eng, out, in0, in1, op):
        a = self._a
        self.op(eng, lambda q: q.tensor_tensor(a(out), a(in0), a(in1), op), [in0, in1], [out])

    def ts(self, eng, out, in0, s1, s2, op0, op1=None, accum_out=None):
        a = self._a
        kw = {}
        if op1 is not None:
            kw["op1"] = op1
        if accum_out is not None:
            kw["accum_out"] = a(accum_out)
        self.op(eng, lambda q: q.tensor_scalar(a(out), a(in0), a(s1), a(s2) if s2 is not None else None, op0, **kw),
                [in0, s1, s2], [out, accum_out])

    def stt(self, eng, out, in0, scalar, in1, op0, op1, accum_out=None):
        a = self._a
        kw = {}
        if accum_out is not None:
            kw["accum_out"] = a(accum_out)
        self.op(eng, lambda q: q.scalar_tensor_tensor(a(out), a(in0), a(scalar), a(in1), op0, op1, **kw),
                [in0, scalar, in1], [out, accum_out])

    def copy(self, eng, out, in_):
        a = self._a
        if eng == "act":
            self.op(eng, lambda q: q.copy(a(out), a(in_)), [in_], [out])
        else:
            self.op(eng, lambda q: q.tensor_copy(a(out), a(in_)), [in_], [out])

    def memset(self, eng, out, val):
        a = self._a
        self.op(eng, lambda q: q.memset(a(out), val), [], [out])

    def reduce(self, eng, out, in_, op, axis=AX.X):
        a = self._a
        self.op(eng, lambda q: q.tensor_reduce(a(out), a(in_), axis, op), [in_], [out])

    def recip(self, out, in_):
        a = self._a
        self.op("dve", lambda q: q.reciprocal(a(out), a(in_)), [in_], [out])

    def gen(self, eng, fn, reads, writes):
        self.op(eng, fn, reads, writes)

    def dma(self, eng, out, in_, **kw):
        a = self._a
        self.op(eng, lambda q: q.dma_start(out=a(out), in_=a(in_), **kw), [in_], [out], dma=True)

    def dma_gen(self, eng, fn, reads, writes):
        self.op(eng, fn, reads, writes, dma=True)

    def finish(self):
        nc = self.nc
        engs = ["pe", "act", "dve", "pool", "sp"]
        qs = {"pe": nc.tensor, "act": nc.scalar, "dve": nc.vector, "pool": nc.gpsimd, "sp": nc.sync}
        sems = {e: nc.alloc_semaphore(f"s_{e}") for e in engs}
        dsems = {e: [nc.alloc_semaphore(f"d_{e}{i}") for i in range(NDMA)] for e in ("act", "pool", "sp")}
        semobj = {}
        for e in engs:
            semobj[("c", e)] = sems[e]
        for e in dsems:
            for i in range(NDMA):
                semobj[("d", e, i)] = dsems[e][i]
        cnt = {e: 0 for e in engs}
        dcnt = {e: 0 for e in dsems}
        dval = {k: 0 for k in semobj}
        waited = {e: {} for e in engs}
        per_eng = {e: [] for e in engs}
        for (eng, fn, rb, wb, dma) in self.ops:
            deps = {}

            def add(tok):
                if tok is None:
                    return
                k, v = tok
                if deps.get(k, 0) < v:
                    deps[k] = v
            for b in rb:
                add(b.lw)
            for b in wb:
                add(b.lw)
                for k, v in b.rd.items():
                    add((k, v))
            waits = []
            for k, v in deps.items():
                if k == ("c", eng) and eng == "pe":
                    continue
                if waited[eng].get(k, 0) >= v:
                    continue
                waited[eng][k] = v
                waits.append((k, v))
            if dma:
                i = dcnt[eng] % NDMA
                dcnt[eng] += 1
                key = ("d", eng, i)
                dval[key] += 16
                tok = (key, dval[key])
                inc = 16
            else:
                cnt[eng] += 1
                key = ("c", eng)
                tok = (key, cnt[eng])
                inc = 1
            per_eng[eng].append((fn, waits, key, inc))
            for b in rb:
                if b.rd.get(tok[0], 0) < tok[1]:
                    b.rd[tok[0]] = tok[1]
            for b in wb:
                b.lw = tok
                b.rd = {}
        fin = []
        for e in engs:
            if cnt[e]:
                fin.append((("c", e), cnt[e]))
        for k, v in dval.items():
            if v:
                fin.append((k, v))
        self.stats = dict(cnt=dict(cnt), dcnt=dict(dcnt))

        with nc.Block() as block:
            def body(eng):
                def f(q):
                    for (fn, waits, key, inc) in per_eng[eng]:
                        for (k, v) in waits:
                            q.wait_ge(semobj[k], v)
                        fn(q).then_inc(semobj[key], inc)
                    if eng == "sp":
                        for (k, v) in fin:
                            q.wait_ge(semobj[k], v)
                return f
            block.tensor(body("pe"))
            block.scalar(body("act"))
            block.vector(body("dve"))
            block.gpsimd(body("pool"))
            block.sync(body("sp"))
        return nc


D = 4096
S_FULL = 16384
NCORE = 8
HA, QKN, QKR, DVA = 16, 128, 64, 128
QL, KVL = 1024, 512
HB, DHB = 8, 128
NG, EPG, DE = 8, 8, 384
EPS = 1e-6
D_IN = 15936
OFF_CQ, OFF_CKV, OFF_KR, OFF_QB, OFF_KB, OFF_VB, OFF_GA, OFF_GB = 0, 1024, 1536, 1600, 3648, 5696, 7744, 11840
KC = D // 128
TWO_PI = 6.283185307179586
PI = 3.141592653589793
PI_LO = 3.1415925
MAGIC = 12582912.0
CW1 = 6.28125
CW2 = TWO_PI - 6.28125


def make_ident(k, dtype, name):
    idf = k.sb([128, 128], F32, name + "_f")
    k.memset("dve", idf.v, 0.0)
    k.gen("pool", lambda q: q.affine_select(idf.base, idf.base, [[-1, 128]], ALU.not_equal, 1.0, base=0,
                                            channel_multiplier=1), [idf.v], [idf.v])
    if dtype == F32:
        return idf
    idb = k.sb([128, 128], dtype, name)
    k.copy("dve", idb.v, idf.v)
    return idb


class Rot:
    def __init__(self, bufs):
        self.bufs = bufs
        self.i = 0

    def next(self):
        b = self.bufs[self.i % len(self.bufs)]
        self.i += 1
        return b


def build_t1(TPC):
    TT = 512
    NT = TPC // TT
    k = KB()
    x = k.dram("x", [TPC, D], F32, "ExternalInput")
    pos = k.dram("pos", [1, TPC], I32, "ExternalInput")
    invf = k.dram("invf", [64, 1], F32, "ExternalInput")
    g_attn = k.dram("g_attn", [D], F32, "ExternalInput")
    w_in = k.dram("w_in", [D, D_IN], F32, "ExternalInput")
    g_q = k.dram("g_q", [QL], F32, "ExternalInput")
    w_uq = k.dram("w_uq", [QL, HA * 192], F32, "ExternalInput")
    g_kv = k.dram("g_kv", [KVL], F32, "ExternalInput")
    w_ukv = k.dram("w_ukv", [KVL, HA * 256], F32, "ExternalInput")
    o_qn = k.dram("o_qn", [HA, 128, TPC], BF16, "ExternalOutput")
    o_qr = k.dram("o_qr", [HA, 64, TPC], BF16, "ExternalOutput")
    o_kn = k.dram("o_kn", [HA, 128, TPC], BF16, "ExternalOutput")
    o_kr = k.dram("o_kr", [64, TPC], BF16, "ExternalOutput")
    o_va = k.dram("o_va", [TPC, HA * 128], BF16, "ExternalOutput")
    o_qb = k.dram("o_qb", [16, 128, TPC], BF16, "ExternalOutput")
    o_kb = k.dram("o_kb", [16, 128, TPC], BF16, "ExternalOutput")
    o_vb = k.dram("o_vb", [TPC, 2048], BF16, "ExternalOutput")
    o_ga = k.dram("o_ga", [32, 128, TPC], BF16, "ExternalOutput")
    o_gb = k.dram("o_gb", [32, 128, TPC], BF16, "ExternalOutput")

    ident = make_ident(k, BF16, "ident")
    ones_f = k.sb([128, 128], F32, "ones_f")
    k.memset("dve", ones_f.v, 1.0)
    gT = k.sb([128, KC], F32, "gT")
    k.dma("sp", gT.v, g_attn.v.rearrange("(kc p) -> p kc", p=128), allow_slow_non_contiguous=True)
    gqT = k.sb([128, 8], F32, "gqT")
    k.dma("sp", gqT.v, g_q.v.rearrange("(kc p) -> p kc", p=128), allow_slow_non_contiguous=True)
    gkvT = k.sb([128, 4], F32, "gkvT")
    k.dma("sp", gkvT.v, g_kv.v.rearrange("(kc p) -> p kc", p=128), allow_slow_non_contiguous=True)
    invt = k.sb([64, 1], F32, "invt")
    k.dma("sp", invt.v, invf.v)

    w_in_v = w_in.v.rearrange("(kc p) n -> p kc n", p=128)
    w_uq_v = w_uq.v.rearrange("(kc p) n -> p kc n", p=128)
    w_ukv_v = w_ukv.v.rearrange("(kc p) n -> p kc n", p=128)

    wkr = k.sb([128, KC, 64], BF16, "wkr")
    wkrr = k.sb([128, KC, 64], BF16, "wkrr")
    k.dma("pool", wkr.v, w_in_v[:, :, OFF_KR:OFF_KR + 64])
    k.dma("pool", wkrr[:, :, 0:32], w_in_v[:, :, OFF_KR + 32:OFF_KR + 64])
    k.dma("pool", wkrr[:, :, 32:64], w_in_v[:, :, OFF_KR:OFF_KR + 32])
    k.gen("act", lambda q: q.mul(wkrr.base[:, :, 0:32], wkrr.base[:, :, 0:32], -1.0), [wkrr.v], [wkrr.v])
    wukv_g = k.sb([128, 4, 1024], BF16, "wukv_g")
    wuq_g = k.sb([128, 8, 768], BF16, "wuq_g")
    wuqr = k.sb([128, 8, HA, 64], BF16, "wuqr")
    wuq_h = w_uq_v.rearrange("p kc (h c) -> p kc h c", c=192)
    for kc in range(8):
        k.dma("pool", wuqr[:, kc, :, 0:32], wuq_h[:, kc, :, 160:192])
        k.dma("pool", wuqr[:, kc, :, 32:64], wuq_h[:, kc, :, 128:160])
    k.gen("act", lambda q: q.mul(wuqr.base[:, :, :, 0:32], wuqr.base[:, :, :, 0:32], -1.0), [wuqr.v], [wuqr.v])

    xin = Rot([k.sb([128, D], F32, f"xin{i}") for i in range(1)])
    hbf = Rot([k.sb([128, D], BF16, f"hbf{i}") for i in range(1)])
    stat = Rot([k.sb([128, 4], F32, f"stat{i}") for i in range(4)])
    hT = k.sb([128, KC, TT], BF16, "hT")
    wbuf = Rot([k.sb([128, KC, 256], BF16, f"wbuf{i}") for i in range(2)])
    cqT = k.sb([128, 8, TT], F32, "cqT")
    ckvT = k.sb([128, 4, TT], F32, "ckvT")
    cqn = k.sb([128, 8, TT], BF16, "cqn")
    ckvn = k.sb([128, 4, TT], BF16, "ckvn")
    sq32 = Rot([k.sb([128, TT], F32, f"sq32_{i}") for i in range(2)])
    rstd_q = k.sb([128, TT], F32, "rstd_q")
    rstd_kv = k.sb([128, TT], F32, "rstd_kv")
    stg = Rot([k.sb([128, TT], BF16, f"stg{i}") for i in range(6)])
    posi = k.sb([64, TT], I32, "posi")
    posf = k.sb([64, TT], F32, "posf")
    ang = k.sb([64, TT], F32, "ang")
    tmpa = k.sb([64, TT], F32, "tmpa")
    cos2 = k.sb([64, TT], F32, "cos2")
    sin2 = k.sb([64, TT], F32, "sin2")
    r1 = Rot([k.sb([64, TT], F32, f"r1_{i}") for i in range(2)])
    r2 = Rot([k.sb([64, TT], F32, f"r2_{i}") for i in range(2)])
    tmpb = k.sb([64, TT], F32, "tmpb")
    epst = k.sb([128, 1], F32, "epst")
    k.memset("dve", epst.v, EPS)

    ptr = Rot([k.ps([128, 8, 128], BF16, f"ptr{i}") for i in range(2)])
    pacc = Rot([k.ps([128, 512], F32, f"pacc{i}") for i in range(5)])
    pss = k.ps([128, 512], F32, "pss")
    evac_i = [0]

    def evac_copy(out, in_):
        evac_i[0] += 1
        k.copy("dve" if evac_i[0] % 2 else "act", out, in_)

    def rope(pa, pb, dst_dram):
        a1 = r1.next()
        a2 = r2.next()
        k.tt("dve", a1.v, pa, cos2.v, ALU.mult)
        k.tt("dve", a2.v, pb, sin2.v, ALU.mult)
        o = stg.next()
        k.tt("dve", o[0:64, :], a1.v, a2.v, ALU.add)
        k.dma("sp", dst_dram, o[0:64, :])

    for t in range(NT):
        t0 = t * TT
        k.dma("sp", posi.v, pos[0:1, t0:t0 + TT].partition_broadcast(64) if False else pos.v[0:1, t0:t0 + TT].ap.partition_broadcast(64) if False else V(pos.base[0:1, t0:t0 + TT].partition_broadcast(64), pos))
        k.copy("dve", posf.v, posi.v)
        k.ts("dve", ang.v, posf.v, invt[:, 0:1], None, ALU.mult)
        def sin_of(dst, src):
            k.ts("dve", tmpa.v, src, 1.0 / TWO_PI, MAGIC, ALU.mult, ALU.add)
            k.ts("dve", tmpa.v, tmpa.v, MAGIC, None, ALU.subtract)
            k.stt("dve", tmpb.v, tmpa.v, -CW1, src, ALU.mult, ALU.add)
            k.stt("dve", tmpb.v, tmpa.v, -CW2, tmpb.v, ALU.mult, ALU.add)
            k.ts("dve", tmpb.v, tmpb.v, PI_LO, -PI_LO, ALU.min, ALU.max)
            k.act(dst, tmpb.v, AF.Sin)
        sin_of(sin2.v, ang.v)
        k.ts("dve", ang.v, ang.v, PI / 2, None, ALU.add)
        sin_of(cos2.v, ang.v)
        for s in range(TT // 128):
            xt = xin.next()
            k.dma("sp", xt.v, x[t0 + s * 128:t0 + (s + 1) * 128, :])
            st = stat.next()
            hb = hbf.next()
            k.act(hb.v, xt.v, AF.Square, accum_out=st[:, 0:1])
            k.act(st[:, 1:2], st[:, 0:1], AF.Sqrt, scale=1.0 / D, bias=epst[:, 0:1])
            k.recip(st[:, 2:3], st[:, 1:2])
            k.ts("dve", hb.v, xt.v, st[:, 2:3], None, ALU.mult)
            for g8 in range(KC // 8):
                pt = ptr.next()
                for j in range(8):
                    kc = g8 * 8 + j
                    k.tr(pt[:, j, :], hb[:, kc * 128:(kc + 1) * 128], ident.v)
                gb = V(gT.base[:, g8 * 8:(g8 + 1) * 8].unsqueeze(2).to_broadcast([128, 8, 128]), gT)
                k.tt("dve", hT[:, g8 * 8:(g8 + 1) * 8, s * 128:(s + 1) * 128], pt.v, gb, ALU.mult)

        def fm_chunk(wb, j, evac):
            pa = pacc.next()
            for kc in range(KC):
                k.mm(pa.v, wb[:, kc, j * 128:(j + 1) * 128], hT[:, kc, :], start=(kc == 0), stop=(kc == KC - 1))
            evac(pa)

        def stream_fm(col0, ncols, evac_fn):
            for c in range(ncols // 256):
                wb = wbuf.next()
                k.dma("pool", wb.v, w_in_v[:, :, col0 + c * 256:col0 + (c + 1) * 256])
                for j in range(2):
                    fm_chunk(wb, j, lambda pa, idx=c * 2 + j: evac_fn(pa, idx))

        stream_fm(OFF_CQ, QL, lambda pa, idx: evac_copy(cqT[:, idx, :], pa.v))
        stream_fm(OFF_CKV, KVL, lambda pa, idx: evac_copy(ckvT[:, idx, :], pa.v))

        pa = pacc.next()
        pb = pacc.next()
        for kc in range(KC):
            k.mm(pa[0:64, :], wkr[:, kc, :], hT[:, kc, :], start=(kc == 0), stop=(kc == KC - 1))
        for kc in range(KC):
            k.mm(pb[0:64, :], wkrr[:, kc, :], hT[:, kc, :], start=(kc == 0), stop=(kc == KC - 1))
        rope(pa[0:64, :], pb[0:64, :], o_kr[:, t0:t0 + TT])

        def subnorm(srcT, nch, width, gvec, rstd, dst):
            for j in range(nch):
                sq = sq32.next()
                k.tt("pool", sq.v, srcT[:, j, :], srcT[:, j, :], ALU.mult)
                k.mm(pss.v, ones_f.v, sq.v, start=(j == 0), stop=(j == nch - 1))
            k.act(rstd.v, pss.v, AF.Sqrt, scale=1.0 / width, bias=epst[:, 0:1])
            k.recip(rstd.v, rstd.v)
            for j in range(nch):
                k.stt("dve", dst[:, j, :], srcT[:, j, :], gvec[:, j:j + 1], rstd.v, ALU.mult, ALU.mult)

        subnorm(cqT, 8, QL, gqT, rstd_q, cqn)
        subnorm(ckvT, 4, KVL, gkvT, rstd_kv, ckvn)

        for h in range(HA):
            if h % 4 == 0:
                k.dma("pool", wuq_g.v, w_uq_v[:, :, h * 192:(h + 4) * 192])
            hl = h % 4
            pa = pacc.next()
            for kc in range(8):
                k.mm(pa.v, wuq_g[:, kc, hl * 192:hl * 192 + 128], cqn[:, kc, :], start=(kc == 0), stop=(kc == 7))
            o = stg.next()
            evac_copy(o.v, pa.v)
            k.dma("sp", o_qn[h, :, t0:t0 + TT], o.v)
            pa = pacc.next()
            pb = pacc.next()
            for kc in range(8):
                k.mm(pa[0:64, :], wuq_g[:, kc, hl * 192 + 128:hl * 192 + 192], cqn[:, kc, :], start=(kc == 0), stop=(kc == 7))
            for kc in range(8):
                k.mm(pb[0:64, :], wuqr[:, kc, h, :], cqn[:, kc, :], start=(kc == 0), stop=(kc == 7))
            rope(pa[0:64, :], pb[0:64, :], o_qr[h, :, t0:t0 + TT])
        wv = V(wukv_g.base.rearrange("p kc (h two c) -> p kc h two c", two=2, c=128), wukv_g)
        for hg in range(4):
            k.dma("pool", wukv_g.v, w_ukv_v[:, :, hg * 1024:(hg + 1) * 1024])
            for hl in range(4):
                h = hg * 4 + hl
                pa = pacc.next()
                for kc in range(4):
                    k.mm(pa.v, wukv_g[:, kc, hl * 256:hl * 256 + 128], ckvn[:, kc, :], start=(kc == 0), stop=(kc == 3))
                o = stg.next()
                evac_copy(o.v, pa.v)
                k.dma("sp", o_kn[h, :, t0:t0 + TT], o.v)
            for s in range(TT // 128):
                pa = pacc.next()
                pav = V(pa.base.rearrange("p (h c) -> p h c", c=128), pa)
                for kc in range(4):
                    k.mm(pav, ckvn[:, kc, s * 128:(s + 1) * 128], wv[:, kc, :, 1, :],
                         start=(kc == 0), stop=(kc == 3))
                o = stg.next()
                evac_copy(o.v, pa.v)
                k.dma("sp", o_va[t0 + s * 128:t0 + (s + 1) * 128, hg * 512:(hg + 1) * 512], o.v)

        def ev_out(dst):
            def f(pa, idx):
                o = stg.next()
                evac_copy(o.v, pa.v)
                k.dma("sp", dst[idx, :, t0:t0 + TT], o.v)
            return f

        def ev_sig(dst):
            def f(pa, idx):
                o = stg.next()
                k.act(o.v, pa.v, AF.Sigmoid)
                k.dma("sp", dst[idx, :, t0:t0 + TT], o.v)
            return f
        stream_fm(OFF_QB, 2048, ev_out(o_qb))
        stream_fm(OFF_KB, 2048, ev_out(o_kb))
        for c in range(2048 // 256):
            wb = wbuf.next()
            k.dma("pool", wb.v, w_in_v[:, :, OFF_VB + c * 256:OFF_VB + (c + 1) * 256])
            for s in range(TT // 128):
                pa = pacc.next()
                for kc in range(KC):
                    k.mm(pa[:, 0:256], hT[:, kc, s * 128:(s + 1) * 128], wb[:, kc, :], start=(kc == 0), stop=(kc == KC - 1))
                o = stg.next()
                evac_copy(o[:, 0:256], pa[:, 0:256])
                k.dma("sp", o_vb[t0 + s * 128:t0 + (s + 1) * 128, c * 256:(c + 1) * 256], o[:, 0:256])
        stream_fm(OFF_GA, D, ev_sig(o_ga))
        stream_fm(OFF_GB, D, ev_sig(o_gb))
    nc = k.finish()
    return nc, k


def build_t1b(TPC):
    TT = 512
    TB = min(1024, TPC)
    NH = TB // TT
    k = KB()
    x = k.dram("x", [TPC, D], F32, "ExternalInput")
    pos = k.dram("pos", [1, TPC], I32, "ExternalInput")
    invf = k.dram("invf", [64, 1], F32, "ExternalInput")
    g_attn = k.dram("g_attn", [D], F32, "ExternalInput")
    w_in = k.dram("w_in", [D, D_IN], F32, "ExternalInput")
    g_q = k.dram("g_q", [QL], F32, "ExternalInput")
    w_uq = k.dram("w_uq", [QL, HA * 192], F32, "ExternalInput")
    g_kv = k.dram("g_kv", [KVL], F32, "ExternalInput")
    w_ukv = k.dram("w_ukv", [KVL, HA * 256], F32, "ExternalInput")
    o_qn = k.dram("o_qn", [HA, 128, TPC], BF16, "ExternalOutput")
    o_qr = k.dram("o_qr", [HA, 64, TPC], BF16, "ExternalOutput")
    o_kn = k.dram("o_kn", [HA, 128, TPC], BF16, "ExternalOutput")
    o_kr = k.dram("o_kr", [64, TPC], BF16, "ExternalOutput")
    o_va = k.dram("o_va", [TPC, HA * 128], BF16, "ExternalOutput")
    o_qb = k.dram("o_qb", [16, 128, TPC], BF16, "ExternalOutput")
    o_kb = k.dram("o_kb", [16, 128, TPC], BF16, "ExternalOutput")
    o_vb = k.dram("o_vb", [TPC, 2048], BF16, "ExternalOutput")
    o_ga = k.dram("o_ga", [32, 128, TPC], BF16, "ExternalOutput")
    o_gb = k.dram("o_gb", [32, 128, TPC], BF16, "ExternalOutput")

    ident = make_ident(k, BF16, "ident")
    ones_f = k.sb([128, 128], F32, "ones_f")
    k.memset("dve", ones_f.v, 1.0)
    gT = k.sb([128, KC], F32, "gT")
    k.dma("sp", gT.v, g_attn.v.rearrange("(kc p) -> p kc", p=128), allow_slow_non_contiguous=True)
    gqT = k.sb([128, 8], F32, "gqT")
    k.dma("sp", gqT.v, g_q.v.rearrange("(kc p) -> p kc", p=128), allow_slow_non_contiguous=True)
    gkvT = k.sb([128, 4], F32, "gkvT")
    k.dma("sp", gkvT.v, g_kv.v.rearrange("(kc p) -> p kc", p=128), allow_slow_non_contiguous=True)
    invt = k.sb([64, 1], F32, "invt")
    k.dma("sp", invt.v, invf.v)

    w_in_v = w_in.v.rearrange("(kc p) n -> p kc n", p=128)
    w_uq_v = w_uq.v.rearrange("(kc p) n -> p kc n", p=128)
    w_ukv_v = w_ukv.v.rearrange("(kc p) n -> p kc n", p=128)

    wkr = k.sb([128, KC, 64], BF16, "wkr")
    wkrr = k.sb([128, KC, 64], BF16, "wkrr")
    k.dma("pool", wkr.v, w_in_v[:, :, OFF_KR:OFF_KR + 64])
    k.dma("pool", wkrr[:, :, 0:32], w_in_v[:, :, OFF_KR + 32:OFF_KR + 64])
    k.dma("pool", wkrr[:, :, 32:64], w_in_v[:, :, OFF_KR:OFF_KR + 32])
    k.gen("act", lambda q: q.mul(wkrr.base[:, :, 0:32], wkrr.base[:, :, 0:32], -1.0), [wkrr.v], [wkrr.v])
    wukv_g = k.sb([128, 4, 1024], BF16, "wukv_g")
    wuq_g = k.sb([128, 8, 384], BF16, "wuq_g")
    wuqr = k.sb([128, 8, 2, 64], BF16, "wuqr")
    wuq_h = w_uq_v.rearrange("p kc (h c) -> p kc h c", c=192)

    def load_wuqr(h0):
        for hh in range(2):
            k.dma("pool", wuqr[:, :, hh, 0:32], wuq_h[:, :, h0 + hh, 160:192])
            k.dma("pool", wuqr[:, :, hh, 32:64], wuq_h[:, :, h0 + hh, 128:160])
        k.gen("act", lambda q: q.mul(wuqr.base[:, :, :, 0:32], wuqr.base[:, :, :, 0:32], -1.0), [wuqr.v], [wuqr.v])
    xin = Rot([k.sb([128, D], F32, f"xin{i}") for i in range(1)])
    hbf = Rot([k.sb([128, D], BF16, f"hbf{i}") for i in range(1)])
    stat = Rot([k.sb([128, 4], F32, f"stat{i}") for i in range(4)])
    hT = k.sb([128, KC, TB], BF16, "hT")
    wbuf = Rot([k.sb([128, KC, 256], BF16, f"wbuf{i}") for i in range(2)])
    cqT = k.sb([128, 8, TT], F32, "cqT")
    ckvT = k.sb([128, 4, TT], F32, "ckvT")
    cqn = k.sb([128, 8, TT], BF16, "cqn")
    ckvn = k.sb([128, 4, TT], BF16, "ckvn")
    sq32 = Rot([k.sb([128, TT], F32, f"sq32_{i}") for i in range(1)])
    rstd_q = k.sb([128, TT], F32, "rstd_q")
    rstd_kv = rstd_q
    stg = Rot([k.sb([128, TT], BF16, f"stg{i}") for i in range(4)])
    posi = k.sb([64, TT], I32, "posi")
    ang = k.sb([64, TT], F32, "ang")
    tmpa = k.sb([64, TT], F32, "tmpa")
    cos2 = k.sb([64, TT], F32, "cos2")
    sin2 = k.sb([64, TT], F32, "sin2")
    r1 = Rot([k.sb([64, TT], F32, f"r1_{i}") for i in range(1)])
    r2 = Rot([k.sb([64, TT], F32, f"r2_{i}") for i in range(1)])
    tmpb = k.sb([64, TT], F32, "tmpb")
    epst = k.sb([128, 1], F32, "epst")
    k.memset("dve", epst.v, EPS)

    ptr = Rot([k.ps([128, 8, 128], BF16, f"ptr{i}") for i in range(2)])
    pacc = Rot([k.ps([128, 512], F32, f"pacc{i}") for i in range(5)])
    pss = k.ps([128, 512], F32, "pss")
    evac_i = [0]

    def evac_copy(out, in_):
        evac_i[0] += 1
        k.copy("dve" if evac_i[0] % 2 else "act", out, in_)

    def rope(pa, pb, dst_dram):
        a1 = r1.next()
        a2 = r2.next()
        k.tt("dve", a1.v, pa, cos2.v, ALU.mult)
        k.tt("dve", a2.v, pb, sin2.v, ALU.mult)
        o = stg.next()
        k.tt("dve", o[0:64, :], a1.v, a2.v, ALU.add)
        k.dma("sp", dst_dram, o[0:64, :])

    for tb in range(TPC // TB):
        tb0 = tb * TB
        for hf_ in range(NH):
            t0 = tb0 + hf_ * TT
            ho = hf_ * TT
            k.dma("sp", posi.v, pos[0:1, t0:t0 + TT].partition_broadcast(64) if False else pos.v[0:1, t0:t0 + TT].ap.partition_broadcast(64) if False else V(pos.base[0:1, t0:t0 + TT].partition_broadcast(64), pos))
            k.copy("dve", ang.v, posi.v)
            k.ts("dve", ang.v, ang.v, invt[:, 0:1], None, ALU.mult)
            def sin_of(dst, src):
                k.ts("dve", tmpa.v, src, 1.0 / TWO_PI, MAGIC, ALU.mult, ALU.add)
                k.ts("dve", tmpa.v, tmpa.v, MAGIC, None, ALU.subtract)
                k.stt("dve", tmpb.v, tmpa.v, -CW1, src, ALU.mult, ALU.add)
                k.stt("dve", tmpb.v, tmpa.v, -CW2, tmpb.v, ALU.mult, ALU.add)
                k.ts("dve", tmpb.v, tmpb.v, PI_LO, -PI_LO, ALU.min, ALU.max)
                k.act(dst, tmpb.v, AF.Sin)
            sin_of(sin2.v, ang.v)
            k.ts("dve", ang.v, ang.v, PI / 2, None, ALU.add)
            sin_of(cos2.v, ang.v)
            for s in range(TT // 128):
                xt = xin.next()
                k.dma("sp", xt.v, x[t0 + s * 128:t0 + (s + 1) * 128, :])
                st = stat.next()
                hb = hbf.next()
                k.act(hb.v, xt.v, AF.Square, accum_out=st[:, 0:1])
                k.act(st[:, 1:2], st[:, 0:1], AF.Sqrt, scale=1.0 / D, bias=epst[:, 0:1])
                k.recip(st[:, 2:3], st[:, 1:2])
                k.ts("dve", hb.v, xt.v, st[:, 2:3], None, ALU.mult)
                for g8 in range(KC // 8):
                    pt = ptr.next()
                    for j in range(8):
                        kc = g8 * 8 + j
                        k.tr(pt[:, j, :], hb[:, kc * 128:(kc + 1) * 128], ident.v)
                    gb = V(gT.base[:, g8 * 8:(g8 + 1) * 8].unsqueeze(2).to_broadcast([128, 8, 128]), gT)
                    k.tt("dve", hT[:, g8 * 8:(g8 + 1) * 8, ho + s * 128:ho + (s + 1) * 128], pt.v, gb, ALU.mult)

            def fm_chunk(wb, j, evac):
                pa = pacc.next()
                for kc in range(KC):
                    k.mm(pa.v, wb[:, kc, j * 128:(j + 1) * 128], hT[:, kc, ho:ho + TT], start=(kc == 0), stop=(kc == KC - 1))
                evac(pa)

            def stream_fm(col0, ncols, evac_fn):
                for c in range(ncols // 256):
                    wb = wbuf.next()
                    k.dma("pool", wb.v, w_in_v[:, :, col0 + c * 256:col0 + (c + 1) * 256])
                    for j in range(2):
                        fm_chunk(wb, j, lambda pa, idx=c * 2 + j: evac_fn(pa, idx))

            stream_fm(OFF_CQ, QL, lambda pa, idx: evac_copy(cqT[:, idx, :], pa.v))
            stream_fm(OFF_CKV, KVL, lambda pa, idx: evac_copy(ckvT[:, idx, :], pa.v))

            pa = pacc.next()
            pb = pacc.next()
            for kc in range(KC):
                k.mm(pa[0:64, :], wkr[:, kc, :], hT[:, kc, ho:ho + TT], start=(kc == 0), stop=(kc == KC - 1))
            for kc in range(KC):
                k.mm(pb[0:64, :], wkrr[:, kc, :], hT[:, kc, ho:ho + TT], start=(kc == 0), stop=(kc == KC - 1))
            rope(pa[0:64, :], pb[0:64, :], o_kr[:, t0:t0 + TT])

            def subnorm(srcT, nch, width, gvec, rstd, dst):
                for j in range(nch):
                    sq = sq32.next()
                    k.tt("pool", sq.v, srcT[:, j, :], srcT[:, j, :], ALU.mult)
                    k.mm(pss.v, ones_f.v, sq.v, start=(j == 0), stop=(j == nch - 1))
                k.act(rstd.v, pss.v, AF.Sqrt, scale=1.0 / width, bias=epst[:, 0:1])
                k.recip(rstd.v, rstd.v)
                for j in range(nch):
                    k.stt("dve", dst[:, j, :], srcT[:, j, :], gvec[:, j:j + 1], rstd.v, ALU.mult, ALU.mult)

            subnorm(cqT, 8, QL, gqT, rstd_q, cqn)
            subnorm(ckvT, 4, KVL, gkvT, rstd_kv, ckvn)

            for h in range(HA):
                if h % 2 == 0:
                    k.dma("pool", wuq_g.v, w_uq_v[:, :, h * 192:(h + 2) * 192])
                    load_wuqr(h)
                hl = h % 2
                pa = pacc.next()
                for kc in range(8):
                    k.mm(pa.v, wuq_g[:, kc, hl * 192:hl * 192 + 128], cqn[:, kc, :], start=(kc == 0), stop=(kc == 7))
                o = stg.next()
                evac_copy(o.v, pa.v)
                k.dma("sp", o_qn[h, :, t0:t0 + TT], o.v)
                pa = pacc.next()
                pb = pacc.next()
                for kc in range(8):
                    k.mm(pa[0:64, :], wuq_g[:, kc, hl * 192 + 128:hl * 192 + 192], cqn[:, kc, :], start=(kc == 0), stop=(kc == 7))
                for kc in range(8):
                    k.mm(pb[0:64, :], wuqr[:, kc, hl, :], cqn[:, kc, :], start=(kc == 0), stop=(kc == 7))
                rope(pa[0:64, :], pb[0:64, :], o_qr[h, :, t0:t0 + TT])
            wv = V(wukv_g.base.rearrange("p kc (h two c) -> p kc h two c", two=2, c=128), wukv_g)
            for hg in range(4):
                k.dma("pool", wukv_g.v, w_ukv_v[:, :, hg * 1024:(hg + 1) * 1024])
                for hl in range(4):
                    h = hg * 4 + hl
                    pa = pacc.next()
                    for kc in range(4):
                        k.mm(pa.v, wukv_g[:, kc, hl * 256:hl * 256 + 128], ckvn[:, kc, :], start=(kc == 0), stop=(kc == 3))
                    o = stg.next()
                    evac_copy(o.v, pa.v)
                    k.dma("sp", o_kn[h, :, t0:t0 + TT], o.v)
                for s in range(TT // 128):
                    pa = pacc.next()
                    pav = V(pa.base.rearrange("p (h c) -> p h c", c=128), pa)
                    for kc in range(4):
                        k.mm(pav, ckvn[:, kc, s * 128:(s + 1) * 128], wv[:, kc, :, 1, :],
                             start=(kc == 0), stop=(kc == 3))
                    o = stg.next()
                    evac_copy(o.v, pa.v)
                    k.dma("sp", o_va[t0 + s * 128:t0 + (s + 1) * 128, hg * 512:(hg + 1) * 512], o.v)

        def ev_out(dst):
            def f(pa, idx, t0):
                o = stg.next()
                evac_copy(o.v, pa.v)
                k.dma("sp", dst[idx, :, t0:t0 + TT], o.v)
            return f

        def ev_sig(dst):
            def f(pa, idx, t0):
                o = stg.next()
                k.act(o.v, pa.v, AF.Sigmoid)
                k.dma("sp", dst[idx, :, t0:t0 + TT], o.v)
            return f

        def stream_bulk(col0, ncols, evac_fn):
            for c in range(ncols // 256):
                wb = wbuf.next()
                k.dma("pool", wb.v, w_in_v[:, :, col0 + c * 256:col0 + (c + 1) * 256])
                for hf in range(NH):
                    ho = hf * TT
                    for j in range(2):
                        pa = pacc.next()
                        for kc in range(KC):
                            k.mm(pa.v, wb[:, kc, j * 128:(j + 1) * 128], hT[:, kc, ho:ho + TT], start=(kc == 0), stop=(kc == KC - 1))
                        evac_fn(pa, c * 2 + j, tb0 + ho)
        stream_bulk(OFF_QB, 2048, ev_out(o_qb))
        stream_bulk(OFF_KB, 2048, ev_out(o_kb))
        for c in range(2048 // 256):
            wb = wbuf.next()
            k.dma("pool", wb.v, w_in_v[:, :, OFF_VB + c * 256:OFF_VB + (c + 1) * 256])
            for s in range(TB // 128):
                pa = pacc.next()
                for kc in range(KC):
                    k.mm(pa[:, 0:256], hT[:, kc, s * 128:(s + 1) * 128], wb[:, kc, :], start=(kc == 0), stop=(kc == KC - 1))
                o = stg.next()
                evac_copy(o[:, 0:256], pa[:, 0:256])
                k.dma("sp", o_vb[tb0 + s * 128:tb0 + (s + 1) * 128, c * 256:(c + 1) * 256], o[:, 0:256])
        stream_bulk(OFF_GA, D, ev_sig(o_ga))
        stream_bulk(OFF_GB, D, ev_sig(o_gb))
    nc = k.finish()
    return nc, k


def build_attn(S):
    NB = S // 128
    NQ = S // 512
    k = KB()
    qn = k.dram("qn", [2, 128, S], BF16, "ExternalInput")
    qr = k.dram("qr", [2, 64, S], BF16, "ExternalInput")
    kn = k.dram("kn", [2, 128, S], BF16, "ExternalInput")
    kr = k.dram("kr", [64, S], BF16, "ExternalInput")
    va = k.dram("va", [2, 128, NB, 128], BF16, "ExternalInput")
    qb = k.dram("qb", [2, 128, S], BF16, "ExternalInput")
    kb = k.dram("kb", [2, 128, S], BF16, "ExternalInput")
    vb = k.dram("vb", [128, NB, 256], BF16, "ExternalInput")
    pos = k.dram("pos", [1, S], I32, "ExternalInput")
    posk = k.dram("posk", [128, NB], I32, "ExternalInput")
    lam = k.dram("lam", [1, 4, 128], F32, "ExternalInput")
    subln = k.dram("subln", [256], F32, "ExternalInput")
    consts = k.dram("consts", [128, 4], F32, "ExternalInput")
    oa = k.dram("oa", [2, 128, S], BF16, "ExternalOutput")
    ob = k.dram("ob", [2, 128, S], BF16, "ExternalOutput")

    SC_A = float((QKN + QKR) ** -0.5)
    SC_B = float(DHB ** -0.5)

    ones_b = k.sb([128, 128], BF16, "ones_b")
    k.memset("dve", ones_b.v, 1.0)
    ones_f = k.sb([128, 128], F32, "ones_f")
    k.memset("dve", ones_f.v, 1.0)
    tri_f = k.sb([128, 128], F32, "tri_f")
    k.memset("dve", tri_f.v, 1.0)
    k.gen("pool", lambda q: q.affine_select(tri_f.base, tri_f.base, [[1, 128]], ALU.is_ge, 0.0, base=0,
                                            channel_multiplier=-1), [tri_f.v], [tri_f.v])
    tri = k.sb([128, 128], BF16, "tri")
    k.copy("dve", tri.v, tri_f.v)
    epst = k.sb([128, 1], F32, "epst")
    k.memset("dve", epst.v, EPS)

    cst = k.sb([128, 4], F32, "cst")
    k.dma("sp", cst.v, consts.v)
    lamt = k.sb([1, 4, 128], F32, "lamt")
    k.dma("sp", lamt.v, lam.v)
    lw = k.sb([1, 8], F32, "lw")
    lj = k.sb([1, 128], F32, "lj")
    k.tt("dve", lj.v, lamt[:, 0, :], lamt[:, 1, :], ALU.mult)
    k.reduce("dve", lw[:, 0:1], lj.v, ALU.add)
    k.tt("dve", lj.v, lamt[:, 2, :], lamt[:, 3, :], ALU.mult)
    k.reduce("dve", lw[:, 1:2], lj.v, ALU.add)
    k.act(lw[:, 2:4], lw[:, 0:2], AF.Exp)
    k.tt("dve", lw[:, 4:5], lw[:, 2:3], lw[:, 3:4], ALU.subtract)
    k.tt("dve", lw[:, 5:6], lw[:, 4:5], cst[0:1, 1:2], ALU.add)
    k.ts("dve", lw[:, 6:7], lw[:, 5:6], -1.0, None, ALU.mult)
    pmisc = k.ps([128, 512], F32, "pmisc")
    k.mm(pmisc[:, 0:1], ones_f[0:1, :], lw[0:1, 6:7])
    neglam = k.sb([128, 1], F32, "neglam")
    k.copy("dve", neglam.v, pmisc[:, 0:1])
    subs = k.sb([128, 2], F32, "subs")
    k.dma("sp", subs.v, subln.v.rearrange("(c p) -> p c", p=128), allow_slow_non_contiguous=True)
    k.ts("dve", subs.v, subs.v, cst[:, 2:3], None, ALU.mult)
    pk_i = k.sb([128, NB], I32, "pk_i")
    k.dma("sp", pk_i.v, posk.v)
    pos0_i = k.sb([128, 1], I32, "pos0_i")
    k.dma("sp", pos0_i.v, V(pos.base[0:1, 0:1].partition_broadcast(128), pos))
    pos0 = k.sb([128, 1], F32, "pos0")
    k.copy("dve", pos0.v, pos0_i.v)
    mps = k.sb([128, 2], F32, "mps")
    k.ts("dve", mps[:, 0:1], cst[:, 0:1], 1.0 / SC_B, None, ALU.mult)
    k.ts("dve", mps[:, 1:2], cst[:, 0:1], -1.0 / SC_B, None, ALU.mult)
    pks = k.sb([128, NB], F32, "pks")
    k.copy("dve", pks.v, pk_i.v)
    k.ts("dve", pks.v, pks.v, pos0[:, 0:1], mps[:, 0:1], ALU.subtract, ALU.mult)

    bufK = k.sb([128, S], BF16, "bufK")
    bufKR = k.sb([64, S], BF16, "bufKR")
    bufV = k.sb([128, NB, 256], BF16, "bufV")
    k.dma("sp", bufKR.v, kr.v)

    qt_a = Rot([k.sb([128, 512], BF16, f"qt_a{i}") for i in range(2)])
    qt_r = Rot([k.sb([64, 512], BF16, f"qt_r{i}") for i in range(2)])
    pT = Rot([k.sb([128, 512], BF16, f"pT{i}") for i in range(4)])
    sfp = Rot([k.sb([128, 512], F32, f"sfp{i}") for i in range(2)])
    qbs_i = k.sb([128, 512], I32, "qbs_i")
    qbs = k.sb([128, 512], F32, "qbs")
    rec = k.sb([128, 512], F32, "rec")
    o1 = k.sb([128, 2, 512], F32, "o1")
    o2 = k.sb([128, 2, 512], F32, "o2")
    sqt = Rot([k.sb([128, 512], F32, f"sqt{i}") for i in range(2)])
    rstd = k.sb([128, 512], F32, "rstd")
    ostg = Rot([k.sb([128, 512], BF16, f"ostg{i}") for i in range(3)])

    psS = Rot([k.ps([128, 512], F32, f"psS{i}") for i in range(2)])
    po = [k.ps([128, 512], F32, f"po{i}") for i in range(2)]
    pd = k.ps([128, 512], F32, "pd")

    def run_pass(j, ndv, qk_fn, exp_fn):
        q0 = j * 512
        nkb = 4 * (j + 1)
        pend = None

        def flush(pp):
            (P, i, c0) = pp
            for c in range(ndv):
                k.mm(po[c][:, c0:], bufV[:, i, c * 128:(c + 1) * 128], P[:, c0:], start=(i == 0), stop=(i == nkb - 1))
            k.mm(pd[:, c0:], ones_b.v, P[:, c0:], start=(i == 0), stop=(i == nkb - 1))
        for i in range(nkb):
            c0 = max(0, 128 * (i - 4 * j))
            ps = psS.next()
            qk_fn(ps, i, c0)
            P = pT.next()
            exp_fn(P, ps, i, c0)
            if i >= 4 * j:
                k.tt("pool", P[:, c0:c0 + 128], P[:, c0:c0 + 128], tri.v, ALU.mult)
            if pend is not None:
                flush(pend)
            pend = (P, i, c0)
        flush(pend)

    for h in range(2):
        k.dma("sp", bufK.v, kn[h])
        k.dma("sp", bufV[:, :, 0:128], va[h])
        for j in range(NQ):
            q0 = j * 512
            qa_t = qt_a.next()
            qr_t = qt_r.next()
            k.dma("sp", qa_t.v, qn[h, :, q0:q0 + 512])
            k.dma("sp", qr_t.v, qr[h, :, q0:q0 + 512])

            def qk_fn(ps, i, c0):
                k.mm(ps[:, c0:], bufK[:, i * 128:(i + 1) * 128], qa_t[:, c0:], start=True, stop=False)
                k.mm(ps[:, c0:], bufKR[:, i * 128:(i + 1) * 128], qr_t[:, c0:], start=False, stop=True)

            def exp_fn(P, ps, i, c0):
                k.act(P[:, c0:], ps[:, c0:], AF.Exp, scale=SC_A)
            run_pass(j, 1, qk_fn, exp_fn)
            k.recip(rec.v, pd.v)
            o = ostg.next()
            k.tt("dve", o.v, po[0].v, rec.v, ALU.mult)
            k.dma("sp", oa[h, :, q0:q0 + 512], o.v)

    k.dma("sp", bufV.v, vb.v)
    osave = [o1, o2]
    bufK2 = bufK
    kmaps = [bufK, None]
    bufKb2 = k.sb([128, S], BF16, "bufKb2")
    kmaps[1] = bufKb2
    k.dma("sp", bufK.v, kb[0])
    k.dma("sp", bufKb2.v, kb[1])
    for j in range(NQ):
        q0 = j * 512
        k.dma("sp", qbs_i.v, V(pos.base[0:1, q0:q0 + 512].partition_broadcast(128), pos))
        k.copy("dve", qbs.v, qbs_i.v)
        k.ts("dve", qbs.v, qbs.v, pos0[:, 0:1], mps[:, 1:2], ALU.subtract, ALU.mult)
        for m in range(2):
            qa_t = qt_a.next()
            k.dma("sp", qa_t.v, qb[m, :, q0:q0 + 512])
            Kb = kmaps[m]

            def qk_fn(ps, i, c0):
                k.mm(ps[:, c0:], Kb[:, i * 128:(i + 1) * 128], qa_t[:, c0:], start=True, stop=True)

            def exp_fn(P, ps, i, c0):
                sf = sfp.next()
                k.stt("dve", sf[:, c0:], ps[:, c0:], pks[:, i:i + 1], qbs[:, c0:], ALU.add, ALU.add)
                k.act(P[:, c0:], sf[:, c0:], AF.Exp, scale=SC_B)
            run_pass(j, 2, qk_fn, exp_fn)
            k.recip(rec.v, pd.v)
            for c in range(2):
                k.tt("dve", osave[m][:, c, :], po[c].v, rec.v, ALU.mult)
        for c in range(2):
            k.stt("dve", o1[:, c, :], o2[:, c, :], neglam[:, 0:1], o1[:, c, :], ALU.mult, ALU.add)
            sq = sqt.next()
            k.tt("pool", sq.v, o1[:, c, :], o1[:, c, :], ALU.mult)
            k.mm(pmisc.v, ones_f.v, sq.v, start=(c == 0), stop=(c == 1))
        k.act(rstd.v, pmisc.v, AF.Sqrt, scale=1.0 / 256, bias=epst[:, 0:1])
        k.recip(rstd.v, rstd.v)
        for c in range(2):
            o = ostg.next()
            k.stt("dve", o.v, o1[:, c, :], subs[:, c:c + 1], rstd.v, ALU.mult, ALU.mult)
            k.dma("sp", ob[c, :, q0:q0 + 512], o.v)
    nc = k.finish()
    return nc, k


def build_attn2(S):
    NB = S // 128
    NQ = S // 512
    k = KB()
    qn = k.dram("qn", [2, 128, S], BF16, "ExternalInput")
    qr = k.dram("qr", [2, 64, S], BF16, "ExternalInput")
    kn = k.dram("kn", [2, 128, S], BF16, "ExternalInput")
    kr = k.dram("kr", [64, S], BF16, "ExternalInput")
    va = k.dram("va", [2, 128, NB, 128], BF16, "ExternalInput")
    qb = k.dram("qb", [2, 128, S], BF16, "ExternalInput")
    kb = k.dram("kb", [2, 128, S], BF16, "ExternalInput")
    vb = k.dram("vb", [128, NB, 256], BF16, "ExternalInput")
    pos = k.dram("pos", [1, S], I32, "ExternalInput")
    posk = k.dram("posk", [128, NB], I32, "ExternalInput")
    lam = k.dram("lam", [1, 4, 128], F32, "ExternalInput")
    subln = k.dram("subln", [256], F32, "ExternalInput")
    consts = k.dram("consts", [128, 4], F32, "ExternalInput")
    oa = k.dram("oa", [2, 128, S], BF16, "ExternalOutput")
    ob = k.dram("ob", [2, 128, S], BF16, "ExternalOutput")

    SC_A = float((QKN + QKR) ** -0.5)
    SC_B = float(DHB ** -0.5)

    ones_b = k.sb([128, 128], BF16, "ones_b")
    k.memset("dve", ones_b.v, 1.0)
    ones_f = k.sb([128, 128], F32, "ones_f")
    k.memset("dve", ones_f.v, 1.0)
    tri_f = k.sb([128, 128], F32, "tri_f")
    k.memset("dve", tri_f.v, 1.0)
    k.gen("pool", lambda q: q.affine_select(tri_f.base, tri_f.base, [[1, 128]], ALU.is_ge, 0.0, base=0,
                                            channel_multiplier=-1), [tri_f.v], [tri_f.v])
    tri = k.sb([128, 128], BF16, "tri")
    k.copy("dve", tri.v, tri_f.v)
    epst = k.sb([128, 1], F32, "epst")
    k.memset("dve", epst.v, EPS)

    cst = k.sb([128, 4], F32, "cst")
    k.dma("sp", cst.v, consts.v)
    lamt = k.sb([1, 4, 128], F32, "lamt")
    k.dma("sp", lamt.v, lam.v)
    lw = k.sb([1, 8], F32, "lw")
    lj = k.sb([1, 128], F32, "lj")
    k.tt("dve", lj.v, lamt[:, 0, :], lamt[:, 1, :], ALU.mult)
    k.reduce("dve", lw[:, 0:1], lj.v, ALU.add)
    k.tt("dve", lj.v, lamt[:, 2, :], lamt[:, 3, :], ALU.mult)
    k.reduce("dve", lw[:, 1:2], lj.v, ALU.add)
    k.act(lw[:, 2:4], lw[:, 0:2], AF.Exp)
    k.tt("dve", lw[:, 4:5], lw[:, 2:3], lw[:, 3:4], ALU.subtract)
    k.tt("dve", lw[:, 5:6], lw[:, 4:5], cst[0:1, 1:2], ALU.add)
    k.ts("dve", lw[:, 6:7], lw[:, 5:6], -1.0, None, ALU.mult)
    pmisc = k.ps([128, 512], F32, "pmisc")
    k.mm(pmisc[:, 0:1], ones_f[0:1, :], lw[0:1, 6:7])
    neglam = k.sb([128, 1], F32, "neglam")
    k.copy("dve", neglam.v, pmisc[:, 0:1])
    subs = k.sb([128, 2], F32, "subs")
    k.dma("sp", subs.v, subln.v.rearrange("(c p) -> p c", p=128), allow_slow_non_contiguous=True)
    k.ts("dve", subs.v, subs.v, cst[:, 2:3], None, ALU.mult)
    pk_i = k.sb([128, NB], I32, "pk_i")
    k.dma("sp", pk_i.v, posk.v)
    pos0_i = k.sb([128, 1], I32, "pos0_i")
    k.dma("sp", pos0_i.v, V(pos.base[0:1, 0:1].partition_broadcast(128), pos))
    pos0 = k.sb([128, 1], F32, "pos0")
    k.copy("dve", pos0.v, pos0_i.v)
    mps = k.sb([128, 2], F32, "mps")
    k.ts("dve", mps[:, 0:1], cst[:, 0:1], 1.0 / SC_B, None, ALU.mult)
    k.ts("dve", mps[:, 1:2], cst[:, 0:1], -1.0 / SC_B, None, ALU.mult)
    pks = k.sb([128, NB], F32, "pks")
    k.copy("dve", pks.v, pk_i.v)
    k.ts("dve", pks.v, pks.v, pos0[:, 0:1], mps[:, 0:1], ALU.subtract, ALU.mult)

    bufK = k.sb([128, S], BF16, "bufK")
    bufKR_full = k.sb([128, S], BF16, "bufKR")
    bufKR = V(bufKR_full.base[0:64, :], bufKR_full)
    bufV = k.sb([128, NB, 256], BF16, "bufV")
    k.memset("dve", bufKR_full[64:128, :], 0.0)
    k.dma("sp", bufKR, kr.v)

    qt_a = Rot([k.sb([128, 512], BF16, f"qt_a{i}") for i in range(2)])
    qt_r = Rot([k.sb([128, 512], BF16, f"qt_r{i}") for i in range(2)])
    for b_ in qt_r.bufs:
        k.memset("dve", b_[64:128, :], 0.0)
    pT = Rot([k.sb([128, 512], BF16, f"pT{i}") for i in range(6)])
    dacc = [k.sb([128, 512], F32, f"dacc{i}") for i in range(2)]
    dsum = k.sb([128, 512], F32, "dsum")
    wq = k.sb([128, 512], F32, "wq")
    qrel = k.sb([128, 512], F32, "qrel")
    ref_i = k.sb([128, 1], I32, "ref_i")
    reff = k.sb([128, 1], F32, "reff")
    biasj = k.sb([128, NB], F32, "biasj")
    pkf = k.sb([128, NB], F32, "pkf")
    k.copy("dve", pkf.v, pk_i.v)
    negm = k.sb([128, 1], F32, "negm")
    k.ts("dve", negm.v, cst[:, 0:1], -1.0, None, ALU.mult)
    otmp = Rot([k.sb([128, 512], F32, f"otmp{i}") for i in range(2)])
    sfp = Rot([k.sb([128, 512], F32, f"sfp{i}") for i in range(2)])
    qbs_i = k.sb([128, 512], I32, "qbs_i")
    qbs = k.sb([128, 512], F32, "qbs")
    rec = k.sb([128, 512], F32, "rec")
    o1 = k.sb([128, 2, 512], F32, "o1")
    o2 = k.sb([128, 2, 512], F32, "o2")
    sqt = Rot([k.sb([128, 512], F32, f"sqt{i}") for i in range(2)])
    rstd = k.sb([128, 512], F32, "rstd")
    ostg = Rot([k.sb([128, 512], BF16, f"ostg{i}") for i in range(3)])

    psS = Rot([k.ps([128, 512], F32, f"psS{i}") for i in range(3)])
    po = [k.ps([128, 512], F32, f"po{i}") for i in range(2)]
    po_d = [k.ps([128, 512], F32, f"po_d{i}") for i in range(2)]

    def run_blocks(j, blks, ndv, pacc_, dac, qk_fn, exp_fn):
        nb_ = len(blks)
        pend = []

        def flush(pp):
            (P, i, c0, idx) = pp
            for c in range(ndv):
                k.mm(pacc_[c][:, c0:], bufV[:, i, c * 128:(c + 1) * 128], P[:, c0:], start=(idx == 0), stop=(idx == nb_ - 1))
        for idx, i in enumerate(blks):
            c0 = max(0, 128 * (i - 4 * j))
            ps = psS.next()
            qk_fn(ps, i, c0)
            P = pT.next()
            exp_fn(P, ps, i, c0)
            if i >= 4 * j:
                k.tt("pool", P[:, c0:c0 + 128], P[:, c0:c0 + 128], tri.v, ALU.mult)
            if idx == 0:
                k.copy("dve", dac.v, P.v)
            else:
                k.tt("dve", dac[:, c0:], dac[:, c0:], P[:, c0:], ALU.add)
            pend.append((P, i, c0, idx))
            if len(pend) > 2:
                flush(pend.pop(0))
        for pp in pend:
            flush(pp)

    for h in range(2):
        k.dma("sp", bufK.v, kn[h])
        k.dma("sp", bufV[:, :, 0:128], va[h])
        for j in range(NQ):
            q0 = j * 512
            qa_t = qt_a.next()
            qr_t = qt_r.next()
            k.dma("sp", qa_t.v, qn[h, :, q0:q0 + 512])
            k.dma("sp", qr_t[0:64, :], qr[h, :, q0:q0 + 512])

            def qk_fn(ps, i, c0):
                k.mm(ps[:, c0:], bufK[:, i * 128:(i + 1) * 128], qa_t[:, c0:], start=True, stop=False)
                k.mm(ps[:, c0:], bufKR_full[:, i * 128:(i + 1) * 128], qr_t[:, c0:], start=False, stop=True)

            def exp_fn(P, ps, i, c0):
                k.act(P[:, c0:], ps[:, c0:], AF.Exp, scale=SC_A)
            run_blocks(j, list(range(4 * (j + 1))), 1, po, dacc[0], qk_fn, exp_fn)
            k.mm(pmisc.v, ones_f.v, dacc[0].v)
            k.recip(rec.v, pmisc.v)
            o = ostg.next()
            k.tt("dve", o.v, po[0].v, rec.v, ALU.mult)
            k.dma("sp", oa[h, :, q0:q0 + 512], o.v)

    k.dma("sp", bufV.v, vb.v)
    osave = [o1, o2]
    bufK2 = bufK
    kmaps = [bufK, None]
    bufKb2 = bufKR_full
    kmaps[1] = bufKb2
    k.dma("sp", bufK.v, kb[0])
    k.dma("sp", bufKb2.v, kb[1])
    for j in range(NQ):
        q0 = j * 512
        k.dma("sp", qbs_i.v, V(pos.base[0:1, q0:q0 + 512].partition_broadcast(128), pos))
        k.copy("dve", qbs.v, qbs_i.v)
        k.dma("sp", ref_i.v, V(pos.base[0:1, q0:q0 + 1].partition_broadcast(128), pos))
        k.copy("dve", reff.v, ref_i.v)
        k.ts("dve", qrel.v, qbs.v, reff[:, 0:1], None, ALU.subtract)
        k.act(wq.v, qrel.v, AF.Exp, scale=negm[:, 0:1])
        if j > 0:
            k.ts("dve", biasj[:, 0:4 * j], pkf[:, 0:4 * j], reff[:, 0:1], cst[:, 0:1], ALU.subtract, ALU.mult)
        k.ts("dve", qbs.v, qbs.v, pos0[:, 0:1], mps[:, 1:2], ALU.subtract, ALU.mult)
        for m in range(2):
            qa_t = qt_a.next()
            k.dma("sp", qa_t.v, qb[m, :, q0:q0 + 512])
            Kb = kmaps[m]

            def qk_fn(ps, i, c0):
                k.mm(ps[:, c0:], Kb[:, i * 128:(i + 1) * 128], qa_t[:, c0:], start=True, stop=True)

            def exp_d(P, ps, i, c0):
                sf = sfp.next()
                k.stt("dve", sf[:, c0:], ps[:, c0:], pks[:, i:i + 1], qbs[:, c0:], ALU.add, ALU.add)
                k.act(P[:, c0:], sf[:, c0:], AF.Exp, scale=SC_B)

            def exp_nd(P, ps, i, c0):
                k.act(P.v, ps.v, AF.Exp, scale=SC_B, bias=biasj[:, i:i + 1])
            if j > 0:
                run_blocks(j, list(range(4 * j)), 2, po, dacc[0], qk_fn, exp_nd)
            run_blocks(j, list(range(4 * j, 4 * j + 4)), 2, po_d, dacc[1], qk_fn, exp_d)
            if j > 0:
                k.tt("dve", dsum.v, dacc[0].v, wq.v, ALU.mult)
                k.tt("dve", dsum.v, dsum.v, dacc[1].v, ALU.add)
                k.mm(pmisc.v, ones_f.v, dsum.v)
            else:
                k.mm(pmisc.v, ones_f.v, dacc[1].v)
            k.recip(rec.v, pmisc.v)
            for c in range(2):
                if j > 0:
                    ot = otmp.next()
                    k.tt("dve", ot.v, po[c].v, wq.v, ALU.mult)
                    k.tt("dve", ot.v, ot.v, po_d[c].v, ALU.add)
                    k.tt("dve", osave[m][:, c, :], ot.v, rec.v, ALU.mult)
                else:
                    k.tt("dve", osave[m][:, c, :], po_d[c].v, rec.v, ALU.mult)
        for c in range(2):
            k.stt("dve", o1[:, c, :], o2[:, c, :], neglam[:, 0:1], o1[:, c, :], ALU.mult, ALU.add)
            sq = sqt.next()
            k.tt("pool", sq.v, o1[:, c, :], o1[:, c, :], ALU.mult)
            k.mm(pmisc.v, ones_f.v, sq.v, start=(c == 0), stop=(c == 1))
        k.act(rstd.v, pmisc.v, AF.Sqrt, scale=1.0 / 256, bias=epst[:, 0:1])
        k.recip(rstd.v, rstd.v)
        for c in range(2):
            o = ostg.next()
            k.stt("dve", o.v, o1[:, c, :], subs[:, c:c + 1], rstd.v, ALU.mult, ALU.mult)
            k.dma("sp", ob[c, :, q0:q0 + 512], o.v)
    nc = k.finish()
    return nc, k


def build_t2(TPC, CAP):
    TT = 512
    NT = TPC // TT
    NSLOT = NG * CAP
    k = KB()
    oa = k.dram("oa", [16, 128, TPC], BF16, "ExternalInput")
    ob = k.dram("ob", [16, 128, TPC], BF16, "ExternalInput")
    ga = k.dram("ga", [32, 128, TPC], BF16, "ExternalInput")
    gb = k.dram("gb", [32, 128, TPC], BF16, "ExternalInput")
    x = k.dram("x", [TPC, D], F32, "ExternalInput")
    w_oa = k.dram("w_oa", [2048, D], F32, "ExternalInput")
    w_ob = k.dram("w_ob", [2048, D], F32, "ExternalInput")
    w_out = k.dram("w_out", [D, D], F32, "ExternalInput")
    g_ffn = k.dram("g_ffn", [1, D], F32, "ExternalInput")
    w_r = k.dram("w_r", [D, 72], F32, "ExternalInput")
    b_r = k.dram("b_r", [1, 72], F32, "ExternalInput")
    goff = k.dram("goff", [1, 8], F32, "ExternalInput")
    x1 = k.dram("x1", [TPC, D], F32, "ExternalOutput")
    xg = k.dram("xg", [NSLOT, D], BF16, "ExternalOutput")
    gate = k.dram("gate", [NSLOT, 8], F32, "ExternalOutput")
    slot = k.dram("slot", [TPC, 1], I32, "ExternalOutput")

    ident_f = make_ident(k, F32, "ident")
    ones_f = k.sb([128, 128], F32, "ones_f")
    k.memset("dve", ones_f.v, 1.0)
    ustr = k.sb([128, 128], F32, "ustr")
    k.memset("dve", ustr.v, 1.0)
    k.gen("pool", lambda q: q.affine_select(ustr.base, ustr.base, [[1, 128]], ALU.is_gt, 0.0, base=0,
                                            channel_multiplier=-1), [ustr.v], [ustr.v])
    epst = k.sb([128, 1], F32, "epst")
    k.memset("dve", epst.v, EPS)
    gft = k.sb([128, D], F32, "gft")
    k.dma("sp", gft.v, V(g_ffn.base[0:1, :].partition_broadcast(128), g_ffn))
    brt = k.sb([128, 72], F32, "brt")
    k.dma("sp", brt.v, V(b_r.base[0:1, :].partition_broadcast(128), b_r))
    gofft = k.sb([128, 8], F32, "gofft")
    k.dma("sp", gofft.v, V(goff.base[0:1, :].partition_broadcast(128), goff))
    wr = k.sb([128, KC, 72], F32, "wr")
    k.dma("sp", wr.v, w_r.v.rearrange("(kc p) n -> p kc n", p=128))
    h2b = k.sb([128, D], BF16, "h2b")
    zb = h2b
    k.memset("pool", zb.v, 0.0)
    zg = k.sb([128, 8], F32, "zg")
    k.memset("pool", zg.v, 0.0)
    for r in range(NSLOT // 128):
        k.dma("sp", xg[r * 128:(r + 1) * 128, :], zb.v)
        k.dma("sp", gate[r * 128:(r + 1) * 128, :], zg.v)

    w_oa_v = w_oa.v.rearrange("(kc p) n -> p kc n", p=128)
    w_ob_v = w_ob.v.rearrange("(kc p) n -> p kc n", p=128)
    w_out_v = w_out.v.rearrange("(kc p) n -> p kc n", p=128)

    oaT = k.sb([128, 16, TT], BF16, "oaT")
    obT = k.sb([128, 16, TT], BF16, "obT")
    mT = k.sb([128, KC, TT], BF16, "mT")
    wab = Rot([k.sb([128, 16, 256], BF16, f"wab{i}") for i in range(2)])
    wo = Rot([k.sb([128, KC, 256], BF16, f"wo{i}") for i in range(2)])
    gt = Rot([k.sb([128, TT], BF16, f"gt{i}") for i in range(4)])
    tmp1 = Rot([k.sb([128, TT], F32, f"tmp1_{i}") for i in range(2)])
    tmp2 = Rot([k.sb([128, TT], F32, f"tmp2_{i}") for i in range(2)])
    xs = Rot([k.sb([128, 256], F32, f"xs{i}") for i in range(3)])
    xo = Rot([k.sb([128, 256], F32, f"xo{i}") for i in range(3)])
    x1t = k.sb([128, D], F32, "x1t")
    h2f = x1t
    h2T = k.sb([128, KC, 128], F32, "h2T")
    gcar = k.sb([128, 8], F32, "gcar")
    k.memset("dve", gcar.v, 0.0)
    sm = Rot([k.sb([128, 256], F32, f"sm{i}") for i in range(2)])
    sloti = Rot([k.sb([128, 1], I32, f"sloti{i}") for i in range(2)])
    g8 = Rot([k.sb([128, 8], F32, f"g8_{i}") for i in range(2)])

    pacc = Rot([k.ps([128, 512], F32, f"pacc{i}") for i in range(4)])
    ptr = Rot([k.ps([128, 4, 128], F32, f"ptr{i}") for i in range(2)])
    plg = k.ps([128, 512], F32, "plg")
    prk = k.ps([128, 512], F32, "prk")

    for t in range(NT):
        t0 = t * TT
        k.dma("sp", oaT.v, oa.v[:, :, t0:t0 + TT].rearrange("c p t -> p c t"))
        k.dma("sp", obT.v, ob.v[:, :, t0:t0 + TT].rearrange("c p t -> p c t"))
        for c in range(D // 256):
            wa = wab.next()
            wb = wab.next()
            k.dma("pool", wa.v, w_oa_v[:, :, c * 256:(c + 1) * 256])
            k.dma("pool", wb.v, w_ob_v[:, :, c * 256:(c + 1) * 256])
            for j in range(2):
                idx = c * 2 + j
                pa = pacc.next()
                pb = pacc.next()
                for kc in range(16):
                    k.mm(pa.v, wa[:, kc, j * 128:(j + 1) * 128], oaT[:, kc, :], start=(kc == 0), stop=(kc == 15))
                for kc in range(16):
                    k.mm(pb.v, wb[:, kc, j * 128:(j + 1) * 128], obT[:, kc, :], start=(kc == 0), stop=(kc == 15))
                gat = gt.next()
                gbt = gt.next()
                k.dma("sp", gat.v, ga[idx, :, t0:t0 + TT])
                k.dma("sp", gbt.v, gb[idx, :, t0:t0 + TT])
                a1 = tmp1.next()
                a2 = tmp2.next()
                k.tt("dve", a1.v, pa.v, gat.v, ALU.mult)
                k.tt("dve", a2.v, pb.v, gbt.v, ALU.mult)
                k.tt("pool", mT[:, idx, :], a1.v, a2.v, ALU.add)
        for c in range(D // 256):
            wt = wo.next()
            k.dma("pool", wt.v, w_out_v[:, :, c * 256:(c + 1) * 256])
            for s in range(TT // 128):
                r0 = t0 + s * 128
                pa = pacc.next()
                for kc in range(KC):
                    k.mm(pa[:, 0:256], mT[:, kc, s * 128:(s + 1) * 128], wt[:, kc, :], start=(kc == 0), stop=(kc == KC - 1))
                xi = xs.next()
                k.dma("sp", xi.v, x[r0:r0 + 128, c * 256:(c + 1) * 256])
                xq = xo.next()
                k.tt("dve", xq.v, pa[:, 0:256], xi.v, ALU.add)
                k.dma("sp", x1[r0:r0 + 128, c * 256:(c + 1) * 256], xq.v)
        for s in range(TT // 128):
            r0 = t0 + s * 128
            k.dma("sp", x1t.v, x1[r0:r0 + 128, :])
            w = sm.next()
            k.act(h2b.v, x1t.v, AF.Square, accum_out=w[:, 0:1])
            k.act(w[:, 1:2], w[:, 0:1], AF.Sqrt, scale=1.0 / D, bias=epst[:, 0:1])
            k.recip(w[:, 2:3], w[:, 1:2])
            k.stt("dve", h2f.v, x1t.v, w[:, 2:3], gft.v, ALU.mult, ALU.mult)
            k.copy("pool", h2b.v, h2f.v)
            for g4 in range(KC // 4):
                pt = ptr.next()
                for j in range(4):
                    kc = g4 * 4 + j
                    k.tr(pt[:, j, :], h2f[:, kc * 128:(kc + 1) * 128], ident_f.v)
                k.copy("act" if g4 % 2 else "dve", h2T[:, g4 * 4:(g4 + 1) * 4, :], pt.v)
            for kc in range(KC):
                k.mm(plg[:, 0:72], h2T[:, kc, :], wr[:, kc, :], start=(kc == 0), stop=(kc == KC - 1))
            lgb = w[:, 96:160]
            k.tt("dve", w[:, 32:40], plg[:, 0:8], brt[:, 0:8], ALU.add)
            k.tt("dve", lgb, plg[:, 8:72], brt[:, 8:72], ALU.add)
            k.reduce("dve", w[:, 3:4], w[:, 32:40], ALU.max)
            k.ts("dve", w[:, 40:48], w[:, 32:40], w[:, 3:4], None, ALU.is_equal)
            k.ts("dve", w[:, 4:5], w[:, 3:4], -1.0, None, ALU.mult)
            k.act(w[:, 80:88], w[:, 32:40], AF.Exp, bias=w[:, 4:5], accum_out=w[:, 5:6])
            k.recip(w[:, 6:7], w[:, 5:6])
            el3 = V(w.base[:, 96:160].rearrange("p (g e) -> p g e", e=8), w)
            pr3 = V(w.base[:, 160:224].rearrange("p (g e) -> p g e", e=8), w)
            Gb = V(w.base[:, 40:48].unsqueeze(2).to_broadcast([128, 8, 8]), w)
            k.tt("dve", pr3, el3, Gb, ALU.mult)
            k.reduce("dve", w[:, 48:56], V(w.base[:, 160:224].rearrange("p (g e) -> p e g", e=8), w), ALU.add)
            k.reduce("dve", w[:, 7:8], w[:, 48:56], ALU.max)
            k.ts("dve", w[:, 56:64], w[:, 48:56], w[:, 7:8], None, ALU.is_equal)
            k.stt("dve", w[:, 64:72], w[:, 56:64], -1e30, w[:, 48:56], ALU.mult, ALU.add)
            k.reduce("dve", w[:, 8:9], w[:, 64:72], ALU.max)
            k.ts("dve", w[:, 72:80], w[:, 64:72], w[:, 8:9], None, ALU.is_equal)
            k.tt("dve", w[:, 9:10], w[:, 8:9], w[:, 7:8], ALU.subtract)
            k.act(w[:, 10:11], w[:, 9:10], AF.Exp)
            k.ts("dve", w[:, 11:12], w[:, 10:11], 1.0, None, ALU.add)
            k.recip(w[:, 12:13], w[:, 11:12])
            k.tt("dve", w[:, 13:14], w[:, 10:11], w[:, 12:13], ALU.mult)
            k.tt("dve", w[:, 14:15], w[:, 12:13], w[:, 6:7], ALU.mult)
            k.tt("dve", w[:, 15:16], w[:, 13:14], w[:, 6:7], ALU.mult)
            gg = g8.next()
            k.ts("dve", gg.v, w[:, 56:64], w[:, 14:15], None, ALU.mult)
            k.stt("dve", gg.v, w[:, 72:80], w[:, 15:16], gg.v, ALU.mult, ALU.add)
            k.mm(prk[:, 0:8], ustr.v, w[:, 40:48], start=True, stop=False)
            k.mm(prk[:, 0:8], ones_f.v, gcar.v, start=False, stop=True)
            k.tt("dve", w[:, 88:96], prk[:, 0:8], gofft.v, ALU.add)
            k.tt("dve", w[:, 88:96], w[:, 88:96], w[:, 40:48], ALU.mult)
            k.reduce("dve", w[:, 16:17], w[:, 88:96], ALU.add)
            k.tt("dve", gcar.v, gcar.v, w[:, 40:48], ALU.add)
            si = sloti.next()
            k.copy("dve", si.v, w[:, 16:17])
            k.dma("sp", slot[r0:r0 + 128, :], si.v)
            k.dma_gen("pool", lambda q, si=si: q.indirect_dma_start(
                out=xg.base[:, :], out_offset=bass.IndirectOffsetOnAxis(ap=si.base[:, :], axis=0),
                in_=h2b.base[:, :], in_offset=None, bounds_check=NSLOT - 1, oob_is_err=False),
                [si.v, h2b.v], [xg.v])
            k.dma_gen("pool", lambda q, si=si, gg=gg: q.indirect_dma_start(
                out=gate.base[:, :], out_offset=bass.IndirectOffsetOnAxis(ap=si.base[:, :], axis=0),
                in_=gg.base[:, :], in_offset=None, bounds_check=NSLOT - 1, oob_is_err=False),
                [si.v, gg.v], [gate.v])
    nc = k.finish()
    return nc, k


def build_e(NSL, SLT):
    NTL = NSL // SLT
    NSUB = SLT // 128
    k = KB()
    xg = k.dram("xg", [NSL, D], BF16, "ExternalInput")
    gate = k.dram("gate", [NSL, 8], F32, "ExternalInput")
    w1 = k.dram("w1", [EPG, D, DE], F32, "ExternalInput")
    w3 = k.dram("w3", [EPG, D, DE], F32, "ExternalInput")
    w2 = k.dram("w2", [EPG, DE, D], F32, "ExternalInput")
    yg = k.dram("yg", [NSL, D], F32, "ExternalOutput")

    ident = make_ident(k, BF16, "ident")
    xrow = Rot([k.sb([128, D], BF16, f"xrow{i}") for i in range(2)])
    XgT = k.sb([128, KC, SLT], BF16, "XgT")
    gt = k.sb([128, NSUB, 8], F32, "gt")
    acc = k.sb([128, NSUB, D], F32, "acc")
    w13 = Rot([k.sb([128, KC, 128], BF16, f"w13_{i}") for i in range(4)])
    w2b = Rot([k.sb([128, 3, D], BF16, f"w2b{i}") for i in range(2)])
    GT = Rot([k.sb([128, 3, SLT], BF16, f"GT{i}") for i in range(2)])
    sl = Rot([k.sb([128, SLT], F32, f"sl{i}") for i in range(2)])
    ptr = Rot([k.ps([128, 8, 128], BF16, f"ptr{i}") for i in range(2)])
    ph = Rot([k.ps([128, 512], F32, f"ph{i}") for i in range(4)])
    py = Rot([k.ps([128, 512], F32, f"py{i}") for i in range(2)])

    for t in range(NTL):
        s0 = t * SLT
        for s in range(NSUB):
            xr = xrow.next()
            k.dma("sp", xr.v, xg[s0 + s * 128:s0 + (s + 1) * 128, :])
            k.dma("sp", gt[:, s, :], gate[s0 + s * 128:s0 + (s + 1) * 128, :])
            for g8 in range(KC // 8):
                pt = ptr.next()
                for j in range(8):
                    kc = g8 * 8 + j
                    k.tr(pt[:, j, :], xr[:, kc * 128:(kc + 1) * 128], ident.v)
                k.copy("dve" if g8 % 2 else "act", XgT[:, g8 * 8:(g8 + 1) * 8, s * 128:(s + 1) * 128], pt.v)
        for e in range(EPG):
            w1v = w1.v[e].rearrange("(kc p) f -> p kc f", p=128)
            w3v = w3.v[e].rearrange("(kc p) f -> p kc f", p=128)
            G = GT.next()
            for fc in range(3):
                wa = w13.next()
                wb = w13.next()
                k.dma("pool", wa.v, w1v[:, :, fc * 128:(fc + 1) * 128])
                k.dma("pool", wb.v, w3v[:, :, fc * 128:(fc + 1) * 128])
                p1 = ph.next()
                p3 = ph.next()
                for kc in range(KC):
                    k.mm(p1[:, 0:SLT], wa[:, kc, :], XgT[:, kc, :], start=(kc == 0), stop=(kc == KC - 1))
                for kc in range(KC):
                    k.mm(p3[:, 0:SLT], wb[:, kc, :], XgT[:, kc, :], start=(kc == 0), stop=(kc == KC - 1))
                sv = sl.next()
                k.act(sv.v, p1[:, 0:SLT], AF.Silu)
                k.tt("dve", G[:, fc, :], sv.v, p3[:, 0:SLT], ALU.mult)
            w2t = w2b.next()
            k.dma("pool", w2t.v, w2.v[e].rearrange("(fc p) d -> p fc d", p=128))
            for s in range(NSUB):
                for dc in range(D // 512):
                    pp = py.next()
                    for fc in range(3):
                        k.mm(pp.v, G[:, fc, s * 128:(s + 1) * 128], w2t[:, fc, dc * 512:(dc + 1) * 512],
                             start=(fc == 0), stop=(fc == 2))
                    dst = acc[:, s, dc * 512:(dc + 1) * 512]
                    if e == 0:
                        k.ts("dve", dst, pp.v, gt[:, s, e:e + 1], None, ALU.mult)
                    else:
                        k.stt("dve", dst, pp.v, gt[:, s, e:e + 1], dst, ALU.mult, ALU.add)
        for s in range(NSUB):
            k.dma("sp", yg[s0 + s * 128:s0 + (s + 1) * 128, :], acc[:, s, :])
    nc = k.finish()
    return nc, k


def build_c(TPC, NSLOT, final):
    k = KB()
    x1 = k.dram("x1", [TPC, D], F32, "ExternalInput")
    slot = k.dram("slot", [TPC, 1], I32, "ExternalInput")
    yg = k.dram("yg", [NSLOT, D], F32, "ExternalInput")
    g_fin = k.dram("g_fin", [1, D], F32, "ExternalInput")
    out = k.dram("out", [TPC, D], F32, "ExternalOutput")
    epst = k.sb([128, 1], F32, "epst")
    k.memset("dve", epst.v, EPS)
    gft = k.sb([128, D], F32, "gft")
    k.dma("sp", gft.v, V(g_fin.base[0:1, :].partition_broadcast(128), g_fin))
    xt = Rot([k.sb([128, D], F32, f"xt{i}") for i in range(2)])
    yt = Rot([k.sb([128, D], F32, f"yt{i}") for i in range(2)])
    si = Rot([k.sb([128, 1], I32, f"si{i}") for i in range(2)])
    st = Rot([k.sb([128, 4], F32, f"st{i}") for i in range(2)])
    for s in range(TPC // 128):
        r0 = s * 128
        x_ = xt.next()
        y_ = yt.next()
        i_ = si.next()
        k.dma("sp", x_.v, x1[r0:r0 + 128, :])
        k.dma("sp", i_.v, slot[r0:r0 + 128, :])
        k.dma_gen("pool", lambda q, i_=i_, y_=y_: q.indirect_dma_start(
            out=y_.base[:, :], out_offset=None, in_=yg.base[:, :],
            in_offset=bass.IndirectOffsetOnAxis(ap=i_.base[:, :], axis=0),
            bounds_check=NSLOT - 1, oob_is_err=False), [i_.v, yg.v], [y_.v])
        k.tt("dve", x_.v, x_.v, y_.v, ALU.add)
        if final:
            w = st.next()
            k.act(y_.v, x_.v, AF.Square, accum_out=w[:, 0:1])
            k.act(w[:, 1:2], w[:, 0:1], AF.Sqrt, scale=1.0 / D, bias=epst[:, 0:1])
            k.recip(w[:, 2:3], w[:, 1:2])
            k.stt("dve", x_.v, x_.v, w[:, 2:3], gft.v, ALU.mult, ALU.mult)
        k.dma("sp", out[r0:r0 + 128, :], x_.v)
    nc = k.finish()
    return nc, k


_PROG = {}


def _prog(key, fn):
    if key not in _PROG:
        _PROG[key] = fn()[0]
    return _PROG[key]


def _run(nc, in_maps):
    res = run_bass_kernel_spmd(nc, in_maps, core_ids=list(range(len(in_maps))))
    return res.results


def _c(a):
    return np.ascontiguousarray(a)


def kernel(x, positions, norm_attn, w_in, q_norm, w_uq, kv_norm, w_ukv, lam_q1, lam_k1, lam_q2, lam_k2,
           subln, w_oa, w_ob, w_out, norm_ffn, w_router_g, b_router_g, w_router_e, b_router_e,
           w1, w3, w2, norm_final):
    import math
    x = np.asarray(x)
    S = x.shape[1]
    depth = np.asarray(w_in).shape[0]
    TPC = S // NCORE
    NB = S // 128
    CAP = max(128, ((TPC // NG) * 3 // 2 + 127) // 128 * 128)
    NSL = NCORE * CAP
    SLT = CAP if CAP <= 512 else 384
    pos = np.asarray(positions).astype(np.int32).reshape(1, S)
    posk = _c(pos[0].reshape(NB, 128).T)
    half = QKR // 2
    inv = (np.float32(10000.0) ** (-np.arange(half, dtype=np.float32) / np.float32(half))).astype(np.float32)
    invf = np.concatenate([inv, inv]).reshape(64, 1).astype(np.float32)
    goff = (np.arange(NG, dtype=np.float32) * CAP).reshape(1, NG)
    X = x.reshape(S, D).astype(np.float32)
    cs = [slice(c * TPC, (c + 1) * TPC) for c in range(NCORE)]

    p_t1 = _prog(("t1", TPC), lambda: build_t1(TPC))
    p_a = _prog(("a2", S), lambda: build_attn2(S))
    p_t2 = _prog(("t2", TPC, CAP), lambda: build_t2(TPC, CAP))
    p_e = _prog(("e", NSL, SLT), lambda: build_e(NSL, SLT))

    for l in range(depth):
        w_in_l = _c(np.asarray(w_in[l], dtype=np.float32))
        w_uq_l = _c(np.asarray(w_uq[l], dtype=np.float32))
        w_ukv_l = _c(np.asarray(w_ukv[l], dtype=np.float32))
        ins = [dict(x=_c(X[cs[c]]), pos=_c(pos[:, cs[c]]), invf=invf, g_attn=_c(np.asarray(norm_attn[l], np.float32)),
                    w_in=w_in_l, g_q=_c(np.asarray(q_norm[l], np.float32)), w_uq=w_uq_l,
                    g_kv=_c(np.asarray(kv_norm[l], np.float32)), w_ukv=w_ukv_l) for c in range(NCORE)]
        r1 = _run(p_t1, ins)
        del ins

        def cat(name, axis):
            return np.concatenate([np.asarray(r1[c][name]) for c in range(NCORE)], axis=axis)
        QN, QR, KN, KR = cat("o_qn", 2), cat("o_qr", 2), cat("o_kn", 2), cat("o_kr", 1)
        VA, QB, KB_, VB = cat("o_va", 0), cat("o_qb", 2), cat("o_kb", 2), cat("o_vb", 0)
        GA = [np.asarray(r1[c]["o_ga"]) for c in range(NCORE)]
        GB = [np.asarray(r1[c]["o_gb"]) for c in range(NCORE)]
        del r1
        lam_init = 0.8 - 0.6 * math.exp(-0.3 * l)
        lam = np.stack([np.asarray(lam_q1[l], np.float32), np.asarray(lam_k1[l], np.float32),
                        np.asarray(lam_q2[l], np.float32), np.asarray(lam_k2[l], np.float32)]).reshape(1, 4, 128)
        ins = []
        for c in range(NCORE):
            consts = np.zeros((128, 4), np.float32)
            consts[:, 0] = 2.0 ** (-8.0 * (c + 1) / HB)
            consts[:, 1] = lam_init
            consts[:, 2] = 1.0 - lam_init
            va_c = np.stack([_c(VA[:, (2 * c + h) * 128:(2 * c + h + 1) * 128].reshape(NB, 128, 128).transpose(1, 0, 2))
                             for h in range(2)])
            vb_c = _c(VB[:, c * 256:(c + 1) * 256].reshape(NB, 128, 256).transpose(1, 0, 2))
            ins.append(dict(qn=_c(QN[2 * c:2 * c + 2]), qr=_c(QR[2 * c:2 * c + 2]), kn=_c(KN[2 * c:2 * c + 2]), kr=_c(KR),
                            va=_c(va_c), qb=_c(QB[2 * c:2 * c + 2]), kb=_c(KB_[2 * c:2 * c + 2]), vb=vb_c,
                            pos=pos, posk=posk, lam=lam, subln=_c(np.asarray(subln[l], np.float32)), consts=consts))
        del QN, QR, KN, KR, VA, QB, KB_, VB
        ra = _run(p_a, ins)
        del ins
        OA = np.concatenate([np.asarray(ra[c]["oa"]) for c in range(NCORE)], axis=0)
        OB = np.concatenate([np.asarray(ra[c]["ob"]) for c in range(NCORE)], axis=0)
        del ra
        w_oa_l = _c(np.asarray(w_oa[l], np.float32))
        w_ob_l = _c(np.asarray(w_ob[l], np.float32))
        w_out_l = _c(np.asarray(w_out[l], np.float32))
        w_r = _c(np.concatenate([np.asarray(w_router_g[l], np.float32), np.asarray(w_router_e[l], np.float32)], axis=1))
        b_r = _c(np.concatenate([np.asarray(b_router_g[l], np.float32), np.asarray(b_router_e[l], np.float32)]).reshape(1, 72))
        ins = [dict(oa=_c(OA[:, :, cs[c]]), ob=_c(OB[:, :, cs[c]]), ga=GA[c], gb=GB[c], x=_c(X[cs[c]]),
                    w_oa=w_oa_l, w_ob=w_ob_l, w_out=w_out_l, g_ffn=_c(np.asarray(norm_ffn[l], np.float32).reshape(1, D)),
                    w_r=w_r, b_r=b_r, goff=goff) for c in range(NCORE)]
        del OA, OB, GA, GB
        r2 = _run(p_t2, ins)
        del ins
        ins = []
        for g in range(NG):
            xg_g = np.concatenate([np.asarray(r2[c]["xg"])[g * CAP:(g + 1) * CAP] for c in range(NCORE)], axis=0)
            gate_g = np.concatenate([np.asarray(r2[c]["gate"])[g * CAP:(g + 1) * CAP] for c in range(NCORE)], axis=0)
            ins.append(dict(xg=_c(xg_g), gate=_c(gate_g),
                            w1=_c(np.asarray(w1[l, g * EPG:(g + 1) * EPG], np.float32)),
                            w3=_c(np.asarray(w3[l, g * EPG:(g + 1) * EPG], np.float32)),
                            w2=_c(np.asarray(w2[l, g * EPG:(g + 1) * EPG], np.float32))))
        re_ = _run(p_e, ins)
        del ins
        final = (l == depth - 1)
        p_c = _prog(("c", TPC, NG * CAP, final), lambda: build_c(TPC, NG * CAP, final))
        ins = []
        for c in range(NCORE):
            yg_c = np.concatenate([np.asarray(re_[g]["yg"])[c * CAP:(c + 1) * CAP] for g in range(NG)], axis=0)
            ins.append(dict(x1=np.asarray(r2[c]["x1"]), slot=np.asarray(r2[c]["slot"]), yg=_c(yg_c),
                            g_fin=_c(np.asarray(norm_final, np.float32).reshape(1, D))))
        del re_, r2
        rc = _run(p_c, ins)
        del ins
        X = np.concatenate([np.asarray(rc[c]["out"]) for c in range(NCORE)], axis=0)
        del rc
    return X.reshape(1, S, D).astype(np.float32)
```

```python
import numpy as np
import concourse.bass as bass
import concourse.mybir as mybir
from concourse.bass_utils import run_bass_kernel_spmd

F32 = mybir.dt.float32
BF16 = mybir.dt.bfloat16
I32 = mybir.dt.int32
AF = mybir.ActivationFunctionType
ALU = mybir.AluOpType
AX = mybir.AxisListType


class V:
    def __init__(self, ap, buf):
        self.ap = ap
        self.buf = buf

    def __getitem__(self, k):
        return V(self.ap[k], self.buf)

    def rearrange(self, *a, **kw):
        return V(self.ap.rearrange(*a, **kw), self.buf)


class Buf:
    def __init__(self, ap, name):
        self.base = ap
        self.name = name
        self.lw = None
        self.rd = {}

    def __getitem__(self, k):
        return V(self.base[k], self)

    @property
    def v(self):
        return V(self.base, self)


NDMA = 4


class KB:
    def __init__(self, name="k"):
        self.nc = bass.Bass("TRN2", target_bir_lowering=False)
        self.ops = []
        self.nbuf = 0

    def sb(self, shape, dtype, name=None):
        self.nbuf += 1
        name = name or f"sb{self.nbuf}"
        t = self.nc.alloc_sbuf_tensor(name, list(shape), dtype)
        return Buf(t[:], name)

    def ps(self, shape, dtype=F32, name=None):
        self.nbuf += 1
        name = name or f"ps{self.nbuf}"
        t = self.nc.alloc_psum_tensor(name, list(shape), dtype)
        return Buf(t[:], name)

    def dram(self, name, shape, dtype, kind="Internal"):
        t = self.nc.dram_tensor(name, list(shape), dtype, kind=kind)
        return Buf(t.ap(), name)

    def op(self, eng, fn, reads, writes, dma=False):
        rb = [x.buf for x in reads if isinstance(x, V)]
        wb = [x.buf for x in writes if isinstance(x, V)]
        self.ops.append((eng, fn, rb, wb, dma))

    @staticmethod
    def _a(x):
        return x.ap if isinstance(x, V) else x

    def mm(self, out, lhsT, rhs, start=True, stop=True):
        a = self._a
        self.op("pe", lambda q: q.matmul(a(out), a(lhsT), a(rhs), start=start, stop=stop),
                [lhsT, rhs] + ([] if start else [out]), [out])

    def tr(self, out, in_, ident):
        a = self._a
        self.op("pe", lambda q: q.transpose(a(out), a(in_), a(ident)), [in_, ident], [out])

    def act(self, out, in_, func, bias=None, scale=1.0, accum_out=None, eng="act"):
        a = self._a
        kw = {}
        if bias is not None:
            kw["bias"] = a(bias)
        if accum_out is not None:
            kw["accum_out"] = a(accum_out)
        self.op(eng, lambda q: q.activation(a(out), a(in_), func, scale=a(scale), **kw),
                [in_, bias, scale], [out, accum_out])

    def tt(self, eng, out, in0, in1, op):
        a = self._a
        self.op(eng, lambda q: q.tensor_tensor(a(out), a(in0), a(in1), op), [in0, in1], [out])

    def ts(self, eng, out, in0, s1, s2, op0, op1=None, accum_out=None):
        a = self._a
        kw = {}
        if op1 is not None:
            kw["op1"] = op1
        if accum_out is not None:
            kw["accum_out"] = a(accum_out)
        self.op(eng, lambda q: q.tensor_scalar(a(out), a(in0), a(s1), a(s2) if s2 is not None else None, op0, **kw),
                [in0, s1, s2], [out, accum_out])

    def stt(self, eng, out, in0, scalar, in1, op0, op1, accum_out=None):
        a = self._a
        kw = {}
        if accum_out is not None:
            kw["accum_out"] = a(accum_out)
        self.op(eng, lambda q: q.scalar_tensor_tensor(a(out), a(in0), a(scalar), a(in1), op0, op1, **kw),
                [in0, scalar, in1], [out, accum_out])

    def copy(self, eng, out, in_):
        a = self._a
        if eng == "act":
            self.op(eng, lambda q: q.copy(a(out), a(in_)), [in_], [out])
        else:
            self.op(eng, lambda q: q.tensor_copy(a(out), a(in_)), [in_], [out])

    def memset(self, eng, out, val):
        a = self._a
        self.op(eng, lambda q: q.memset(a(out), val), [], [out])

    def reduce(self, eng, out, in_, op, axis=AX.X):
        a = self._a
        self.op(eng, lambda q: q.tensor_reduce(a(out), a(in_), axis, op), [in_], [out])

    def recip(self, out, in_):
        a = self._a
        self.op("dve", lambda q: q.reciprocal(a(out), a(in_)), [in_], [out])

    def gen(self, eng, fn, reads, writes):
        self.op(eng, fn, reads, writes)

    def dma(self, eng, out, in_, **kw):
        a = self._a
        self.op(eng, lambda q: q.dma_start(out=a(out), in_=a(in_), **kw), [in_], [out], dma=True)

    def dma_gen(self, eng, fn, reads, writes):
        self.op(eng, fn, reads, writes, dma=True)

    def finish(self):
        nc = self.nc
        engs = ["pe", "act", "dve", "pool", "sp"]
        qs = {"pe": nc.tensor, "act": nc.scalar, "dve": nc.vector, "pool": nc.gpsimd, "sp": nc.sync}
        sems = {e: nc.alloc_semaphore(f"s_{e}") for e in engs}
        dsems = {e: [nc.alloc_semaphore(f"d_{e}{i}") for i in range(NDMA)] for e in ("act", "pool", "sp")}
        semobj = {}
        for e in engs:
            semobj[("c", e)] = sems[e]
        for e in dsems:
            for i in range(NDMA):
                semobj[("d", e, i)] = dsems[e][i]
        cnt = {e: 0 for e in engs}
        dcnt = {e: 0 for e in dsems}
        dval = {k: 0 for k in semobj}
        waited = {e: {} for e in engs}
        per_eng = {e: [] for e in engs}
        for (eng, fn, rb, wb, dma) in self.ops:
            deps = {}

            def add(tok):
                if tok is None:
                    return
                k, v = tok
                if deps.get(k, 0) < v:
                    deps[k] = v
            for b in rb:
                add(b.lw)
            for b in wb:
                add(b.lw)
                for k, v in b.rd.items():
                    add((k, v))
            waits = []
            for k, v in deps.items():
                if k == ("c", eng) and eng == "pe":
                    continue
                if waited[eng].get(k, 0) >= v:
                    continue
                waited[eng][k] = v
                waits.append((k, v))
            if dma:
                i = dcnt[eng] % NDMA
                dcnt[eng] += 1
                key = ("d", eng, i)
                dval[key] += 16
                tok = (key, dval[key])
                inc = 16
            else:
                cnt[eng] += 1
                key = ("c", eng)
                tok = (key, cnt[eng])
                inc = 1
            per_eng[eng].append((fn, waits, key, inc))
            for b in rb:
                if b.rd.get(tok[0], 0) < tok[1]:
                    b.rd[tok[0]] = tok[1]
            for b in wb:
                b.lw = tok
                b.rd = {}
        fin = []
        for e in engs:
            if cnt[e]:
                fin.append((("c", e), cnt[e]))
        for k, v in dval.items():
            if v:
                fin.append((k, v))
        self.stats = dict(cnt=dict(cnt), dcnt=dict(dcnt))

        with nc.Block() as block:
            def body(eng):
                def f(q):
                    for (fn, waits, key, inc) in per_eng[eng]:
                        for (k, v) in waits:
                            q.wait_ge(semobj[k], v)
                        fn(q).then_inc(semobj[key], inc)
                    if eng == "sp":
                        for (k, v) in fin:
                            q.wait_ge(semobj[k], v)
                return f
            block.tensor(body("pe"))
            block.scalar(body("act"))
            block.vector(body("dve"))
            block.gpsimd(body("pool"))
            block.sync(body("sp"))
        return nc


D = 4096
S_FULL = 16384
NCORE = 8
HA, QKN, QKR, DVA = 16, 128, 64, 128
QL, KVL = 1024, 512
HB, DHB = 8, 128
NG, EPG, DE = 8, 8, 384
EPS = 1e-6
D_IN = 15936
OFF_CQ, OFF_CKV, OFF_KR, OFF_QB, OFF_KB, OFF_VB, OFF_GA, OFF_GB = 0, 1024, 1536, 1600, 3648, 5696, 7744, 11840
KC = D // 128
TWO_PI = 6.283185307179586
PI = 3.141592653589793
PI_LO = 3.1415925
MAGIC = 12582912.0
CW1 = 6.28125
CW2 = TWO_PI - 6.28125


def make_ident(k, dtype, name):
    idf = k.sb([128, 128], F32, name + "_f")
    k.memset("dve", idf.v, 0.0)
    k.gen("pool", lambda q: q.affine_select(idf.base, idf.base, [[-1, 128]], ALU.not_equal, 1.0, base=0,
                                            channel_multiplier=1), [idf.v], [idf.v])
    if dtype == F32:
        return idf
    idb = k.sb([128, 128], dtype, name)
    k.copy("dve", idb.v, idf.v)
    return idb


class Rot:
    def __init__(self, bufs):
        self.bufs = bufs
        self.i = 0

    def next(self):
        b = self.bufs[self.i % len(self.bufs)]
        self.i += 1
        return b


def build_t1(TPC):
    TT = 512
    NT = TPC // TT
    k = KB()
    x = k.dram("x", [TPC, D], F32, "ExternalInput")
    pos = k.dram("pos", [1, TPC], I32, "ExternalInput")
    invf = k.dram("invf", [64, 1], F32, "ExternalInput")
    g_attn = k.dram("g_attn", [D], F32, "ExternalInput")
    w_in = k.dram("w_in", [D, D_IN], F32, "ExternalInput")
    g_q = k.dram("g_q", [QL], F32, "ExternalInput")
    w_uq = k.dram("w_uq", [QL, HA * 192], F32, "ExternalInput")
    g_kv = k.dram("g_kv", [KVL], F32, "ExternalInput")
    w_ukv = k.dram("w_ukv", [KVL, HA * 256], F32, "ExternalInput")
    o_qn = k.dram("o_qn", [HA, 128, TPC], BF16, "ExternalOutput")
    o_qr = k.dram("o_qr", [HA, 64, TPC], BF16, "ExternalOutput")
    o_kn = k.dram("o_kn", [HA, 128, TPC], BF16, "ExternalOutput")
    o_kr = k.dram("o_kr", [64, TPC], BF16, "ExternalOutput")
    o_va = k.dram("o_va", [TPC, HA * 128], BF16, "ExternalOutput")
    o_qb = k.dram("o_qb", [16, 128, TPC], BF16, "ExternalOutput")
    o_kb = k.dram("o_kb", [16, 128, TPC], BF16, "ExternalOutput")
    o_vb = k.dram("o_vb", [TPC, 2048], BF16, "ExternalOutput")
    o_ga = k.dram("o_ga", [32, 128, TPC], BF16, "ExternalOutput")
    o_gb = k.dram("o_gb", [32, 128, TPC], BF16, "ExternalOutput")

    ident = make_ident(k, BF16, "ident")
    ones_f = k.sb([128, 128], F32, "ones_f")
    k.memset("dve", ones_f.v, 1.0)
    gT = k.sb([128, KC], F32, "gT")
    k.dma("sp", gT.v, g_attn.v.rearrange("(kc p) -> p kc", p=128), allow_slow_non_contiguous=True)
    gqT = k.sb([128, 8], F32, "gqT")
    k.dma("sp", gqT.v, g_q.v.rearrange("(kc p) -> p kc", p=128), allow_slow_non_contiguous=True)
    gkvT = k.sb([128, 4], F32, "gkvT")
    k.dma("sp", gkvT.v, g_kv.v.rearrange("(kc p) -> p kc", p=128), allow_slow_non_contiguous=True)
    invt = k.sb([64, 1], F32, "invt")
    k.dma("sp", invt.v, invf.v)

    w_in_v = w_in.v.rearrange("(kc p) n -> p kc n", p=128)
    w_uq_v = w_uq.v.rearrange("(kc p) n -> p kc n", p=128)
    w_ukv_v = w_ukv.v.rearrange("(kc p) n -> p kc n", p=128)

    wkr = k.sb([128, KC, 64], BF16, "wkr")
    wkrr = k.sb([128, KC, 64], BF16, "wkrr")
    k.dma("pool", wkr.v, w_in_v[:, :, OFF_KR:OFF_KR + 64])
    k.dma("pool", wkrr[:, :, 0:32], w_in_v[:, :, OFF_KR + 32:OFF_KR + 64])
    k.dma("pool", wkrr[:, :, 32:64], w_in_v[:, :, OFF_KR:OFF_KR + 32])
    k.gen("act", lambda q: q.mul(wkrr.base[:, :, 0:32], wkrr.base[:, :, 0:32], -1.0), [wkrr.v], [wkrr.v])
    wukv_g = k.sb([128, 4, 1024], BF16, "wukv_g")
    wuq_g = k.sb([128, 8, 768], BF16, "wuq_g")
    wuqr = k.sb([128, 8, HA, 64], BF16, "wuqr")
    wuq_h = w_uq_v.rearrange("p kc (h c) -> p kc h c", c=192)
    for kc in range(8):
        k.dma("pool", wuqr[:, kc, :, 0:32], wuq_h[:, kc, :, 160:192])
        k.dma("pool", wuqr[:, kc, :, 32:64], wuq_h[:, kc, :, 128:160])
    k.gen("act", lambda q: q.mul(wuqr.base[:, :, :, 0:32], wuqr.base[:, :, :, 0:32], -1.0), [wuqr.v], [wuqr.v])

    xin = Rot([k.sb([128, D], F32, f"xin{i}") for i in range(1)])
    hbf = Rot([k.sb([128, D], BF16, f"hbf{i}") for i in range(1)])
    stat = Rot([k.sb([128, 4], F32, f"stat{i}") for i in range(4)])
    hT = k.sb([128, KC, TT], BF16, "hT")
    wbuf = Rot([k.sb([128, KC, 256], BF16, f"wbuf{i}") for i in range(2)])
    cqT = k.sb([128, 8, TT], F32, "cqT")
    ckvT = k.sb([128, 4, TT], F32, "ckvT")
    cqn = k.sb([128, 8, TT], BF16, "cqn")
    ckvn = k.sb([128, 4, TT], BF16, "ckvn")
    sq32 = Rot([k.sb([128, TT], F32, f"sq32_{i}") for i in range(2)])
    rstd_q = k.sb([128, TT], F32, "rstd_q")
    rstd_kv = k.sb([128, TT], F32, "rstd_kv")
    stg = Rot([k.sb([128, TT], BF16, f"stg{i}") for i in range(6)])
    posi = k.sb([64, TT], I32, "posi")
    posf = k.sb([64, TT], F32, "posf")
    ang = k.sb([64, TT], F32, "ang")
    tmpa = k.sb([64, TT], F32, "tmpa")
    cos2 = k.sb([64, TT], F32, "cos2")
    sin2 = k.sb([64, TT], F32, "sin2")
    r1 = Rot([k.sb([64, TT], F32, f"r1_{i}") for i in range(2)])
    r2 = Rot([k.sb([64, TT], F32, f"r2_{i}") for i in range(2)])
    tmpb = k.sb([64, TT], F32, "tmpb")
    epst = k.sb([128, 1], F32, "epst")
    k.memset("dve", epst.v, EPS)

    ptr = Rot([k.ps([128, 8, 128], BF16, f"ptr{i}") for i in range(2)])
    pacc = Rot([k.ps([128, 512], F32, f"pacc{i}") for i in range(5)])
    pss = k.ps([128, 512], F32, "pss")
    evac_i = [0]

    def evac_copy(out, in_):
        evac_i[0] += 1
        k.copy("dve" if evac_i[0] % 2 else "act", out, in_)

    def rope(pa, pb, dst_dram):
        a1 = r1.next()
        a2 = r2.next()
        k.tt("dve", a1.v, pa, cos2.v, ALU.mult)
        k.tt("dve", a2.v, pb, sin2.v, ALU.mult)
        o = stg.next()
        k.tt("dve", o[0:64, :], a1.v, a2.v, ALU.add)
        k.dma("sp", dst_dram, o[0:64, :])

    for t in range(NT):
        t0 = t * TT
        k.dma("sp", posi.v, pos[0:1, t0:t0 + TT].partition_broadcast(64) if False else pos.v[0:1, t0:t0 + TT].ap.partition_broadcast(64) if False else V(pos.base[0:1, t0:t0 + TT].partition_broadcast(64), pos))
        k.copy("dve", posf.v, posi.v)
        k.ts("dve", ang.v, posf.v, invt[:, 0:1], None, ALU.mult)
        def sin_of(dst, src):
            k.ts("dve", tmpa.v, src, 1.0 / TWO_PI, MAGIC, ALU.mult, ALU.add)
            k.ts("dve", tmpa.v, tmpa.v, MAGIC, None, ALU.subtract)
            k.stt("dve", tmpb.v, tmpa.v, -CW1, src, ALU.mult, ALU.add)
            k.stt("dve", tmpb.v, tmpa.v, -CW2, tmpb.v, ALU.mult, ALU.add)
            k.ts("dve", tmpb.v, tmpb.v, PI_LO, -PI_LO, ALU.min, ALU.max)
            k.act(dst, tmpb.v, AF.Sin)
        sin_of(sin2.v, ang.v)
        k.ts("dve", ang.v, ang.v, PI / 2, None, ALU.add)
        sin_of(cos2.v, ang.v)
        for s in range(TT // 128):
            xt = xin.next()
            k.dma("sp", xt.v, x[t0 + s * 128:t0 + (s + 1) * 128, :])
            st = stat.next()
            hb = hbf.next()
            k.act(hb.v, xt.v, AF.Square, accum_out=st[:, 0:1])
            k.act(st[:, 1:2], st[:, 0:1], AF.Sqrt, scale=1.0 / D, bias=epst[:, 0:1])
            k.recip(st[:, 2:3], st[:, 1:2])
            k.ts("dve", hb.v, xt.v, st[:, 2:3], None, ALU.mult)
            for g8 in range(KC // 8):
                pt = ptr.next()
                for j in range(8):
                    kc = g8 * 8 + j
                    k.tr(pt[:, j, :], hb[:, kc * 128:(kc + 1) * 128], ident.v)
                gb = V(gT.base[:, g8 * 8:(g8 + 1) * 8].unsqueeze(2).to_broadcast([128, 8, 128]), gT)
                k.tt("dve", hT[:, g8 * 8:(g8 + 1) * 8, s * 128:(s + 1) * 128], pt.v, gb, ALU.mult)

        def fm_chunk(wb, j, evac):
            pa = pacc.next()
            for kc in range(KC):
                k.mm(pa.v, wb[:, kc, j * 128:(j + 1) * 128], hT[:, kc, :], start=(kc == 0), stop=(kc == KC - 1))
            evac(pa)

        def stream_fm(col0, ncols, evac_fn):
            for c in range(ncols // 256):
                wb = wbuf.next()
                k.dma("pool", wb.v, w_in_v[:, :, col0 + c * 256:col0 + (c + 1) * 256])
                for j in range(2):
                    fm_chunk(wb, j, lambda pa, idx=c * 2 + j: evac_fn(pa, idx))

        stream_fm(OFF_CQ, QL, lambda pa, idx: evac_copy(cqT[:, idx, :], pa.v))
        stream_fm(OFF_CKV, KVL, lambda pa, idx: evac_copy(ckvT[:, idx, :], pa.v))

        pa = pacc.next()
        pb = pacc.next()
        for kc in range(KC):
            k.mm(pa[0:64, :], wkr[:, kc, :], hT[:, kc, :], start=(kc == 0), stop=(kc == KC - 1))
        for kc in range(KC):
            k.mm(pb[0:64, :], wkrr[:, kc, :], hT[:, kc, :], start=(kc == 0), stop=(kc == KC - 1))
        rope(pa[0:64, :], pb[0:64, :], o_kr[:, t0:t0 + TT])

        def subnorm(srcT, nch, width, gvec, rstd, dst):
            for j in range(nch):
                sq = sq32.next()
                k.tt("pool", sq.v, srcT[:, j, :], srcT[:, j, :], ALU.mult)
                k.mm(pss.v, ones_f.v, sq.v, start=(j == 0), stop=(j == nch - 1))
            k.act(rstd.v, pss.v, AF.Sqrt, scale=1.0 / width, bias=epst[:, 0:1])
            k.recip(rstd.v, rstd.v)
            for j in range(nch):
                k.stt("dve", dst[:, j, :], srcT[:, j, :], gvec[:, j:j + 1], rstd.v, ALU.mult, ALU.mult)

        subnorm(cqT, 8, QL, gqT, rstd_q, cqn)
        subnorm(ckvT, 4, KVL, gkvT, rstd_kv, ckvn)

        for h in range(HA):
            if h % 4 == 0:
                k.dma("pool", wuq_g.v, w_uq_v[:, :, h * 192:(h + 4) * 192])
            hl = h % 4
            pa = pacc.next()
            for kc in range(8):
                k.mm(pa.v, wuq_g[:, kc, hl * 192:hl * 192 + 128], cqn[:, kc, :], start=(kc == 0), stop=(kc == 7))
            o = stg.next()
            evac_copy(o.v, pa.v)
            k.dma("sp", o_qn[h, :, t0:t0 + TT], o.v)
            pa = pacc.next()
            pb = pacc.next()
            for kc in range(8):
                k.mm(pa[0:64, :], wuq_g[:, kc, hl * 192 + 128:hl * 192 + 192], cqn[:, kc, :], start=(kc == 0), stop=(kc == 7))
            for kc in range(8):
                k.mm(pb[0:64, :], wuqr[:, kc, h, :], cqn[:, kc, :], start=(kc == 0), stop=(kc == 7))
            rope(pa[0:64, :], pb[0:64, :], o_qr[h, :, t0:t0 + TT])
        wv = V(wukv_g.base.rearrange("p kc (h two c) -> p kc h two c", two=2, c=128), wukv_g)
        for hg in range(4):
            k.dma("pool", wukv_g.v, w_ukv_v[:, :, hg * 1024:(hg + 1) * 1024])
            for hl in range(4):
                h = hg * 4 + hl
                pa = pacc.next()
                for kc in range(4):
                    k.mm(pa.v, wukv_g[:, kc, hl * 256:hl * 256 + 128], ckvn[:, kc, :], start=(kc == 0), stop=(kc == 3))
                o = stg.next()
                evac_copy(o.v, pa.v)
                k.dma("sp", o_kn[h, :, t0:t0 + TT], o.v)
            for s in range(TT // 128):
                pa = pacc.next()
                pav = V(pa.base.rearrange("p (h c) -> p h c", c=128), pa)
                for kc in range(4):
                    k.mm(pav, ckvn[:, kc, s * 128:(s + 1) * 128], wv[:, kc, :, 1, :],
                         start=(kc == 0), stop=(kc == 3))
                o = stg.next()
                evac_copy(o.v, pa.v)
                k.dma("sp", o_va[t0 + s * 128:t0 + (s + 1) * 128, hg * 512:(hg + 1) * 512], o.v)

        def ev_out(dst):
            def f(pa, idx):
                o = stg.next()
                evac_copy(o.v, pa.v)
                k.dma("sp", dst[idx, :, t0:t0 + TT], o.v)
            return f

        def ev_sig(dst):
            def f(pa, idx):
                o = stg.next()
                k.act(o.v, pa.v, AF.Sigmoid)
                k.dma("sp", dst[idx, :, t0:t0 + TT], o.v)
            return f
        stream_fm(OFF_QB, 2048, ev_out(o_qb))
        stream_fm(OFF_KB, 2048, ev_out(o_kb))
        for c in range(2048 // 256):
            wb = wbuf.next()
            k.dma("pool", wb.v, w_in_v[:, :, OFF_VB + c * 256:OFF_VB + (c + 1) * 256])
            for s in range(TT // 128):
                pa = pacc.next()
                for kc in range(KC):
                    k.mm(pa[:, 0:256], hT[:, kc, s * 128:(s + 1) * 128], wb[:, kc, :], start=(kc == 0), stop=(kc == KC - 1))
                o = stg.next()
                evac_copy(o[:, 0:256], pa[:, 0:256])
                k.dma("sp", o_vb[t0 + s * 128:t0 + (s + 1) * 128, c * 256:(c + 1) * 256], o[:, 0:256])
        stream_fm(OFF_GA, D, ev_sig(o_ga))
        stream_fm(OFF_GB, D, ev_sig(o_gb))
    nc = k.finish()
    return nc, k


def build_t1b(TPC):
    TT = 512
    TB = min(1024, TPC)
    NH = TB // TT
    k = KB()
    x = k.dram("x", [TPC, D], F32, "ExternalInput")
    pos = k.dram("pos", [1, TPC], I32, "ExternalInput")
    invf = k.dram("invf", [64, 1], F32, "ExternalInput")
    g_attn = k.dram("g_attn", [D], F32, "ExternalInput")
    w_in = k.dram("w_in", [D, D_IN], F32, "ExternalInput")
    g_q = k.dram("g_q", [QL], F32, "ExternalInput")
    w_uq = k.dram("w_uq", [QL, HA * 192], F32, "ExternalInput")
    g_kv = k.dram("g_kv", [KVL], F32, "ExternalInput")
    w_ukv = k.dram("w_ukv", [KVL, HA * 256], F32, "ExternalInput")
    o_qn = k.dram("o_qn", [HA, 128, TPC], BF16, "ExternalOutput")
    o_qr = k.dram("o_qr", [HA, 64, TPC], BF16, "ExternalOutput")
    o_kn = k.dram("o_kn", [HA, 128, TPC], BF16, "ExternalOutput")
    o_kr = k.dram("o_kr", [64, TPC], BF16, "ExternalOutput")
    o_va = k.dram("o_va", [TPC, HA * 128], BF16, "ExternalOutput")
    o_qb = k.dram("o_qb", [16, 128, TPC], BF16, "ExternalOutput")
    o_kb = k.dram("o_kb", [16, 128, TPC], BF16, "ExternalOutput")
    o_vb = k.dram("o_vb", [TPC, 2048], BF16, "ExternalOutput")
    o_ga = k.dram("o_ga", [32, 128, TPC], BF16, "ExternalOutput")
    o_gb = k.dram("o_gb", [32, 128, TPC], BF16, "ExternalOutput")

    ident = make_ident(k, BF16, "ident")
    ones_f = k.sb([128, 128], F32, "ones_f")
    k.memset("dve", ones_f.v, 1.0)
    gT = k.sb([128, KC], F32, "gT")
    k.dma("sp", gT.v, g_attn.v.rearrange("(kc p) -> p kc", p=128), allow_slow_non_contiguous=True)
    gqT = k.sb([128, 8], F32, "gqT")
    k.dma("sp", gqT.v, g_q.v.rearrange("(kc p) -> p kc", p=128), allow_slow_non_contiguous=True)
    gkvT = k.sb([128, 4], F32, "gkvT")
    k.dma("sp", gkvT.v, g_kv.v.rearrange("(kc p) -> p kc", p=128), allow_slow_non_contiguous=True)
    invt = k.sb([64, 1], F32, "invt")
    k.dma("sp", invt.v, invf.v)

    w_in_v = w_in.v.rearrange("(kc p) n -> p kc n", p=128)
    w_uq_v = w_uq.v.rearrange("(kc p) n -> p kc n", p=128)
    w_ukv_v = w_ukv.v.rearrange("(kc p) n -> p kc n", p=128)

    wkr = k.sb([128, KC, 64], BF16, "wkr")
    wkrr = k.sb([128, KC, 64], BF16, "wkrr")
    k.dma("pool", wkr.v, w_in_v[:, :, OFF_KR:OFF_KR + 64])
    k.dma("pool", wkrr[:, :, 0:32], w_in_v[:, :, OFF_KR + 32:OFF_KR + 64])
    k.dma("pool", wkrr[:, :, 32:64], w_in_v[:, :, OFF_KR:OFF_KR + 32])
    k.gen("act", lambda q: q.mul(wkrr.base[:, :, 0:32], wkrr.base[:, :, 0:32], -1.0), [wkrr.v], [wkrr.v])
    wukv_g = k.sb([128, 4, 1024], BF16, "wukv_g")
    wuq_g = k.sb([128, 8, 384], BF16, "wuq_g")
    wuqr = k.sb([128, 8, 2, 64], BF16, "wuqr")
    wuq_h = w_uq_v.rearrange("p kc (h c) -> p kc h c", c=192)

    def load_wuqr(h0):
        for hh in range(2):
            k.dma("pool", wuqr[:, :, hh, 0:32], wuq_h[:, :, h0 + hh, 160:192])
            k.dma("pool", wuqr[:, :, hh, 32:64], wuq_h[:, :, h0 + hh, 128:160])
        k.gen("act", lambda q: q.mul(wuqr.base[:, :, :, 0:32], wuqr.base[:, :, :, 0:32], -1.0), [wuqr.v], [wuqr.v])
    xin = Rot([k.sb([128, D], F32, f"xin{i}") for i in range(1)])
    hbf = Rot([k.sb([128, D], BF16, f"hbf{i}") for i in range(1)])
    stat = Rot([k.sb([128, 4], F32, f"stat{i}") for i in range(4)])
    hT = k.sb([128, KC, TB], BF16, "hT")
    wbuf = Rot([k.sb([128, KC, 256], BF16, f"wbuf{i}") for i in range(2)])
    cqT = k.sb([128, 8, TT], F32, "cqT")
    ckvT = k.sb([128, 4, TT], F32, "ckvT")
    cqn = k.sb([128, 8, TT], BF16, "cqn")
    ckvn = k.sb([128, 4, TT], BF16, "ckvn")
    sq32 = Rot([k.sb([128, TT], F32, f"sq32_{i}") for i in range(1)])
    rstd_q = k.sb([128, TT], F32, "rstd_q")
    rstd_kv = rstd_q
    stg = Rot([k.sb([128, TT], BF16, f"stg{i}") for i in range(4)])
    posi = k.sb([64, TT], I32, "posi")
    ang = k.sb([64, TT], F32, "ang")
    tmpa = k.sb([64, TT], F32, "tmpa")
    cos2 = k.sb([64, TT], F32, "cos2")
    sin2 = k.sb([64, TT], F32, "sin2")
    r1 = Rot([k.sb([64, TT], F32, f"r1_{i}") for i in range(1)])
    r2 = Rot([k.sb([64, TT], F32, f"r2_{i}") for i in range(1)])
    tmpb = k.sb([64, TT], F32, "tmpb")
    epst = k.sb([128, 1], F32, "epst")
    k.memset("dve", epst.v, EPS)

    ptr = Rot([k.ps([128, 8, 128], BF16, f"ptr{i}") for i in range(2)])
    pacc = Rot([k.ps([128, 512], F32, f"pacc{i}") for i in range(5)])
    pss = k.ps([128, 512], F32, "pss")
    evac_i = [0]

    def evac_copy(out, in_):
        evac_i[0] += 1
        k.copy("dve" if evac_i[0] % 2 else "act", out, in_)

    def rope(pa, pb, dst_dram):
        a1 = r1.next()
        a2 = r2.next()
        k.tt("dve", a1.v, pa, cos2.v, ALU.mult)
        k.tt("dve", a2.v, pb, sin2.v, ALU.mult)
        o = stg.next()
        k.tt("dve", o[0:64, :], a1.v, a2.v, ALU.add)
        k.dma("sp", dst_dram, o[0:64, :])

    for tb in range(TPC // TB):
        tb0 = tb * TB
        for hf_ in range(NH):
            t0 = tb0 + hf_ * TT
            ho = hf_ * TT
            k.dma("sp", posi.v, pos[0:1, t0:t0 + TT].partition_broadcast(64) if False else pos.v[0:1, t0:t0 + TT].ap.partition_broadcast(64) if False else V(pos.base[0:1, t0:t0 + TT].partition_broadcast(64), pos))
            k.copy("dve", ang.v, posi.v)
            k.ts("dve", ang.v, ang.v, invt[:, 0:1], None, ALU.mult)
            def sin_of(dst, src):
                k.ts("dve", tmpa.v, src, 1.0 / TWO_PI, MAGIC, ALU.mult, ALU.add)
                k.ts("dve", tmpa.v, tmpa.v, MAGIC, None, ALU.subtract)
                k.stt("dve", tmpb.v, tmpa.v, -CW1, src, ALU.mult, ALU.add)
                k.stt("dve", tmpb.v, tmpa.v, -CW2, tmpb.v, ALU.mult, ALU.add)
                k.ts("dve", tmpb.v, tmpb.v, PI_LO, -PI_LO, ALU.min, ALU.max)
                k.act(dst, tmpb.v, AF.Sin)
            sin_of(sin2.v, ang.v)
            k.ts("dve", ang.v, ang.v, PI / 2, None, ALU.add)
            sin_of(cos2.v, ang.v)
            for s in range(TT // 128):
                xt = xin.next()
                k.dma("sp", xt.v, x[t0 + s * 128:t0 + (s + 1) * 128, :])
                st = stat.next()
                hb = hbf.next()
                k.act(hb.v, xt.v, AF.Square, accum_out=st[:, 0:1])
                k.act(st[:, 1:2], st[:, 0:1], AF.Sqrt, scale=1.0 / D, bias=epst[:, 0:1])
                k.recip(st[:, 2:3], st[:, 1:2])
                k.ts("dve", hb.v, xt.v, st[:, 2:3], None, ALU.mult)
                for g8 in range(KC // 8):
                    pt = ptr.next()
                    for j in range(8):
                        kc = g8 * 8 + j
                        k.tr(pt[:, j, :], hb[:, kc * 128:(kc + 1) * 128], ident.v)
                    gb = V(gT.base[:, g8 * 8:(g8 + 1) * 8].unsqueeze(2).to_broadcast([128, 8, 128]), gT)
                    k.tt("dve", hT[:, g8 * 8:(g8 + 1) * 8, ho + s * 128:ho + (s + 1) * 128], pt.v, gb, ALU.mult)

            def fm_chunk(wb, j, evac):
                pa = pacc.next()
                for kc in range(KC):
                    k.mm(pa.v, wb[:, kc, j * 128:(j + 1) * 128], hT[:, kc, ho:ho + TT], start=(kc == 0), stop=(kc == KC - 1))
                evac(pa)

            def stream_fm(col0, ncols, evac_fn):
                for c in range(ncols // 256):
                    wb = wbuf.next()
                    k.dma("pool", wb.v, w_in_v[:, :, col0 + c * 256:col0 + (c + 1) * 256])
                    for j in range(2):
                        fm_chunk(wb, j, lambda pa, idx=c * 2 + j: evac_fn(pa, idx))

            stream_fm(OFF_CQ, QL, lambda pa, idx: evac_copy(cqT[:, idx, :], pa.v))
            stream_fm(OFF_CKV, KVL, lambda pa, idx: evac_copy(ckvT[:, idx, :], pa.v))

            pa = pacc.next()
            pb = pacc.next()
            for kc in range(KC):
                k.mm(pa[0:64, :], wkr[:, kc, :], hT[:, kc, ho:ho + TT], start=(kc == 0), stop=(kc == KC - 1))
            for kc in range(KC):
                k.mm(pb[0:64, :], wkrr[:, kc, :], hT[:, kc, ho:ho + TT], start=(kc == 0), stop=(kc == KC - 1))
            rope(pa[0:64, :], pb[0:64, :], o_kr[:, t0:t0 + TT])

            def subnorm(srcT, nch, width, gvec, rstd, dst):
                for j in range(nch):
                    sq = sq32.next()
                    k.tt("pool", sq.v, srcT[:, j, :], srcT[:, j, :], ALU.mult)
                    k.mm(pss.v, ones_f.v, sq.v, start=(j == 0), stop=(j == nch - 1))
                k.act(rstd.v, pss.v, AF.Sqrt, scale=1.0 / width, bias=epst[:, 0:1])
                k.recip(rstd.v, rstd.v)
                for j in range(nch):
                    k.stt("dve", dst[:, j, :], srcT[:, j, :], gvec[:, j:j + 1], rstd.v, ALU.mult, ALU.mult)

            subnorm(cqT, 8, QL, gqT, rstd_q, cqn)
            subnorm(ckvT, 4, KVL, gkvT, rstd_kv, ckvn)

            for h in range(HA):
                if h % 2 == 0:
                    k.dma("pool", wuq_g.v, w_uq_v[:, :, h * 192:(h + 2) * 192])
                    load_wuqr(h)
                hl = h % 2
                pa = pacc.next()
                for kc in range(8):
                    k.mm(pa.v, wuq_g[:, kc, hl * 192:hl * 192 + 128], cqn[:, kc, :], start=(kc == 0), stop=(kc == 7))
                o = stg.next()
                evac_copy(o.v, pa.v)
                k.dma("sp", o_qn[h, :, t0:t0 + TT], o.v)
                pa = pacc.next()
                pb = pacc.next()
                for kc in range(8):
                    k.mm(pa[0:64, :], wuq_g[:, kc, hl * 192 + 128:hl * 192 + 192], cqn[:, kc, :], start=(kc == 0), stop=(kc == 7))
                for kc in range(8):
                    k.mm(pb[0:64, :], wuqr[:, kc, hl, :], cqn[:, kc, :], start=(kc == 0), stop=(kc == 7))
                rope(pa[0:64, :], pb[0:64, :], o_qr[h, :, t0:t0 + TT])
            wv = V(wukv_g.base.rearrange("p kc (h two c) -> p kc h two c", two=2, c=128), wukv_g)
            for hg in range(4):
                k.dma("pool", wukv_g.v, w_ukv_v[:, :, hg * 1024:(hg + 1) * 1024])
                for hl in range(4):
                    h = hg * 4 + hl
                    pa = pacc.next()
                    for kc in range(4):
                        k.mm(pa.v, wukv_g[:, kc, hl * 256:hl * 256 + 128], ckvn[:, kc, :], start=(kc == 0), stop=(kc == 3))
                    o = stg.next()
                    evac_copy(o.v, pa.v)
                    k.dma("sp", o_kn[h, :, t0:t0 + TT], o.v)
                for s in range(TT // 128):
                    pa = pacc.next()
                    pav = V(pa.base.rearrange("p (h c) -> p h c", c=128), pa)
                    for kc in range(4):
                        k.mm(pav, ckvn[:, kc, s * 128:(s + 1) * 128], wv[:, kc, :, 1, :],
                             start=(kc == 0), stop=(kc == 3))
                    o = stg.next()
                    evac_copy(o.v, pa.v)
                    k.dma("sp", o_va[t0 + s * 128:t0 + (s + 1) * 128, hg * 512:(hg + 1) * 512], o.v)

        def ev_out(dst):
            def f(pa, idx, t0):
                o = stg.next()
                evac_copy(o.v, pa.v)
                k.dma("sp", dst[idx, :, t0:t0 + TT], o.v)
            return f

        def ev_sig(dst):
            def f(pa, idx, t0):
                o = stg.next()
                k.act(o.v, pa.v, AF.Sigmoid)
                k.dma("sp", dst[idx, :, t0:t0 + TT], o.v)
            return f

        def stream_bulk(col0, ncols, evac_fn):
            for c in range(ncols // 256):
                wb = wbuf.next()
                k.dma("pool", wb.v, w_in_v[:, :, col0 + c * 256:col0 + (c + 1) * 256])
                for hf in range(NH):
                    ho = hf * TT
                    for j in range(2):
                        pa = pacc.next()
                        for kc in range(KC):
                            k.mm(pa.v, wb[:, kc, j * 128:(j + 1) * 128], hT[:, kc, ho:ho + TT], start=(kc == 0), stop=(kc == KC - 1))
                        evac_fn(pa, c * 2 + j, tb0 + ho)
        stream_bulk(OFF_QB, 2048, ev_out(o_qb))
        stream_bulk(OFF_KB, 2048, ev_out(o_kb))
        for c in range(2048 // 256):
            wb = wbuf.next()
            k.dma("pool", wb.v, w_in_v[:, :, OFF_VB + c * 256:OFF_VB + (c + 1) * 256])
            for s in range(TB // 128):
                pa = pacc.next()
                for kc in range(KC):
                    k.mm(pa[:, 0:256], hT[:, kc, s * 128:(s + 1) * 128], wb[:, kc, :], start=(kc == 0), stop=(kc == KC - 1))
                o = stg.next()
                evac_copy(o[:, 0:256], pa[:, 0:256])
                k.dma("sp", o_vb[tb0 + s * 128:tb0 + (s + 1) * 128, c * 256:(c + 1) * 256], o[:, 0:256])
        stream_bulk(OFF_GA, D, ev_sig(o_ga))
        stream_bulk(OFF_GB, D, ev_sig(o_gb))
    nc = k.finish()
    return nc, k


def build_attn(S):
    NB = S // 128
    NQ = S // 512
    k = KB()
    qn = k.dram("qn", [2, 128, S], BF16, "ExternalInput")
    qr = k.dram("qr", [2, 64, S], BF16, "ExternalInput")
    kn = k.dram("kn", [2, 128, S], BF16, "ExternalInput")
    kr = k.dram("kr", [64, S], BF16, "ExternalInput")
    va = k.dram("va", [2, 128, NB, 128], BF16, "ExternalInput")
    qb = k.dram("qb", [2, 128, S], BF16, "ExternalInput")
    kb = k.dram("kb", [2, 128, S], BF16, "ExternalInput")
    vb = k.dram("vb", [128, NB, 256], BF16, "ExternalInput")
    pos = k.dram("pos", [1, S], I32, "ExternalInput")
    posk = k.dram("posk", [128, NB], I32, "ExternalInput")
    lam = k.dram("lam", [1, 4, 128], F32, "ExternalInput")
    subln = k.dram("subln", [256], F32, "ExternalInput")
    consts = k.dram("consts", [128, 4], F32, "ExternalInput")
    oa = k.dram("oa", [2, 128, S], BF16, "ExternalOutput")
    ob = k.dram("ob", [2, 128, S], BF16, "ExternalOutput")

    SC_A = float((QKN + QKR) ** -0.5)
    SC_B = float(DHB ** -0.5)

    ones_b = k.sb([128, 128], BF16, "ones_b")
    k.memset("dve", ones_b.v, 1.0)
    ones_f = k.sb([128, 128], F32, "ones_f")
    k.memset("dve", ones_f.v, 1.0)
    tri_f = k.sb([128, 128], F32, "tri_f")
    k.memset("dve", tri_f.v, 1.0)
    k.gen("pool", lambda q: q.affine_select(tri_f.base, tri_f.base, [[1, 128]], ALU.is_ge, 0.0, base=0,
                                            channel_multiplier=-1), [tri_f.v], [tri_f.v])
    tri = k.sb([128, 128], BF16, "tri")
    k.copy("dve", tri.v, tri_f.v)
    epst = k.sb([128, 1], F32, "epst")
    k.memset("dve", epst.v, EPS)

    cst = k.sb([128, 4], F32, "cst")
    k.dma("sp", cst.v, consts.v)
    lamt = k.sb([1, 4, 128], F32, "lamt")
    k.dma("sp", lamt.v, lam.v)
    lw = k.sb([1, 8], F32, "lw")
    lj = k.sb([1, 128], F32, "lj")
    k.tt("dve", lj.v, lamt[:, 0, :], lamt[:, 1, :], ALU.mult)
    k.reduce("dve", lw[:, 0:1], lj.v, ALU.add)
    k.tt("dve", lj.v, lamt[:, 2, :], lamt[:, 3, :], ALU.mult)
    k.reduce("dve", lw[:, 1:2], lj.v, ALU.add)
    k.act(lw[:, 2:4], lw[:, 0:2], AF.Exp)
    k.tt("dve", lw[:, 4:5], lw[:, 2:3], lw[:, 3:4], ALU.subtract)
    k.tt("dve", lw[:, 5:6], lw[:, 4:5], cst[0:1, 1:2], ALU.add)
    k.ts("dve", lw[:, 6:7], lw[:, 5:6], -1.0, None, ALU.mult)
    pmisc = k.ps([128, 512], F32, "pmisc")
    k.mm(pmisc[:, 0:1], ones_f[0:1, :], lw[0:1, 6:7])
    neglam = k.sb([128, 1], F32, "neglam")
    k.copy("dve", neglam.v, pmisc[:, 0:1])
    subs = k.sb([128, 2], F32, "subs")
    k.dma("sp", subs.v, subln.v.rearrange("(c p) -> p c", p=128), allow_slow_non_contiguous=True)
    k.ts("dve", subs.v, subs.v, cst[:, 2:3], None, ALU.mult)
    pk_i = k.sb([128, NB], I32, "pk_i")
    k.dma("sp", pk_i.v, posk.v)
    pos0_i = k.sb([128, 1], I32, "pos0_i")
    k.dma("sp", pos0_i.v, V(pos.base[0:1, 0:1].partition_broadcast(128), pos))
    pos0 = k.sb([128, 1], F32, "pos0")
    k.copy("dve", pos0.v, pos0_i.v)
    mps = k.sb([128, 2], F32, "mps")
    k.ts("dve", mps[:, 0:1], cst[:, 0:1], 1.0 / SC_B, None, ALU.mult)
    k.ts("dve", mps[:, 1:2], cst[:, 0:1], -1.0 / SC_B, None, ALU.mult)
    pks = k.sb([128, NB], F32, "pks")
    k.copy("dve", pks.v, pk_i.v)
    k.ts("dve", pks.v, pks.v, pos0[:, 0:1], mps[:, 0:1], ALU.subtract, ALU.mult)

    bufK = k.sb([128, S], BF16, "bufK")
    bufKR = k.sb([64, S], BF16, "bufKR")
    bufV = k.sb([128, NB, 256], BF16, "bufV")
    k.dma("sp", bufKR.v, kr.v)

    qt_a = Rot([k.sb([128, 512], BF16, f"qt_a{i}") for i in range(2)])
    qt_r = Rot([k.sb([64, 512], BF16, f"qt_r{i}") for i in range(2)])
    pT = Rot([k.sb([128, 512], BF16, f"pT{i}") for i in range(4)])
    sfp = Rot([k.sb([128, 512], F32, f"sfp{i}") for i in range(2)])
    qbs_i = k.sb([128, 512], I32, "qbs_i")
    qbs = k.sb([128, 512], F32, "qbs")
    rec = k.sb([128, 512], F32, "rec")
    o1 = k.sb([128, 2, 512], F32, "o1")
    o2 = k.sb([128, 2, 512], F32, "o2")
    sqt = Rot([k.sb([128, 512], F32, f"sqt{i}") for i in range(2)])
    rstd = k.sb([128, 512], F32, "rstd")
    ostg = Rot([k.sb([128, 512], BF16, f"ostg{i}") for i in range(3)])

    psS = Rot([k.ps([128, 512], F32, f"psS{i}") for i in range(2)])
    po = [k.ps([128, 512], F32, f"po{i}") for i in range(2)]
    pd = k.ps([128, 512], F32, "pd")

    def run_pass(j, ndv, qk_fn, exp_fn):
        q0 = j * 512
        nkb = 4 * (j + 1)
        pend = None

        def flush(pp):
            (P, i, c0) = pp
            for c in range(ndv):
                k.mm(po[c][:, c0:], bufV[:, i, c * 128:(c + 1) * 128], P[:, c0:], start=(i == 0), stop=(i == nkb - 1))
            k.mm(pd[:, c0:], ones_b.v, P[:, c0:], start=(i == 0), stop=(i == nkb - 1))
        for i in range(nkb):
            c0 = max(0, 128 * (i - 4 * j))
            ps = psS.next()
            qk_fn(ps, i, c0)
            P = pT.next()
            exp_fn(P, ps, i, c0)
            if i >= 4 * j:
                k.tt("pool", P[:, c0:c0 + 128], P[:, c0:c0 + 128], tri.v, ALU.mult)
            if pend is not None:
                flush(pend)
            pend = (P, i, c0)
        flush(pend)

    for h in range(2):
        k.dma("sp", bufK.v, kn[h])
        k.dma("sp", bufV[:, :, 0:128], va[h])
        for j in range(NQ):
            q0 = j * 512
            qa_t = qt_a.next()
            qr_t = qt_r.next()
            k.dma("sp", qa_t.v, qn[h, :, q0:q0 + 512])
            k.dma("sp", qr_t.v, qr[h, :, q0:q0 + 512])

            def qk_fn(ps, i, c0):
                k.mm(ps[:, c0:], bufK[:, i * 128:(i + 1) * 128], qa_t[:, c0:], start=True, stop=False)
                k.mm(ps[:, c0:], bufKR[:, i * 128:(i + 1) * 128], qr_t[:, c0:], start=False, stop=True)

            def exp_fn(P, ps, i, c0):
                k.act(P[:, c0:], ps[:, c0:], AF.Exp, scale=SC_A)
            run_pass(j, 1, qk_fn, exp_fn)
            k.recip(rec.v, pd.v)
            o = ostg.next()
            k.tt("dve", o.v, po[0].v, rec.v, ALU.mult)
            k.dma("sp", oa[h, :, q0:q0 + 512], o.v)

    k.dma("sp", bufV.v, vb.v)
    osave = [o1, o2]
    bufK2 = bufK
    kmaps = [bufK, None]
    bufKb2 = k.sb([128, S], BF16, "bufKb2")
    kmaps[1] = bufKb2
    k.dma("sp", bufK.v, kb[0])
    k.dma("sp", bufKb2.v, kb[1])
    for j in range(NQ):
        q0 = j * 512
        k.dma("sp", qbs_i.v, V(pos.base[0:1, q0:q0 + 512].partition_broadcast(128), pos))
        k.copy("dve", qbs.v, qbs_i.v)
        k.ts("dve", qbs.v, qbs.v, pos0[:, 0:1], mps[:, 1:2], ALU.subtract, ALU.mult)
        for m in range(2):
            qa_t = qt_a.next()
            k.dma("sp", qa_t.v, qb[m, :, q0:q0 + 512])
            Kb = kmaps[m]

            def qk_fn(ps, i, c0):
                k.mm(ps[:, c0:], Kb[:, i * 128:(i + 1) * 128], qa_t[:, c0:], start=True, stop=True)

            def exp_fn(P, ps, i, c0):
                sf = sfp.next()
                k.stt("dve", sf[:, c0:], ps[:, c0:], pks[:, i:i + 1], qbs[:, c0:], ALU.add, ALU.add)
                k.act(P[:, c0:], sf[:, c0:], AF.Exp, scale=SC_B)
            run_pass(j, 2, qk_fn, exp_fn)
            k.recip(rec.v, pd.v)
            for c in range(2):
                k.tt("dve", osave[m][:, c, :], po[c].v, rec.v, ALU.mult)
        for c in range(2):
            k.stt("dve", o1[:, c, :], o2[:, c, :], neglam[:, 0:1], o1[:, c, :], ALU.mult, ALU.add)
            sq = sqt.next()
            k.tt("pool", sq.v, o1[:, c, :], o1[:, c, :], ALU.mult)
            k.mm(pmisc.v, ones_f.v, sq.v, start=(c == 0), stop=(c == 1))
        k.act(rstd.v, pmisc.v, AF.Sqrt, scale=1.0 / 256, bias=epst[:, 0:1])
        k.recip(rstd.v, rstd.v)
        for c in range(2):
            o = ostg.next()
            k.stt("dve", o.v, o1[:, c, :], subs[:, c:c + 1], rstd.v, ALU.mult, ALU.mult)
            k.dma("sp", ob[c, :, q0:q0 + 512], o.v)
    nc = k.finish()
    return nc, k


def build_attn2(S):
    NB = S // 128
    NQ = S // 512
    k = KB()
    qn = k.dram("qn", [2, 128, S], BF16, "ExternalInput")
    qr = k.dram("qr", [2, 64, S], BF16, "ExternalInput")
    kn = k.dram("kn", [2, 128, S], BF16, "ExternalInput")
    kr = k.dram("kr", [64, S], BF16, "ExternalInput")
    va = k.dram("va", [2, 128, NB, 128], BF16, "ExternalInput")
    qb = k.dram("qb", [2, 128, S], BF16, "ExternalInput")
    kb = k.dram("kb", [2, 128, S], BF16, "ExternalInput")
    vb = k.dram("vb", [128, NB, 256], BF16, "ExternalInput")
    pos = k.dram("pos", [1, S], I32, "ExternalInput")
    posk = k.dram("posk", [128, NB], I32, "ExternalInput")
    lam = k.dram("lam", [1, 4, 128], F32, "ExternalInput")
    subln = k.dram("subln", [256], F32, "ExternalInput")
    consts = k.dram("consts", [128, 4], F32, "ExternalInput")
    oa = k.dram("oa", [2, 128, S], BF16, "ExternalOutput")
    ob = k.dram("ob", [2, 128, S], BF16, "ExternalOutput")

    SC_A = float((QKN + QKR) ** -0.5)
    SC_B = float(DHB ** -0.5)

    ones_b = k.sb([128, 128], BF16, "ones_b")
    k.memset("dve", ones_b.v, 1.0)
    ones_f = k.sb([128, 128], F32, "ones_f")
    k.memset("dve", ones_f.v, 1.0)
    tri_f = k.sb([128, 128], F32, "tri_f")
    k.memset("dve", tri_f.v, 1.0)
    k.gen("pool", lambda q: q.affine_select(tri_f.base, tri_f.base, [[1, 128]], ALU.is_ge, 0.0, base=0,
                                            channel_multiplier=-1), [tri_f.v], [tri_f.v])
    tri = k.sb([128, 128], BF16, "tri")
    k.copy("dve", tri.v, tri_f.v)
    epst = k.sb([128, 1], F32, "epst")
    k.memset("dve", epst.v, EPS)

    cst = k.sb([128, 4], F32, "cst")
    k.dma("sp", cst.v, consts.v)
    lamt = k.sb([1, 4, 128], F32, "lamt")
    k.dma("sp", lamt.v, lam.v)
    lw = k.sb([1, 8], F32, "lw")
    lj = k.sb([1, 128], F32, "lj")
    k.tt("dve", lj.v, lamt[:, 0, :], lamt[:, 1, :], ALU.mult)
    k.reduce("dve", lw[:, 0:1], lj.v, ALU.add)
    k.tt("dve", lj.v, lamt[:, 2, :], lamt[:, 3, :], ALU.mult)
    k.reduce("dve", lw[:, 1:2], lj.v, ALU.add)
    k.act(lw[:, 2:4], lw[:, 0:2], AF.Exp)
    k.tt("dve", lw[:, 4:5], lw[:, 2:3], lw[:, 3:4], ALU.subtract)
    k.tt("dve", lw[:, 5:6], lw[:, 4:5], cst[0:1, 1:2], ALU.add)
    k.ts("dve", lw[:, 6:7], lw[:, 5:6], -1.0, None, ALU.mult)
    pmisc = k.ps([128, 512], F32, "pmisc")
    k.mm(pmisc[:, 0:1], ones_f[0:1, :], lw[0:1, 6:7])
    neglam = k.sb([128, 1], F32, "neglam")
    k.copy("dve", neglam.v, pmisc[:, 0:1])
    subs = k.sb([128, 2], F32, "subs")
    k.dma("sp", subs.v, subln.v.rearrange("(c p) -> p c", p=128), allow_slow_non_contiguous=True)
    k.ts("dve", subs.v, subs.v, cst[:, 2:3], None, ALU.mult)
    pk_i = k.sb([128, NB], I32, "pk_i")
    k.dma("sp", pk_i.v, posk.v)
    pos0_i = k.sb([128, 1], I32, "pos0_i")
    k.dma("sp", pos0_i.v, V(pos.base[0:1, 0:1].partition_broadcast(128), pos))
    pos0 = k.sb([128, 1], F32, "pos0")
    k.copy("dve", pos0.v, pos0_i.v)
    mps = k.sb([128, 2], F32, "mps")
    k.ts("dve", mps[:, 0:1], cst[:, 0:1], 1.0 / SC_B, None, ALU.mult)
    k.ts("dve", mps[:, 1:2], cst[:, 0:1], -1.0 / SC_B, None, ALU.mult)
    pks = k.sb([128, NB], F32, "pks")
    k.copy("dve", pks.v, pk_i.v)
    k.ts("dve", pks.v, pks.v, pos0[:, 0:1], mps[:, 0:1], ALU.subtract, ALU.mult)

    bufK = k.sb([128, S], BF16, "bufK")
    bufKR_full = k.sb([128, S], BF16, "bufKR")
    bufKR = V(bufKR_full.base[0:64, :], bufKR_full)
    bufV = k.sb([128, NB, 256], BF16, "bufV")
    k.memset("dve", bufKR_full[64:128, :], 0.0)
    k.dma("sp", bufKR, kr.v)

    qt_a = Rot([k.sb([128, 512], BF16, f"qt_a{i}") for i in range(2)])
    qt_r = Rot([k.sb([128, 512], BF16, f"qt_r{i}") for i in range(2)])
    for b_ in qt_r.bufs:
        k.memset("dve", b_[64:128, :], 0.0)
    pT = Rot([k.sb([128, 512], BF16, f"pT{i}") for i in range(6)])
    dacc = [k.sb([128, 512], F32, f"dacc{i}") for i in range(2)]
    dsum = k.sb([128, 512], F32, "dsum")
    wq = k.sb([128, 512], F32, "wq")
    qrel = k.sb([128, 512], F32, "qrel")
    ref_i = k.sb([128, 1], I32, "ref_i")
    reff = k.sb([128, 1], F32, "reff")
    biasj = k.sb([128, NB], F32, "biasj")
    pkf = k.sb([128, NB], F32, "pkf")
    k.copy("dve", pkf.v, pk_i.v)
    negm = k.sb([128, 1], F32, "negm")
    k.ts("dve", negm.v, cst[:, 0:1], -1.0, None, ALU.mult)
    otmp = Rot([k.sb([128, 512], F32, f"otmp{i}") for i in range(2)])
    sfp = Rot([k.sb([128, 512], F32, f"sfp{i}") for i in range(2)])
    qbs_i = k.sb([128, 512], I32, "qbs_i")
    qbs = k.sb([128, 512], F32, "qbs")
    rec = k.sb([128, 512], F32, "rec")
    o1 = k.sb([128, 2, 512], F32, "o1")
    o2 = k.sb([128, 2, 512], F32, "o2")
    sqt = Rot([k.sb([128, 512], F32, f"sqt{i}") for i in range(2)])
    rstd = k.sb([128, 512], F32, "rstd")
    ostg = Rot([k.sb([128, 512], BF16, f"ostg{i}") for i in range(3)])

    psS = Rot([k.ps([128, 512], F32, f"psS{i}") for i in range(3)])
    po = [k.ps([128, 512], F32, f"po{i}") for i in range(2)]
    po_d = [k.ps([128, 512], F32, f"po_d{i}") for i in range(2)]

    def run_blocks(j, blks, ndv, pacc_, dac, qk_fn, exp_fn):
        nb_ = len(blks)
        pend = []

        def flush(pp):
            (P, i, c0, idx) = pp
            for c in range(ndv):
                k.mm(pacc_[c][:, c0:], bufV[:, i, c * 128:(c + 1) * 128], P[:, c0:], start=(idx == 0), stop=(idx == nb_ - 1))
        for idx, i in enumerate(blks):
            c0 = max(0, 128 * (i - 4 * j))
            ps = psS.next()
            qk_fn(ps, i, c0)
            P = pT.next()
            exp_fn(P, ps, i, c0)
            if i >= 4 * j:
                k.tt("pool", P[:, c0:c0 + 128], P[:, c0:c0 + 128], tri.v, ALU.mult)
            if idx == 0:
                k.copy("dve", dac.v, P.v)
            else:
                k.tt("dve", dac[:, c0:], dac[:, c0:], P[:, c0:], ALU.add)
            pend.append((P, i, c0, idx))
            if len(pend) > 2:
                flush(pend.pop(0))
        for pp in pend:
            flush(pp)

    for h in range(2):
        k.dma("sp", bufK.v, kn[h])
        k.dma("sp", bufV[:, :, 0:128], va[h])
        for j in range(NQ):
            q0 = j * 512
            qa_t = qt_a.next()
            qr_t = qt_r.next()
            k.dma("sp", qa_t.v, qn[h, :, q0:q0 + 512])
            k.dma("sp", qr_t[0:64, :], qr[h, :, q0:q0 + 512])

            def qk_fn(ps, i, c0):
                k.mm(ps[:, c0:], bufK[:, i * 128:(i + 1) * 128], qa_t[:, c0:], start=True, stop=False)
                k.mm(ps[:, c0:], bufKR_full[:, i * 128:(i + 1) * 128], qr_t[:, c0:], start=False, stop=True)

            def exp_fn(P, ps, i, c0):
                k.act(P[:, c0:], ps[:, c0:], AF.Exp, scale=SC_A)
            run_blocks(j, list(range(4 * (j + 1))), 1, po, dacc[0], qk_fn, exp_fn)
            k.mm(pmisc.v, ones_f.v, dacc[0].v)
            k.recip(rec.v, pmisc.v)
            o = ostg.next()
            k.tt("dve", o.v, po[0].v, rec.v, ALU.mult)
            k.dma("sp", oa[h, :, q0:q0 + 512], o.v)

    k.dma("sp", bufV.v, vb.v)
    osave = [o1, o2]
    bufK2 = bufK
    kmaps = [bufK, None]
    bufKb2 = bufKR_full
    kmaps[1] = bufKb2
    k.dma("sp", bufK.v, kb[0])
    k.dma("sp", bufKb2.v, kb[1])
    for j in range(NQ):
        q0 = j * 512
        k.dma("sp", qbs_i.v, V(pos.base[0:1, q0:q0 + 512].partition_broadcast(128), pos))
        k.copy("dve", qbs.v, qbs_i.v)
        k.dma("sp", ref_i.v, V(pos.base[0:1, q0:q0 + 1].partition_broadcast(128), pos))
        k.copy("dve", reff.v, ref_i.v)
        k.ts("dve", qrel.v, qbs.v, reff[:, 0:1], None, ALU.subtract)
        k.act(wq.v, qrel.v, AF.Exp, scale=negm[:, 0:1])
        if j > 0:
            k.ts("dve", biasj[:, 0:4 * j], pkf[:, 0:4 * j], reff[:, 0:1], cst[:, 0:1], ALU.subtract, ALU.mult)
        k.ts("dve", qbs.v, qbs.v, pos0[:, 0:1], mps[:, 1:2], ALU.subtract, ALU.mult)
        for m in range(2):
            qa_t = qt_a.next()
            k.dma("sp", qa_t.v, qb[m, :, q0:q0 + 512])
            Kb = kmaps[m]

            def qk_fn(ps, i, c0):
                k.mm(ps[:, c0:], Kb[:, i * 128:(i + 1) * 128], qa_t[:, c0:], start=True, stop=True)

            def exp_d(P, ps, i, c0):
                sf = sfp.next()
                k.stt("dve", sf[:, c0:], ps[:, c0:], pks[:, i:i + 1], qbs[:, c0:], ALU.add, ALU.add)
                k.act(P[:, c0:], sf[:, c0:], AF.Exp, scale=SC_B)

            def exp_nd(P, ps, i, c0):
                k.act(P.v, ps.v, AF.Exp, scale=SC_B, bias=biasj[:, i:i + 1])
            if j > 0:
                run_blocks(j, list(range(4 * j)), 2, po, dacc[0], qk_fn, exp_nd)
            run_blocks(j, list(range(4 * j, 4 * j + 4)), 2, po_d, dacc[1], qk_fn, exp_d)
            if j > 0:
                k.tt("dve", dsum.v, dacc[0].v, wq.v, ALU.mult)
                k.tt("dve", dsum.v, dsum.v, dacc[1].v, ALU.add)
                k.mm(pmisc.v, ones_f.v, dsum.v)
            else:
                k.mm(pmisc.v, ones_f.v, dacc[1].v)
            k.recip(rec.v, pmisc.v)
            for c in range(2):
                if j > 0:
                    ot = otmp.next()
                    k.tt("dve", ot.v, po[c].v, wq.v, ALU.mult)
                    k.tt("dve", ot.v, ot.v, po_d[c].v, ALU.add)
                    k.tt("dve", osave[m][:, c, :], ot.v, rec.v, ALU.mult)
                else:
                    k.tt("dve", osave[m][:, c, :], po_d[c].v, rec.v, ALU.mult)
        for c in range(2):
            k.stt("dve", o1[:, c, :], o2[:, c, :], neglam[:, 0:1], o1[:, c, :], ALU.mult, ALU.add)
            sq = sqt.next()
            k.tt("pool", sq.v, o1[:, c, :], o1[:, c, :], ALU.mult)
            k.mm(pmisc.v, ones_f.v, sq.v, start=(c == 0), stop=(c == 1))
        k.act(rstd.v, pmisc.v, AF.Sqrt, scale=1.0 / 256, bias=epst[:, 0:1])
        k.recip(rstd.v, rstd.v)
        for c in range(2):
            o = ostg.next()
            k.stt("dve", o.v, o1[:, c, :], subs[:, c:c + 1], rstd.v, ALU.mult, ALU.mult)
            k.dma("sp", ob[c, :, q0:q0 + 512], o.v)
    nc = k.finish()
    return nc, k


def build_t2(TPC, CAP):
    TT = 512
    NT = TPC // TT
    NSLOT = NG * CAP
    k = KB()
    oa = k.dram("oa", [16, 128, TPC], BF16, "ExternalInput")
    ob = k.dram("ob", [16, 128, TPC], BF16, "ExternalInput")
    ga = k.dram("ga", [32, 128, TPC], BF16, "ExternalInput")
    gb = k.dram("gb", [32, 128, TPC], BF16, "ExternalInput")
    x = k.dram("x", [TPC, D], F32, "ExternalInput")
    w_oa = k.dram("w_oa", [2048, D], F32, "ExternalInput")
    w_ob = k.dram("w_ob", [2048, D], F32, "ExternalInput")
    w_out = k.dram("w_out", [D, D], F32, "ExternalInput")
    g_ffn = k.dram("g_ffn", [1, D], F32, "ExternalInput")
    w_r = k.dram("w_r", [D, 72], F32, "ExternalInput")
    b_r = k.dram("b_r", [1, 72], F32, "ExternalInput")
    goff = k.dram("goff", [1, 8], F32, "ExternalInput")
    x1 = k.dram("x1", [TPC, D], F32, "ExternalOutput")
    xg = k.dram("xg", [NSLOT, D], BF16, "ExternalOutput")
    gate = k.dram("gate", [NSLOT, 8], F32, "ExternalOutput")
    slot = k.dram("slot", [TPC, 1], I32, "ExternalOutput")

    ident_f = make_ident(k, F32, "ident")
    ones_f = k.sb([128, 128], F32, "ones_f")
    k.memset("dve", ones_f.v, 1.0)
    ustr = k.sb([128, 128], F32, "ustr")
    k.memset("dve", ustr.v, 1.0)
    k.gen("pool", lambda q: q.affine_select(ustr.base, ustr.base, [[1, 128]], ALU.is_gt, 0.0, base=0,
                                            channel_multiplier=-1), [ustr.v], [ustr.v])
    epst = k.sb([128, 1], F32, "epst")
    k.memset("dve", epst.v, EPS)
    gft = k.sb([128, D], F32, "gft")
    k.dma("sp", gft.v, V(g_ffn.base[0:1, :].partition_broadcast(128), g_ffn))
    brt = k.sb([128, 72], F32, "brt")
    k.dma("sp", brt.v, V(b_r.base[0:1, :].partition_broadcast(128), b_r))
    gofft = k.sb([128, 8], F32, "gofft")
    k.dma("sp", gofft.v, V(goff.base[0:1, :].partition_broadcast(128), goff))
    wr = k.sb([128, KC, 72], F32, "wr")
    k.dma("sp", wr.v, w_r.v.rearrange("(kc p) n -> p kc n", p=128))
    h2b = k.sb([128, D], BF16, "h2b")
    zb = h2b
    k.memset("pool", zb.v, 0.0)
    zg = k.sb([128, 8], F32, "zg")
    k.memset("pool", zg.v, 0.0)
    for r in range(NSLOT // 128):
        k.dma("sp", xg[r * 128:(r + 1) * 128, :], zb.v)
        k.dma("sp", gate[r * 128:(r + 1) * 128, :], zg.v)

    w_oa_v = w_oa.v.rearrange("(kc p) n -> p kc n", p=128)
    w_ob_v = w_ob.v.rearrange("(kc p) n -> p kc n", p=128)
    w_out_v = w_out.v.rearrange("(kc p) n -> p kc n", p=128)

    oaT = k.sb([128, 16, TT], BF16, "oaT")
    obT = k.sb([128, 16, TT], BF16, "obT")
    mT = k.sb([128, KC, TT], BF16, "mT")
    wab = Rot([k.sb([128, 16, 256], BF16, f"wab{i}") for i in range(2)])
    wo = Rot([k.sb([128, KC, 256], BF16, f"wo{i}") for i in range(2)])
    gt = Rot([k.sb([128, TT], BF16, f"gt{i}") for i in range(4)])
    tmp1 = Rot([k.sb([128, TT], F32, f"tmp1_{i}") for i in range(2)])
    tmp2 = Rot([k.sb([128, TT], F32, f"tmp2_{i}") for i in range(2)])
    xs = Rot([k.sb([128, 256], F32, f"xs{i}") for i in range(3)])
    xo = Rot([k.sb([128, 256], F32, f"xo{i}") for i in range(3)])
    x1t = k.sb([128, D], F32, "x1t")
    h2f = x1t
    h2T = k.sb([128, KC, 128], F32, "h2T")
    gcar = k.sb([128, 8], F32, "gcar")
    k.memset("dve", gcar.v, 0.0)
    sm = Rot([k.sb([128, 256], F32, f"sm{i}") for i in range(2)])
    sloti = Rot([k.sb([128, 1], I32, f"sloti{i}") for i in range(2)])
    g8 = Rot([k.sb([128, 8], F32, f"g8_{i}") for i in range(2)])

    pacc = Rot([k.ps([128, 512], F32, f"pacc{i}") for i in range(4)])
    ptr = Rot([k.ps([128, 4, 128], F32, f"ptr{i}") for i in range(2)])
    plg = k.ps([128, 512], F32, "plg")
    prk = k.ps([128, 512], F32, "prk")

    for t in range(NT):
        t0 = t * TT
        k.dma("sp", oaT.v, oa.v[:, :, t0:t0 + TT].rearrange("c p t -> p c t"))
        k.dma("sp", obT.v, ob.v[:, :, t0:t0 + TT].rearrange("c p t -> p c t"))
        for c in range(D // 256):
            wa = wab.next()
            wb = wab.next()
            k.dma("pool", wa.v, w_oa_v[:, :, c * 256:(c + 1) * 256])
            k.dma("pool", wb.v, w_ob_v[:, :, c * 256:(c + 1) * 256])
            for j in range(2):
                idx = c * 2 + j
                pa = pacc.next()
                pb = pacc.next()
                for kc in range(16):
                    k.mm(pa.v, wa[:, kc, j * 128:(j + 1) * 128], oaT[:, kc, :], start=(kc == 0), stop=(kc == 15))
                for kc in range(16):
                    k.mm(pb.v, wb[:, kc, j * 128:(j + 1) * 128], obT[:, kc, :], start=(kc == 0), stop=(kc == 15))
                gat = gt.next()
                gbt = gt.next()
                k.dma("sp", gat.v, ga[idx, :, t0:t0 + TT])
                k.dma("sp", gbt.v, gb[idx, :, t0:t0 + TT])
                a1 = tmp1.next()
                a2 = tmp2.next()
                k.tt("dve", a1.v, pa.v, gat.v, ALU.mult)
                k.tt("dve", a2.v, pb.v, gbt.v, ALU.mult)
                k.tt("pool", mT[:, idx, :], a1.v, a2.v, ALU.add)
        for c in range(D // 256):
            wt = wo.next()
            k.dma("pool", wt.v, w_out_v[:, :, c * 256:(c + 1) * 256])
            for s in range(TT // 128):
                r0 = t0 + s * 128
                pa = pacc.next()
                for kc in range(KC):
                    k.mm(pa[:, 0:256], mT[:, kc, s * 128:(s + 1) * 128], wt[:, kc, :], start=(kc == 0), stop=(kc == KC - 1))
                xi = xs.next()
                k.dma("sp", xi.v, x[r0:r0 + 128, c * 256:(c + 1) * 256])
                xq = xo.next()
                k.tt("dve", xq.v, pa[:, 0:256], xi.v, ALU.add)
                k.dma("sp", x1[r0:r0 + 128, c * 256:(c + 1) * 256], xq.v)
        for s in range(TT // 128):
            r0 = t0 + s * 128
            k.dma("sp", x1t.v, x1[r0:r0 + 128, :])
            w = sm.next()
            k.act(h2b.v, x1t.v, AF.Square, accum_out=w[:, 0:1])
            k.act(w[:, 1:2], w[:, 0:1], AF.Sqrt, scale=1.0 / D, bias=epst[:, 0:1])
            k.recip(w[:, 2:3], w[:, 1:2])
            k.stt("dve", h2f.v, x1t.v, w[:, 2:3], gft.v, ALU.mult, ALU.mult)
            k.copy("pool", h2b.v, h2f.v)
            for g4 in range(KC // 4):
                pt = ptr.next()
                for j in range(4):
                    kc = g4 * 4 + j
                    k.tr(pt[:, j, :], h2f[:, kc * 128:(kc + 1) * 128], ident_f.v)
                k.copy("act" if g4 % 2 else "dve", h2T[:, g4 * 4:(g4 + 1) * 4, :], pt.v)
            for kc in range(KC):
                k.mm(plg[:, 0:72], h2T[:, kc, :], wr[:, kc, :], start=(kc == 0), stop=(kc == KC - 1))
            lgb = w[:, 96:160]
            k.tt("dve", w[:, 32:40], plg[:, 0:8], brt[:, 0:8], ALU.add)
            k.tt("dve", lgb, plg[:, 8:72], brt[:, 8:72], ALU.add)
            k.reduce("dve", w[:, 3:4], w[:, 32:40], ALU.max)
            k.ts("dve", w[:, 40:48], w[:, 32:40], w[:, 3:4], None, ALU.is_equal)
            k.ts("dve", w[:, 4:5], w[:, 3:4], -1.0, None, ALU.mult)
            k.act(w[:, 80:88], w[:, 32:40], AF.Exp, bias=w[:, 4:5], accum_out=w[:, 5:6])
            k.recip(w[:, 6:7], w[:, 5:6])
            el3 = V(w.base[:, 96:160].rearrange("p (g e) -> p g e", e=8), w)
            pr3 = V(w.base[:, 160:224].rearrange("p (g e) -> p g e", e=8), w)
            Gb = V(w.base[:, 40:48].unsqueeze(2).to_broadcast([128, 8, 8]), w)
            k.tt("dve", pr3, el3, Gb, ALU.mult)
            k.reduce("dve", w[:, 48:56], V(w.base[:, 160:224].rearrange("p (g e) -> p e g", e=8), w), ALU.add)
            k.reduce("dve", w[:, 7:8], w[:, 48:56], ALU.max)
            k.ts("dve", w[:, 56:64], w[:, 48:56], w[:, 7:8], None, ALU.is_equal)
            k.stt("dve", w[:, 64:72], w[:, 56:64], -1e30, w[:, 48:56], ALU.mult, ALU.add)
            k.reduce("dve", w[:, 8:9], w[:, 64:72], ALU.max)
            k.ts("dve", w[:, 72:80], w[:, 64:72], w[:, 8:9], None, ALU.is_equal)
            k.tt("dve", w[:, 9:10], w[:, 8:9], w[:, 7:8], ALU.subtract)
            k.act(w[:, 10:11], w[:, 9:10], AF.Exp)
            k.ts("dve", w[:, 11:12], w[:, 10:11], 1.0, None, ALU.add)
            k.recip(w[:, 12:13], w[:, 11:12])
            k.tt("dve", w[:, 13:14], w[:, 10:11], w[:, 12:13], ALU.mult)
            k.tt("dve", w[:, 14:15], w[:, 12:13], w[:, 6:7], ALU.mult)
            k.tt("dve", w[:, 15:16], w[:, 13:14], w[:, 6:7], ALU.mult)
            gg = g8.next()
            k.ts("dve", gg.v, w[:, 56:64], w[:, 14:15], None, ALU.mult)
            k.stt("dve", gg.v, w[:, 72:80], w[:, 15:16], gg.v, ALU.mult, ALU.add)
            k.mm(prk[:, 0:8], ustr.v, w[:, 40:48], start=True, stop=False)
            k.mm(prk[:, 0:8], ones_f.v, gcar.v, start=False, stop=True)
            k.tt("dve", w[:, 88:96], prk[:, 0:8], gofft.v, ALU.add)
            k.tt("dve", w[:, 88:96], w[:, 88:96], w[:, 40:48], ALU.mult)
            k.reduce("dve", w[:, 16:17], w[:, 88:96], ALU.add)
            k.tt("dve", gcar.v, gcar.v, w[:, 40:48], ALU.add)
            si = sloti.next()
            k.copy("dve", si.v, w[:, 16:17])
            k.dma("sp", slot[r0:r0 + 128, :], si.v)
            k.dma_gen("pool", lambda q, si=si: q.indirect_dma_start(
                out=xg.base[:, :], out_offset=bass.IndirectOffsetOnAxis(ap=si.base[:, :], axis=0),
                in_=h2b.base[:, :], in_offset=None, bounds_check=NSLOT - 1, oob_is_err=False),
                [si.v, h2b.v], [xg.v])
            k.dma_gen("pool", lambda q, si=si, gg=gg: q.indirect_dma_start(
                out=gate.base[:, :], out_offset=bass.IndirectOffsetOnAxis(ap=si.base[:, :], axis=0),
                in_=gg.base[:, :], in_offset=None, bounds_check=NSLOT - 1, oob_is_err=False),
                [si.v, gg.v], [gate.v])
    nc = k.finish()
    return nc, k


def build_e(NSL, SLT):
    NTL = NSL // SLT
    NSUB = SLT // 128
    k = KB()
    xg = k.dram("xg", [NSL, D], BF16, "ExternalInput")
    gate = k.dram("gate", [NSL, 8], F32, "ExternalInput")
    w1 = k.dram("w1", [EPG, D, DE], F32, "ExternalInput")
    w3 = k.dram("w3", [EPG, D, DE], F32, "ExternalInput")
    w2 = k.dram("w2", [EPG, DE, D], F32, "ExternalInput")
    yg = k.dram("yg", [NSL, D], F32, "ExternalOutput")

    ident = make_ident(k, BF16, "ident")
    xrow = Rot([k.sb([128, D], BF16, f"xrow{i}") for i in range(2)])
    XgT = k.sb([128, KC, SLT], BF16, "XgT")
    gt = k.sb([128, NSUB, 8], F32, "gt")
    acc = k.sb([128, NSUB, D], F32, "acc")
    w13 = Rot([k.sb([128, KC, 128], BF16, f"w13_{i}") for i in range(4)])
    w2b = Rot([k.sb([128, 3, D], BF16, f"w2b{i}") for i in range(2)])
    GT = Rot([k.sb([128, 3, SLT], BF16, f"GT{i}") for i in range(2)])
    sl = Rot([k.sb([128, SLT], F32, f"sl{i}") for i in range(2)])
    ptr = Rot([k.ps([128, 8, 128], BF16, f"ptr{i}") for i in range(2)])
    ph = Rot([k.ps([128, 512], F32, f"ph{i}") for i in range(4)])
    py = Rot([k.ps([128, 512], F32, f"py{i}") for i in range(2)])

    for t in range(NTL):
        s0 = t * SLT
        for s in range(NSUB):
            xr = xrow.next()
            k.dma("sp", xr.v, xg[s0 + s * 128:s0 + (s + 1) * 128, :])
            k.dma("sp", gt[:, s, :], gate[s0 + s * 128:s0 + (s + 1) * 128, :])
            for g8 in range(KC // 8):
                pt = ptr.next()
                for j in range(8):
                    kc = g8 * 8 + j
                    k.tr(pt[:, j, :], xr[:, kc * 128:(kc + 1) * 128], ident.v)
                k.copy("dve" if g8 % 2 else "act", XgT[:, g8 * 8:(g8 + 1) * 8, s * 128:(s + 1) * 128], pt.v)
        for e in range(EPG):
            w1v = w1.v[e].rearrange("(kc p) f -> p kc f", p=128)
            w3v = w3.v[e].rearrange("(kc p) f -> p kc f", p=128)
            G = GT.next()
            for fc in range(3):
                wa = w13.next()
                wb = w13.next()
                k.dma("pool", wa.v, w1v[:, :, fc * 128:(fc + 1) * 128])
                k.dma("pool", wb.v, w3v[:, :, fc * 128:(fc + 1) * 128])
                p1 = ph.next()
                p3 = ph.next()
                for kc in range(KC):
                    k.mm(p1[:, 0:SLT], wa[:, kc, :], XgT[:, kc, :], start=(kc == 0), stop=(kc == KC - 1))
                for kc in range(KC):
                    k.mm(p3[:, 0:SLT], wb[:, kc, :], XgT[:, kc, :], start=(kc == 0), stop=(kc == KC - 1))
                sv = sl.next()
                k.act(sv.v, p1[:, 0:SLT], AF.Silu)
                k.tt("dve", G[:, fc, :], sv.v, p3[:, 0:SLT], ALU.mult)
            w2t = w2b.next()
            k.dma("pool", w2t.v, w2.v[e].rearrange("(fc p) d -> p fc d", p=128))
            for s in range(NSUB):
                for dc in range(D // 512):
                    pp = py.next()
                    for fc in range(3):
                        k.mm(pp.v, G[:, fc, s * 128:(s + 1) * 128], w2t[:, fc, dc * 512:(dc + 1) * 512],
                             start=(fc == 0), stop=(fc == 2))
                    dst = acc[:, s, dc * 512:(dc + 1) * 512]
                    if e == 0:
                        k.ts("dve", dst, pp.v, gt[:, s, e:e + 1], None, ALU.mult)
                    else:
                        k.stt("dve", dst, pp.v, gt[:, s, e:e + 1], dst, ALU.mult, ALU.add)
        for s in range(NSUB):
            k.dma("sp", yg[s0 + s * 128:s0 + (s + 1) * 128, :], acc[:, s, :])
    nc = k.finish()
    return nc, k


def build_c(TPC, NSLOT, final):
    k = KB()
    x1 = k.dram("x1", [TPC, D], F32, "ExternalInput")
    slot = k.dram("slot", [TPC, 1], I32, "ExternalInput")
    yg = k.dram("yg", [NSLOT, D], F32, "ExternalInput")
    g_fin = k.dram("g_fin", [1, D], F32, "ExternalInput")
    out = k.dram("out", [TPC, D], F32, "ExternalOutput")
    epst = k.sb([128, 1], F32, "epst")
    k.memset("dve", epst.v, EPS)
    gft = k.sb([128, D], F32, "gft")
    k.dma("sp", gft.v, V(g_fin.base[0:1, :].partition_broadcast(128), g_fin))
    xt = Rot([k.sb([128, D], F32, f"xt{i}") for i in range(2)])
    yt = Rot([k.sb([128, D], F32, f"yt{i}") for i in range(2)])
    si = Rot([k.sb([128, 1], I32, f"si{i}") for i in range(2)])
    st = Rot([k.sb([128, 4], F32, f"st{i}") for i in range(2)])
    for s in range(TPC // 128):
        r0 = s * 128
        x_ = xt.next()
        y_ = yt.next()
        i_ = si.next()
        k.dma("sp", x_.v, x1[r0:r0 + 128, :])
        k.dma("sp", i_.v, slot[r0:r0 + 128, :])
        k.dma_gen("pool", lambda q, i_=i_, y_=y_: q.indirect_dma_start(
            out=y_.base[:, :], out_offset=None, in_=yg.base[:, :],
            in_offset=bass.IndirectOffsetOnAxis(ap=i_.base[:, :], axis=0),
            bounds_check=NSLOT - 1, oob_is_err=False), [i_.v, yg.v], [y_.v])
        k.tt("dve", x_.v, x_.v, y_.v, ALU.add)
        if final:
            w = st.next()
            k.act(y_.v, x_.v, AF.Square, accum_out=w[:, 0:1])
            k.act(w[:, 1:2], w[:, 0:1], AF.Sqrt, scale=1.0 / D, bias=epst[:, 0:1])
            k.recip(w[:, 2:3], w[:, 1:2])
            k.stt("dve", x_.v, x_.v, w[:, 2:3], gft.v, ALU.mult, ALU.mult)
        k.dma("sp", out[r0:r0 + 128, :], x_.v)
    nc = k.finish()
    return nc, k


_PROG = {}


def _prog(key, fn):
    if key not in _PROG:
        _PROG[key] = fn()[0]
    return _PROG[key]


def _run(nc, in_maps):
    res = run_bass_kernel_spmd(nc, in_maps, core_ids=list(range(len(in_maps))))
    return res.results


def _c(a):
    return np.ascontiguousarray(a)


def kernel(x, positions, norm_attn, w_in, q_norm, w_uq, kv_norm, w_ukv, lam_q1, lam_k1, lam_q2, lam_k2,
           subln, w_oa, w_ob, w_out, norm_ffn, w_router_g, b_router_g, w_router_e, b_router_e,
           w1, w3, w2, norm_final):
    import math
    x = np.asarray(x)
    S = x.shape[1]
    depth = np.asarray(w_in).shape[0]
    TPC = S // NCORE
    NB = S // 128
    CAP = max(128, ((TPC // NG) * 3 // 2 + 127) // 128 * 128)
    NSL = NCORE * CAP
    SLT = 512 if NSL % 512 == 0 else (CAP if CAP <= 512 else 384)
    pos = np.asarray(positions).astype(np.int32).reshape(1, S)
    posk = _c(pos[0].reshape(NB, 128).T)
    half = QKR // 2
    inv = (np.float32(10000.0) ** (-np.arange(half, dtype=np.float32) / np.float32(half))).astype(np.float32)
    invf = np.concatenate([inv, inv]).reshape(64, 1).astype(np.float32)
    goff = (np.arange(NG, dtype=np.float32) * CAP).reshape(1, NG)
    X = x.reshape(S, D).astype(np.float32)
    cs = [slice(c * TPC, (c + 1) * TPC) for c in range(NCORE)]

    p_t1 = _prog(("t1", TPC), lambda: build_t1(TPC))
    p_a = _prog(("a2", S), lambda: build_attn2(S))
    p_t2 = _prog(("t2", TPC, CAP), lambda: build_t2(TPC, CAP))
    p_e = _prog(("e", NSL, SLT), lambda: build_e(NSL, SLT))

    for l in range(depth):
        w_in_l = _c(np.asarray(w_in[l], dtype=np.float32))
        w_uq_l = _c(np.asarray(w_uq[l], dtype=np.float32))
        w_ukv_l = _c(np.asarray(w_ukv[l], dtype=np.float32))
        ins = [dict(x=_c(X[cs[c]]), pos=_c(pos[:, cs[c]]), invf=invf, g_attn=_c(np.asarray(norm_attn[l], np.float32)),
                    w_in=w_in_l, g_q=_c(np.asarray(q_norm[l], np.float32)), w_uq=w_uq_l,
                    g_kv=_c(np.asarray(kv_norm[l], np.float32)), w_ukv=w_ukv_l) for c in range(NCORE)]
        r1 = _run(p_t1, ins)
        del ins

        def cat(name, axis):
            return np.concatenate([np.asarray(r1[c][name]) for c in range(NCORE)], axis=axis)
        QN, QR, KN, KR = cat("o_qn", 2), cat("o_qr", 2), cat("o_kn", 2), cat("o_kr", 1)
        VA, QB, KB_, VB = cat("o_va", 0), cat("o_qb", 2), cat("o_kb", 2), cat("o_vb", 0)
        GA = [np.asarray(r1[c]["o_ga"]) for c in range(NCORE)]
        GB = [np.asarray(r1[c]["o_gb"]) for c in range(NCORE)]
        del r1
        lam_init = 0.8 - 0.6 * math.exp(-0.3 * l)
        lam = np.stack([np.asarray(lam_q1[l], np.float32), np.asarray(lam_k1[l], np.float32),
                        np.asarray(lam_q2[l], np.float32), np.asarray(lam_k2[l], np.float32)]).reshape(1, 4, 128)
        ins = []
        for c in range(NCORE):
            consts = np.zeros((128, 4), np.float32)
            consts[:, 0] = 2.0 ** (-8.0 * (c + 1) / HB)
            consts[:, 1] = lam_init
            consts[:, 2] = 1.0 - lam_init
            va_c = np.stack([_c(VA[:, (2 * c + h) * 128:(2 * c + h + 1) * 128].reshape(NB, 128, 128).transpose(1, 0, 2))
                             for h in range(2)])
            vb_c = _c(VB[:, c * 256:(c + 1) * 256].reshape(NB, 128, 256).transpose(1, 0, 2))
            ins.append(dict(qn=_c(QN[2 * c:2 * c + 2]), qr=_c(QR[2 * c:2 * c + 2]), kn=_c(KN[2 * c:2 * c + 2]), kr=_c(KR),
                            va=_c(va_c), qb=_c(QB[2 * c:2 * c + 2]), kb=_c(KB_[2 * c:2 * c + 2]), vb=vb_c,
                            pos=pos, posk=posk, lam=lam, subln=_c(np.asarray(subln[l], np.float32)), consts=consts))
        del QN, QR, KN, KR, VA, QB, KB_, VB
        ra = _run(p_a, ins)
        del ins
        OA = np.concatenate([np.asarray(ra[c]["oa"]) for c in range(NCORE)], axis=0)
        OB = np.concatenate([np.asarray(ra[c]["ob"]) for c in range(NCORE)], axis=0)
        del ra
        w_oa_l = _c(np.asarray(w_oa[l], np.float32))
        w_ob_l = _c(np.asarray(w_ob[l], np.float32))
        w_out_l = _c(np.asarray(w_out[l], np.float32))
        w_r = _c(np.concatenate([np.asarray(w_router_g[l], np.float32), np.asarray(w_router_e[l], np.float32)], axis=1))
        b_r = _c(np.concatenate([np.asarray(b_router_g[l], np.float32), np.asarray(b_router_e[l], np.float32)]).reshape(1, 72))
        ins = [dict(oa=_c(OA[:, :, cs[c]]), ob=_c(OB[:, :, cs[c]]), ga=GA[c], gb=GB[c], x=_c(X[cs[c]]),
                    w_oa=w_oa_l, w_ob=w_ob_l, w_out=w_out_l, g_ffn=_c(np.asarray(norm_ffn[l], np.float32).reshape(1, D)),
                    w_r=w_r, b_r=b_r, goff=goff) for c in range(NCORE)]
        del OA, OB, GA, GB
        r2 = _run(p_t2, ins)
        del ins
        ins = []
        for g in range(NG):
            xg_g = np.concatenate([np.asarray(r2[c]["xg"])[g * CAP:(g + 1) * CAP] for c in range(NCORE)], axis=0)
            gate_g = np.concatenate([np.asarray(r2[c]["gate"])[g * CAP:(g + 1) * CAP] for c in range(NCORE)], axis=0)
            ins.append(dict(xg=_c(xg_g), gate=_c(gate_g),
                            w1=_c(np.asarray(w1[l, g * EPG:(g + 1) * EPG], np.float32)),
                            w3=_c(np.asarray(w3[l, g * EPG:(g + 1) * EPG], np.float32)),
                            w2=_c(np.asarray(w2[l, g * EPG:(g + 1) * EPG], np.float32))))
        re_ = _run(p_e, ins)
        del ins
        final = (l == depth - 1)
        p_c = _prog(("c", TPC, NG * CAP, final), lambda: build_c(TPC, NG * CAP, final))
        ins = []
        for c in range(NCORE):
            yg_c = np.concatenate([np.asarray(re_[g]["yg"])[c * CAP:(c + 1) * CAP] for g in range(NG)], axis=0)
            ins.append(dict(x1=np.asarray(r2[c]["x1"]), slot=np.asarray(r2[c]["slot"]), yg=_c(yg_c),
                            g_fin=_c(np.asarray(norm_final, np.float32).reshape(1, D))))
        del re_, r2
        rc = _run(p_c, ins)
        del ins
        X = np.concatenate([np.asarray(rc[c]["out"]) for c in range(NCORE)], axis=0)
        del rc
    return X.reshape(1, S, D).astype(np.float32)
```
